# Optimizing a Trainium2 kernel written in Bass

```python
import math
import jax
import jax.numpy as jnp
from jax import lax
import numpy as np

D_MODEL = 1024
BATCH = 8
SEQ = 8192
DEPTH = 1

CHUNK = 64
GMLP_GROUPS = 8
GMLP_GROUP_DIM = 64
GMLP_WIDTH = GMLP_GROUPS * GMLP_GROUP_DIM
GMLP_BLOCK = 128
DIFF_HEADS = 8
DIFF_QK_DIM = 64
DIFF_V_DIM = 2 * DIFF_QK_DIM
DIFF_QK_WIDTH = DIFF_HEADS * 2 * DIFF_QK_DIM
DIFF_WIDTH = DIFF_HEADS * DIFF_V_DIM
Q_BLOCK = 128
N_GROUPS = 4
EXPERTS_PER_GROUP = 8
N_EXPERTS = N_GROUPS * EXPERTS_PER_GROUP
EXPERT_TOPK = 2
EXPERT_HIDDEN = 512
DISPATCH_BLOCK = 256
ALPHA = (2.0 * DEPTH) ** 0.25
BETA = (8.0 * DEPTH) ** -0.25
LN_EPS = 1e-5
RMS_EPS = 1e-5
OFF_U = 0
OFF_V = OFF_U + GMLP_WIDTH
OFF_Q = OFF_V + GMLP_WIDTH
OFF_K = OFF_Q + DIFF_QK_WIDTH
OFF_VAL = OFF_K + DIFF_QK_WIDTH
OFF_GA = OFF_VAL + DIFF_WIDTH
OFF_GB = OFF_GA + D_MODEL
IN_WIDTH = OFF_GB + D_MODEL

kernel_name = 'hybrid_gmlp_diffattn_hmoe_deepnorm'


def _layer_norm(x, g, b):
    xf = x.astype(jnp.float32)
    mu = jnp.mean(xf, axis=-1, keepdims=True)
    var = jnp.mean(jnp.square(xf - mu), axis=-1, keepdims=True)
    return ((xf - mu) * lax.rsqrt(var + LN_EPS)).astype(x.dtype) * g + b


def _spatial_gating(u, v, ln_g, ln_b, w_s, b_s):
    B, S, _ = u.shape
    n_blk = S // GMLP_BLOCK
    v = _layer_norm(v, ln_g, ln_b)
    v = v.reshape(B, n_blk, GMLP_BLOCK, GMLP_GROUPS, GMLP_GROUP_DIM)
    chunk_id = jnp.arange(GMLP_BLOCK) // CHUNK
    causal = chunk_id[:, None] >= chunk_id[None, :]
    w = jnp.where(causal[None], w_s, 0)
    mixed = jnp.einsum('gts,bnsgc->bntgc', w, v) + b_s.T[:, :, None]
    return u * mixed.reshape(B, S, GMLP_WIDTH)


def _diff_attention(q, k, v, lam, subln_g, lambda_init):
    B, S = q.shape[0], q.shape[1]
    n_blk = S // Q_BLOCK
    q = jnp.transpose(q * (DIFF_QK_DIM ** -0.5), (0, 2, 3, 1, 4))
    k = jnp.transpose(k, (0, 2, 3, 1, 4))
    v = jnp.transpose(v, (0, 2, 1, 3))
    q_blocks = jnp.moveaxis(q.reshape(B, DIFF_HEADS, 2, n_blk, Q_BLOCK, DIFF_QK_DIM), 3, 0)
    slopes = 2.0 ** (-8.0 * jnp.arange(1, DIFF_HEADS + 1, dtype=jnp.float32) / DIFF_HEADS)
    key_pos = jnp.arange(S)
    lam = lam.astype(jnp.float32)

    def one_block(args):
        q_blk, i = args
        q_pos = i * Q_BLOCK + jnp.arange(Q_BLOCK)
        visible = (key_pos[None, :] // CHUNK) <= (q_pos[:, None] // CHUNK)
        dist = jnp.abs(q_pos[:, None] - key_pos[None, :]).astype(jnp.float32)
        alibi = -slopes[:, None, None] * dist
        s = jnp.einsum('bhcqd,bhckd->bhcqk', q_blk, k).astype(jnp.float32) + alibi[None, :, None]
        s = jnp.where(visible, s, -jnp.inf)
        p = jax.nn.softmax(s, axis=-1)
        a = p[:, :, 0] - lam * p[:, :, 1]
        return jnp.einsum('bhqk,bhkd->bhqd', a.astype(v.dtype), v)

    out = lax.map(one_block, (q_blocks, jnp.arange(n_blk)))
    out = jnp.transpose(out, (1, 0, 3, 2, 4)).reshape(B, S, DIFF_HEADS, DIFF_V_DIM)
    of = out.astype(jnp.float32)
    of = of * lax.rsqrt(jnp.mean(jnp.square(of), axis=-1, keepdims=True) + RMS_EPS)
    out = of.astype(v.dtype) * subln_g * (1.0 - lambda_init)
    return out.reshape(B, S, DIFF_WIDTH)


def _token_mixer(h, layer, w_in, b_in, sg_ln_g, sg_ln_b, sg_w, sg_b, w_branch_a,
                 lam_q1, lam_k1, lam_q2, lam_k2, subln_g, w_branch_b, w_out):
    B, S, _ = h.shape

    def proj(lo, hi):
        return h @ w_in[:, lo:hi] + b_in[lo:hi]

    u = jax.nn.gelu(proj(OFF_U, OFF_V), approximate=False)
    v = jax.nn.gelu(proj(OFF_V, OFF_Q), approximate=False)
    y_a = _spatial_gating(u, v, sg_ln_g, sg_ln_b, sg_w, sg_b) @ w_branch_a
    q = proj(OFF_Q, OFF_K).reshape(B, S, DIFF_HEADS, 2, DIFF_QK_DIM)
    k = proj(OFF_K, OFF_VAL).reshape(B, S, DIFF_HEADS, 2, DIFF_QK_DIM)
    val = proj(OFF_VAL, OFF_GA).reshape(B, S, DIFF_HEADS, DIFF_V_DIM)
    lambda_init = 0.8 - 0.6 * math.exp(-0.3 * layer)
    lam = (jnp.exp(jnp.sum((lam_q1 * lam_k1).astype(jnp.float32)))
           - jnp.exp(jnp.sum((lam_q2 * lam_k2).astype(jnp.float32))) + lambda_init)
    y_b = _diff_attention(q, k, val, lam, subln_g, lambda_init) @ w_branch_b
    g_a = jax.nn.sigmoid(proj(OFF_GA, OFF_GB))
    g_b = jax.nn.sigmoid(proj(OFF_GB, IN_WIDTH))
    return (g_a * y_a + g_b * y_b) @ w_out


def _hier_moe(x, w_group, b_group, w_expert, b_expert, w_gate, w_up, w_down):
    B, S, D = x.shape
    t = x.reshape(B * S, D)
    T = B * S
    g_logits = (t @ w_group).astype(jnp.float32) + b_group.astype(jnp.float32)
    g_sel = jnp.argmax(g_logits, axis=-1)
    g_w = jnp.take_along_axis(jax.nn.softmax(g_logits, axis=-1), g_sel[:, None], axis=-1)
    e_logits = ((t @ w_expert).astype(jnp.float32) + b_expert.astype(jnp.float32))
    e_logits = e_logits.reshape(T, N_GROUPS, EXPERTS_PER_GROUP)
    e_logits = jnp.take_along_axis(e_logits, g_sel[:, None, None], axis=1)[:, 0]
    top_v, top_i = lax.top_k(e_logits, EXPERT_TOPK)
    top_w = jax.nn.softmax(top_v, axis=-1) * g_w
    expert_id = (g_sel[:, None] * EXPERTS_PER_GROUP + top_i).astype(jnp.int32)
    A = T * EXPERT_TOPK
    flat_e = expert_id.reshape(A)
    flat_w = top_w.reshape(A)
    flat_tok = jnp.arange(A, dtype=jnp.int32) // EXPERT_TOPK
    order = jnp.argsort(flat_e)
    sorted_e = flat_e[order]
    counts = jnp.zeros((N_EXPERTS,), jnp.int32).at[flat_e].add(1)
    padded = (counts + DISPATCH_BLOCK - 1) // DISPATCH_BLOCK * DISPATCH_BLOCK
    start = jnp.cumsum(counts) - counts
    pad_end = jnp.cumsum(padded)
    pad_start = pad_end - padded
    dest = pad_start[sorted_e] + jnp.arange(A, dtype=jnp.int32) - start[sorted_e]
    P = A + N_EXPERTS * DISPATCH_BLOCK
    n_blocks = P // DISPATCH_BLOCK
    slot_tok = jnp.zeros((P,), jnp.int32).at[dest].set(flat_tok[order])
    slot_w = jnp.zeros((P,), jnp.float32).at[dest].set(flat_w[order])
    block_start = jnp.arange(n_blocks, dtype=jnp.int32) * DISPATCH_BLOCK
    block_e = jnp.minimum(jnp.searchsorted(pad_end, block_start, side='right'), N_EXPERTS - 1)

    def expert_block(args):
        tok, wgt, e = args
        xb = t[tok]
        hb = jax.nn.silu(xb @ w_gate[e]) * (xb @ w_up[e])
        return (hb @ w_down[e]) * wgt[:, None].astype(xb.dtype)

    out = lax.map(expert_block, (slot_tok.reshape(n_blocks, DISPATCH_BLOCK),
                                 slot_w.reshape(n_blocks, DISPATCH_BLOCK), block_e))
    y = jnp.zeros_like(t).at[slot_tok].add(out.reshape(P, D))
    return y.reshape(B, S, D)


def setup_inputs(seed: int = 0) -> dict:
    key = jax.random.key(seed)
    ks = jax.random.split(key, 32)
    L = DEPTH

    def nrm(k, shape, scale):
        return jax.random.normal(k, shape, jnp.float32) * scale

    col_scale = np.ones((IN_WIDTH,), np.float32)
    col_scale[OFF_VAL:OFF_GA] = BETA
    return {
        'x': nrm(ks[0], (BATCH, SEQ, D_MODEL), 1.0),
        'w_in': nrm(ks[1], (L, D_MODEL, IN_WIDTH), D_MODEL ** -0.5) * jnp.asarray(col_scale),
        'b_in': nrm(ks[2], (L, IN_WIDTH), 0.01),
        'sg_ln_g': 1.0 + nrm(ks[3], (L, GMLP_WIDTH), 0.05),
        'sg_ln_b': nrm(ks[4], (L, GMLP_WIDTH), 0.01),
        'sg_w': nrm(ks[5], (L, GMLP_GROUPS, GMLP_BLOCK, GMLP_BLOCK), GMLP_BLOCK ** -0.5),
        'sg_b': 1.0 + nrm(ks[6], (L, GMLP_GROUPS, GMLP_BLOCK), 0.1),
        'w_branch_a': nrm(ks[7], (L, GMLP_WIDTH, D_MODEL), BETA * GMLP_WIDTH ** -0.5),
        'lam_q1': nrm(ks[8], (L, DIFF_QK_DIM), 0.1),
        'lam_k1': nrm(ks[9], (L, DIFF_QK_DIM), 0.1),
        'lam_q2': nrm(ks[10], (L, DIFF_QK_DIM), 0.1),
        'lam_k2': nrm(ks[11], (L, DIFF_QK_DIM), 0.1),
        'subln_g': 1.0 + nrm(ks[12], (L, DIFF_V_DIM), 0.05),
        'w_branch_b': nrm(ks[13], (L, DIFF_WIDTH, D_MODEL), BETA * DIFF_WIDTH ** -0.5),
        'w_out': nrm(ks[14], (L, D_MODEL, D_MODEL), BETA * D_MODEL ** -0.5),
        'ln1_g': 1.0 + nrm(ks[15], (L, D_MODEL), 0.05),
        'ln1_b': nrm(ks[16], (L, D_MODEL), 0.01),
        'w_group': nrm(ks[17], (L, D_MODEL, N_GROUPS), D_MODEL ** -0.5),
        'b_group': nrm(ks[18], (L, N_GROUPS), 0.01),
        'w_expert': nrm(ks[19], (L, D_MODEL, N_EXPERTS), D_MODEL ** -0.5),
        'b_expert': nrm(ks[20], (L, N_EXPERTS), 0.01),
        'w_gate': nrm(ks[21], (L, N_EXPERTS, D_MODEL, EXPERT_HIDDEN), D_MODEL ** -0.5),
        'w_up': nrm(ks[22], (L, N_EXPERTS, D_MODEL, EXPERT_HIDDEN), D_MODEL ** -0.5),
        'w_down': nrm(ks[23], (L, N_EXPERTS, EXPERT_HIDDEN, D_MODEL), BETA * EXPERT_HIDDEN ** -0.5),
        'ln2_g': 1.0 + nrm(ks[24], (L, D_MODEL), 0.05),
        'ln2_b': nrm(ks[25], (L, D_MODEL), 0.01),
    }


def reference(x, w_in, b_in, sg_ln_g, sg_ln_b, sg_w, sg_b, w_branch_a,
              lam_q1, lam_k1, lam_q2, lam_k2, subln_g, w_branch_b, w_out,
              ln1_g, ln1_b, w_group, b_group, w_expert, b_expert,
              w_gate, w_up, w_down, ln2_g, ln2_b):
    h = x
    for layer in range(DEPTH):
        mix = _token_mixer(h, layer, w_in[layer], b_in[layer], sg_ln_g[layer], sg_ln_b[layer],
                           sg_w[layer], sg_b[layer], w_branch_a[layer],
                           lam_q1[layer], lam_k1[layer], lam_q2[layer], lam_k2[layer],
                           subln_g[layer], w_branch_b[layer], w_out[layer])
        h = _layer_norm(ALPHA * h + mix, ln1_g[layer], ln1_b[layer])
        ffn = _hier_moe(h, w_group[layer], b_group[layer], w_expert[layer], b_expert[layer],
                        w_gate[layer], w_up[layer], w_down[layer])
        h = _layer_norm(ALPHA * h + ffn, ln2_g[layer], ln2_b[layer])
    return h
```

```python
import math
from contextlib import ExitStack

import ml_dtypes
import numpy as np

import concourse.bass as bass
import concourse.mybir as mybir
from concourse.bass_utils import run_bass_kernel_spmd

F32 = mybir.dt.float32
BF16 = mybir.dt.bfloat16
I32 = mybir.dt.int32
AF = mybir.ActivationFunctionType
ALU = mybir.AluOpType
AX = mybir.AxisListType

D = 1024
H = 8
NE = 32
EH = 512
IN_W = 6144
OFF_U, OFF_V, OFF_Q, OFF_K, OFF_VAL, OFF_GA, OFF_GB = 0, 512, 1024, 2048, 3072, 4096, 5120
ALPHA = 2.0 ** 0.25
LN_EPS = 1e-5
RMS_EPS = 1e-5
LAMBDA_INIT = 0.8 - 0.6 * math.exp(-0.3 * 0)
MBLK = 256


class Sem:
    def __init__(self, h):
        self.h = h
        self.v = 0


class Prog:
    ENG = {"pe": "tensor", "act": "scalar", "dve": "vector", "pool": "gpsimd", "sp": "sync"}

    def __init__(self, nc, es):
        self.nc = nc
        self.es = es
        self.q = {k: [] for k in self.ENG}
        self.esem = {k: self.sem("e_" + k) for k in self.ENG}
        self.nsem = 0

    def sem(self, name):
        sm = Sem(self.es.enter_context(self.nc.semaphore(name)))
        if not hasattr(self, "all_sems"):
            self.all_sems = []
        self.all_sems.append(sm)
        return sm

    def barrier(self):
        toks = [(sm, sm.v) for sm in self.all_sems if sm.v > 0]
        for k in self.ENG:
            self.q[k].append((lambda e: e.nop(), self._w(toks), None, 1))

    def op(self, eng, fn, waits=(), sig=True):
        if eng in ("dve", "pool"):
            sig = True
            if self.esem[eng].v:
                waits = list(waits) + [(self.esem[eng], self.esem[eng].v)]
        s = self.esem[eng] if sig else None
        tok = None
        if s is not None:
            s.v += 1
            tok = (s, s.v)
        self.q[eng].append((fn, self._w(waits), s, 1))
        return tok

    def dma(self, eng, fn, sem, waits=()):
        sem.v += 16
        self.q[eng].append((fn, self._w(waits), sem, 16))
        return (sem, sem.v)

    @staticmethod
    def _w(waits):
        best = {}
        for t in waits:
            if t is None:
                continue
            s, v = t
            if id(s) not in best or best[id(s)][1] < v:
                best[id(s)] = (s, v)
        return tuple(best.values())

    def replay(self):
        nc = self.nc
        with nc.Block() as block:
            for k, bn in self.ENG.items():
                items = self.q[k]
                own = self.esem[k]

                def body(eng, items=items, own=own):
                    for fn, waits, s, amt in items:
                        for (ws, wv) in waits:
                            eng.wait_ge(ws.h, wv)
                        ins = fn(eng)
                        if s is not None:
                            ins.then_inc(s.h, amt)

                getattr(block, bn)(body)
        self.q = {k: [] for k in self.ENG}


def _slopes():
    return [2.0 ** (-8.0 * (h + 1) / H) for h in range(H)]


def make_consts(S):
    bf = ml_dtypes.bfloat16
    c = {}
    c["ident_f"] = np.eye(128, dtype=np.float32)
    c["ident_b"] = np.eye(128, dtype=np.float32).astype(bf)
    tri = (np.arange(128)[:, None] < np.arange(128)[None, :]).astype(np.float32)
    c["triu_b"] = tri.astype(bf)
    c["ones_b"] = np.ones((128, 128), np.float32).astype(bf)
    pos = np.arange(S)
    blk = (pos // 128).astype(np.float32)
    r = (pos % 128).astype(np.float32)
    qa = np.zeros((H, 4, S), np.float32)
    ka = np.zeros((H, 4, S), np.float32)
    corr = np.zeros((H, 128, 128), np.float32)
    kr = np.arange(128)[:, None]
    qr = np.arange(128)[None, :]
    for h, sl in enumerate(_slopes()):
        qa[h, 0] = 1.0
        qa[h, 1] = 1.0
        qa[h, 2] = -sl * 128.0 * blk
        qa[h, 3] = -sl * r
        ka[h, 0] = sl * 128.0 * blk
        ka[h, 1] = sl * r
        ka[h, 2] = 1.0
        ka[h, 3] = 1.0
        cm = np.where(kr > qr, -2.0 * sl * (kr - qr), 0.0)
        cm = np.where((kr // 64) > (qr // 64), -30000.0, cm)
        corr[h] = cm
    c["qaug"] = qa.astype(bf)
    c["kaug"] = ka.astype(bf)
    c["corrT"] = np.ascontiguousarray(corr.transpose(1, 0, 2)).astype(bf)
    nb = S // 128
    c["tokidx"] = (np.arange(nb)[None, :] * 128 + np.arange(128)[:, None]).astype(np.int32)
    nblk = 2 * S // MBLK + NE
    c["blkstart"] = np.broadcast_to((np.arange(nblk) * float(MBLK))[None, :], (128, nblk)).astype(np.float32).copy()
    return c


CONST_DT = {"ident_f": F32, "ident_b": BF16, "triu_b": BF16, "ones_b": BF16, "qaug": BF16, "kaug": BF16,
            "corrT": BF16, "tokidx": I32, "blkstart": F32}

IN_SHAPES = {
    "w_in": [1, D, IN_W], "b_in": [1, IN_W], "sg_ln_g": [1, 512], "sg_ln_b": [1, 512],
    "sg_w": [1, 8, 128, 128], "sg_b": [1, 8, 128], "w_branch_a": [1, 512, D],
    "lam_q1": [1, 64], "lam_k1": [1, 64], "lam_q2": [1, 64], "lam_k2": [1, 64], "subln_g": [1, 128],
    "w_branch_b": [1, D, D], "w_out": [1, D, D], "ln1_g": [1, D], "ln1_b": [1, D],
    "w_group": [1, D, 4], "b_group": [1, 4], "w_expert": [1, D, NE], "b_expert": [1, NE],
    "w_gate": [1, NE, D, EH], "w_up": [1, NE, D, EH], "w_down": [1, NE, EH, D],
    "ln2_g": [1, D], "ln2_b": [1, D],
}


def build(S, debug=False, phases=(1, 2, 3, 4)):
    nc = bass.Bass("TRN2", target_bir_lowering=False)
    NT = S // 512
    NB = S // 128
    NBLK = 2 * S // MBLK + NE
    NSLOT = NBLK * MBLK
    NCH = NSLOT // 128
    consts = make_consts(S)

    A = {}
    A["x"] = nc.dram_tensor("x", [S, D], F32, kind="ExternalInput").ap()
    for k, shp in IN_SHAPES.items():
        A[k] = nc.dram_tensor(k, shp, F32, kind="ExternalInput").ap()
    for k, v in consts.items():
        A[k] = nc.dram_tensor(k, list(v.shape), CONST_DT[k], kind="ExternalInput").ap()
    out = nc.dram_tensor("out", [S, D], F32, kind="ExternalOutput").ap()
    skind = "ExternalOutput" if debug else "Internal"

    def scratch(name, shape, dt):
        return nc.dram_tensor(name, shape, dt, kind=skind).ap()

    qT_s = scratch("qT_s", [H, 2, 64, S], BF16)
    kT_s = scratch("kT_s", [H, 2, 64, S], BF16)
    v_s = scratch("v_s", [S, D], BF16)
    gbT_s = scratch("gbT_s", [D, S], F32)
    maT_s = scratch("maT_s", [D, S], F32)
    aT_s = scratch("aT_s", [D, S], BF16)
    h1_s = scratch("h1_s", [S, D], F32)
    h1b_s = scratch("h1b_s", [S, D], BF16)
    slot_s = scratch("slot_s", [128, NCH], I32)
    out_s = scratch("out_s", [NSLOT, D], F32)
    wgu_r = nc.dram_tensor("wgu_r", [NE * 128, 2 * 8 * EH], BF16).ap()
    wd_r = nc.dram_tensor("wd_r", [NE * 128, 4 * D], BF16).ap()
    dbg_s = scratch("dbg_s", [128, 4096], F32) if debug else None

    with ExitStack() as es:
        P = Prog(nc, es)

        def sb(name, shape, dt, stack=es):
            return stack.enter_context(nc.sbuf_tensor("sb_" + name, shape, dt))

        bank = [es.enter_context(nc.psum_tensor(f"bank{i}", [128, 512], F32)) for i in range(7)]
        bank.append(es.enter_context(nc.psum_tensor("bank7b", [128, 1024], BF16)))
        bank_free = [None] * 8

        s_const = P.sem("const")
        s_scr = P.sem("scr")

        ident_f = sb("ident_f", [128, 128], F32)
        ident_b = sb("ident_b", [128, 128], BF16)
        P.dma("sp", lambda e: e.dma_start(out=ident_f[:], in_=A["ident_f"]), s_const)
        t_const = P.dma("sp", lambda e: e.dma_start(out=ident_b[:], in_=A["ident_b"]), s_const)

        def dbg(name, tile, tok):
            if not debug:
                return
            shp = list(tile.shape)
            d = nc.dram_tensor("dbg_" + name, shp, tile.dtype, kind="ExternalOutput").ap()
            P.dma("sp", lambda e: e.dma_start(out=d, in_=tile[:]), s_scr, waits=[tok])

        if 1 in phases:
            t_ph1 = phase1(nc, P, A, S, NT, sb, bank, bank_free, ident_f, t_const, s_scr,
                           qT_s, kT_s, v_s, gbT_s, maT_s, dbg)
        NBk = S // 128
        logits_all = sb("logits_all", [128, NBk, 36], F32)
        s_prep = P.sem("wprep")
        prep_fns = []
        if 4 in phases:
            for ex in range(NE):
                for gu, nm in enumerate(("w_gate", "w_up")):
                    prep_fns.append(lambda ex=ex, gu=gu, nm=nm: P.dma("pool", lambda e: e.dma_start(
                        out=wgu_r[ex * 128:(ex + 1) * 128, gu * 8 * EH:(gu + 1) * 8 * EH].rearrange("p (kc f) -> p kc f", kc=8),
                        in_=A[nm][0, ex].rearrange("(kc p) f -> p kc f", p=128)), s_prep))
                prep_fns.append(lambda ex=ex: P.dma("pool", lambda e: e.dma_start(
                    out=wd_r[ex * 128:(ex + 1) * 128, :].rearrange("p (kc f) -> p kc f", kc=4),
                    in_=A["w_down"][0, ex].rearrange("(kc p) f -> p kc f", p=128)), s_prep))
        if 2 not in phases:
            for f in prep_fns:
                f()
            prep_fns = []
        if 2 in phases:
            phase2(nc, P, A, S, bank, bank_free, ident_f, ident_b, t_const, s_scr, qT_s, kT_s, v_s, aT_s, dbg, prep_fns)
        t_prep = (s_prep, s_prep.v)
        if 3 in phases:
            phase3(nc, P, A, S, bank, bank_free, ident_f, t_const, s_scr, gbT_s, maT_s, aT_s, h1_s, h1b_s, logits_all, dbg)
        if 4 in phases:
            phase4(nc, P, A, S, bank, bank_free, ident_b, t_const, s_scr, h1_s, h1b_s, slot_s, out_s, out, logits_all, dbg,
                   wgu_r, wd_r, t_prep)
        P.op("sp", lambda e: e.nop(), waits=[(s_scr, s_scr.v)], sig=False)
        P.replay()
    return nc


def phase1(nc, P, A, S, NT, sb_outer, bank, bank_free, ident_f, t_const, s_scr,
           qT_s, kT_s, v_s, gbT_s, maT_s, dbg):
    w_in = A["w_in"]
    with ExitStack() as ps:
        def sb(name, shape, dt):
            return ps.enter_context(nc.sbuf_tensor("sb_" + name, shape, dt))

        s_w = P.sem("p1w")
        braw = sb("braw", [48, 128], F32)
        bias_fm = sb("bias_fm", [128, 48], F32)
        bq8 = sb("bq8", [128, 8], F32)
        bV = sb("bV", [128, 512], F32)
        bVAL = sb("bVAL", [128, 1024], F32)
        lng = sb("sglng", [128, 512], F32)
        lnb = sb("sglnb", [128, 512], F32)
        bsT = sb("bsT", [128, 4, 128], F32)
        wsraw = sb("wsraw", [128, 8, 128], F32)
        wsT = sb("wsT", [128, 8, 128], BF16)
        wba = sb("wba", [128, 4, D], BF16)
        P.dma("sp", lambda e: e.dma_start(out=braw[:], in_=A["b_in"][0].rearrange("(c p) -> c p", p=128)), s_w)
        P.dma("sp", lambda e: e.dma_start(out=bV[:], in_=A["b_in"][0, OFF_V:OFF_Q].partition_broadcast(128)), s_w)
        P.dma("sp", lambda e: e.dma_start(out=bVAL[:], in_=A["b_in"][0, OFF_VAL:OFF_GA].partition_broadcast(128)), s_w)
        P.dma("sp", lambda e: e.dma_start(out=lng[:], in_=A["sg_ln_g"][0].partition_broadcast(128)), s_w)
        P.dma("sp", lambda e: e.dma_start(out=lnb[:], in_=A["sg_ln_b"][0].partition_broadcast(128)), s_w)
        for g in range(8):
            P.dma("sp", lambda e, g=g: e.dma_start(out=bsT[(g % 2) * 64:(g % 2) * 64 + 64, g // 2, :],
                                                  in_=A["sg_b"][0, g].partition_broadcast(64)), s_w)
        P.dma("sp", lambda e: e.dma_start(out=wsraw[:], in_=A["sg_w"][0].rearrange("g t s -> t g s")), s_w)
        s_wba = P.sem("p1wba")
        for kc in range(4):
            P.dma("pool", lambda e, kc=kc: e.dma_start(out=wba[:, kc, :], in_=A["w_branch_a"][0, kc * 128:(kc + 1) * 128, :]), s_wba)
        t_w = (s_w, s_w.v)
        t_wba = (s_wba, s_wba.v)
        wB = sb("wB", [128, 8, 4096], BF16)
        s_wB = P.sem("wB")

        t = P.op("pe", lambda e: e.transpose(out=bank[0][:, 0:48], in_=braw[:], identity=ident_f[0:48, 0:48]),
                 waits=[t_w, t_const])
        t = P.op("dve", lambda e: e.tensor_copy(out=bias_fm[:], in_=bank[0][:, 0:48]), waits=[t])
        t_bias = P.op("dve", lambda e: e.tensor_scalar(out=bq8[:], in0=bias_fm[:, 8:16], scalar1=0.125, scalar2=None,
                                                       op0=ALU.mult), waits=[t])
        tt = []
        for g in range(8):
            tt.append(P.op("pe", lambda e, g=g: e.transpose(out=bank[1 + g // 4][:, (g % 4) * 128:(g % 4) * 128 + 128],
                                                            in_=wsraw[:, g, :], identity=ident_f[:]), waits=[t_w]))
        t = P.op("dve", lambda e: e.tensor_copy(out=wsT[:, 0:4, :], in_=bank[1][:].rearrange("p (g t) -> p g t", g=4)),
                 waits=[tt[3]])
        t = P.op("dve", lambda e: e.tensor_copy(out=wsT[:, 4:8, :], in_=bank[2][:].rearrange("p (g t) -> p g t", g=4)),
                 waits=[tt[7]])
        t_ws = P.op("dve", lambda e: e.memset(wsT[64:128, :, 0:64], 0.0))
        for b in range(3):
            bank_free[b] = t_ws

        with ExitStack() as pa:
            def sba(name, shape, dt):
                return pa.enter_context(nc.sbuf_tensor("sb_" + name, shape, dt))
            wA = sba("wA", [128, 8, 2048], BF16)
            s_wA = P.sem("wA")
            for kc in range(8):
                for (dst, src, n) in ((0, OFF_U, 1024), (1024, OFF_GA, 1024)):
                    P.dma("pool", lambda e, kc=kc, dst=dst, src=src, n=n: e.dma_start(
                        out=wA[:, kc, dst:dst + n], in_=w_in[0, kc * 128:(kc + 1) * 128, src:src + n]), s_wA)
            t_wA = (s_wA, s_wA.v)
            for kc in range(8):
                for (dst, src) in ((0, OFF_Q), (1024, OFF_K), (2048, OFF_VAL), (3072, OFF_GB)):
                    P.dma("pool", lambda e, kc=kc, dst=dst, src=src: e.dma_start(
                        out=wB[:, kc, dst:dst + 1024], in_=w_in[0, kc * 128:(kc + 1) * 128, src:src + 1024]), s_wB)
            xt = [sba("xtA0", [128, 4, D], F32)] * 2
            xt_shared = True
            xT = [sba(f"xTA{i}", [128, 8, 512], BF16) for i in range(2)]
            s_xt = [P.sem("xtA0")] * 2
            uT = sba("uT", [128, 4, 512], BF16)
            sguT = sba("sguT", [128, 4, 512], BF16)
            ga = sba("ga", [128, 8, 512], F32)
            ma_st = [sba(f"ma_st{i}", [128, 512], F32) for i in range(2)]
            s_ma = [P.sem(f"ma{i}") for i in range(2)]
            vg0 = [sba(f"vg0_{i}", [128, 512], F32) for i in range(4)]
            vg0_free = [None] * 4
            neghalfA = sba("neghalfA", [128, 1], F32)
            P.op("dve", lambda e: e.memset(neghalfA[:], -0.5))
            vg1 = sba("vg1", [128, 512], F32)
            vg2 = sba("vg2", [128, 512], F32)
            vln = [sba(f"vln{i}", [128, 512], BF16) for i in range(4)]
            mixs = sba("mixs", [128, 512], F32)
            st6 = sba("st6", [128, 6], F32)
            mv = sba("mv", [128, 2], F32)
            rs = sba("rs", [128, 2], F32)

            xt_free = [None, None]
            xT_free = [None, None]
            ma_free = [None, None]
            vln_free = [None] * 4
            uT_free = None
            sgu_free = None
            ga_free = None
            vg_free = None
            mixs_free = None
            acc_rr = [0]

            def acc_bank():
                b = 2 + acc_rr[0] % 3
                acc_rr[0] += 1
                return b

            for tt_i in range(NT):
                tok0 = tt_i * 512
                xb = tt_i % 2
                if tt_i == 0:
                    t_ld_next = P.dma("sp", lambda e: e.dma_start(
                        out=xt[0][:], in_=A["x"][0:512, :].rearrange("(b p) d -> p b d", p=128)), s_xt[0])
                t_ld = t_ld_next
                t_cp = None
                for kc in range(8):
                    pb = kc % 2
                    for tb in range(4):
                        t = P.op("pe", lambda e, pb=pb, tb=tb, kc=kc, xb=xb: e.transpose(
                            out=bank[pb][:, tb * 128:(tb + 1) * 128], in_=xt[xb][:, tb, kc * 128:(kc + 1) * 128],
                            identity=ident_f[:]), waits=[t_ld, bank_free[pb], t_const] if tb == 0 else [])
                    if kc % 2 == 0:
                        t_cp = P.op("act", lambda e, pb=pb, kc=kc, xb=xb: e.copy(out=xT[xb][:, kc, :], in_=bank[pb][:]),
                                    waits=[t, xT_free[xb]])
                    else:
                        t_cp = P.op("dve", lambda e, pb=pb, kc=kc, xb=xb: e.tensor_copy(out=xT[xb][:, kc, :], in_=bank[pb][:]),
                                    waits=[t, xT_free[xb]])
                    bank_free[pb] = t_cp
                    if kc == 6:
                        t_cp6 = t_cp
                xt_free[0] = t
                xt_free[1] = t
                t_xT = [t_cp6, t_cp]
                if tt_i + 1 < NT:
                    t_ld_next = P.dma("sp", lambda e, tok1=tok0 + 512: e.dma_start(
                        out=xt[0][:], in_=A["x"][tok1:tok1 + 512, :].rearrange("(b p) d -> p b d", p=128)),
                        s_xt[0], waits=[t])

                def fm_group(col0, b, first_waits):
                    tk = None
                    for kc in range(8):
                        tk = P.op("pe", lambda e, kc=kc, col0=col0, b=b, xb=xb: e.matmul(
                            bank[b][:], wA[:, kc, col0:col0 + 128], xT[xb][:, kc, :], start=(kc == 0), stop=(kc == 7)),
                            waits=first_waits if kc == 0 else [], sig=(kc == 7))
                    return tk

                for ch in range(4):
                    b = acc_bank()
                    t = fm_group(ch * 128, b, [t_wA, bank_free[b]] + t_xT)
                    t = P.op("act", lambda e, b=b, ch=ch: e.activation(out=uT[:, ch, :], in_=bank[b][:], func=AF.Gelu,
                                                                       bias=bias_fm[:, ch:ch + 1]),
                             waits=[t, t_bias, uT_free])
                    bank_free[b] = t
                t_uT = t
                t_vln = [None] * 4
                for tb in range(4):
                    b = acc_bank()
                    tk = None
                    for kc in range(8):
                        tk = P.op("pe", lambda e, kc=kc, b=b, tb=tb, xb=xb: e.matmul(
                            bank[b][:], xT[xb][:, kc, tb * 128:(tb + 1) * 128], wA[:, kc, 512:1024],
                            start=(kc == 0), stop=(kc == 7)),
                            waits=[t_wA, bank_free[b]] + t_xT if kc == 0 else [], sig=(kc == 7))
                    t = P.op("dve", lambda e, b=b, tb=tb: e.tensor_tensor(out=vg0[tb][:], in0=bank[b][:], in1=bV[:], op=ALU.add),
                             waits=[tk, t_w, vg0_free[tb]])
                    bank_free[b] = t
                    t = P.op("act", lambda e, tb=tb: e.activation(out=vg1[:], in_=vg0[tb][:], func=AF.Gelu), waits=[t, vg_free])
                    vg0_free[tb] = t
                    t = P.op("dve", lambda e: e.bn_stats(out=st6[:], in_=vg1[:]), waits=[t])
                    t = P.op("dve", lambda e: e.bn_aggr(out=mv[:], in_=st6[:]), waits=[t])
                    t = P.op("dve", lambda e: e.tensor_scalar(out=rs[:, 0:1], in0=mv[:, 1:2], scalar1=LN_EPS, scalar2=None,
                                                              op0=ALU.add), waits=[t])
                    t = P.op("pool", lambda e: e.tensor_tensor(out=rs[:, 1:2], in0=rs[:, 0:1], in1=neghalfA[:], op=ALU.pow), waits=[t])
                    t = P.op("dve", lambda e: e.tensor_scalar(out=vg2[:], in0=vg1[:], scalar1=mv[:, 0:1], scalar2=rs[:, 1:2],
                                                              op0=ALU.subtract, op1=ALU.mult), waits=[t])
                    vg_free = t
                    t = P.op("pool", lambda e: e.tensor_tensor(out=vg2[:], in0=vg2[:], in1=lng[:], op=ALU.mult), waits=[t])
                    t = P.op("pool", lambda e, tb=tb: e.tensor_tensor(out=vln[tb][:], in0=vg2[:], in1=lnb[:], op=ALU.add),
                             waits=[t, vln_free[tb]])
                    t_vln[tb] = t
                for m in range(8):
                    b = acc_bank()
                    t = fm_group(1024 + m * 128, b, [t_wA, bank_free[b]] + t_xT)
                    if m == 7:
                        xT_free[xb] = t
                    t = P.op("act", lambda e, b=b, m=m: e.activation(out=ga[:, m, :], in_=bank[b][:], func=AF.Sigmoid,
                                                                     bias=bias_fm[:, 32 + m:33 + m]),
                             waits=[t, t_bias, ga_free])
                    bank_free[b] = t
                t_ga = t
                for tb in range(4):
                    for gp in range(4):
                        P.op("pe", lambda e, tb=tb, gp=gp: e.matmul(
                            bank[5][:, gp * 128:(gp + 1) * 128], vln[tb][:, gp * 128:(gp + 1) * 128], wsT[:, 2 * gp, :],
                            start=True, stop=True), waits=[t_vln[tb], t_ws, bank_free[5]] if gp == 0 else [], sig=False)
                    for gp in range(4):
                        tm = P.op("pe", lambda e, tb=tb, gp=gp: e.matmul(
                            bank[6][:, gp * 128:(gp + 1) * 128], vln[tb][:, gp * 128:(gp + 1) * 128], wsT[:, 2 * gp + 1, :],
                            start=True, stop=True), waits=[bank_free[6]] if gp == 0 else [], sig=(gp == 3))
                    vln_free[tb] = tm
                    t = P.op("dve", lambda e: e.tensor_tensor(out=mixs[0:64, :], in0=bank[5][0:64, :],
                                                              in1=bsT[0:64, :, :].rearrange("p g t -> p (g t)"), op=ALU.add),
                             waits=[tm, mixs_free])
                    bank_free[5] = t
                    t = P.op("dve", lambda e: e.tensor_tensor(out=mixs[64:128, :], in0=bank[6][64:128, :],
                                                              in1=bsT[64:128, :, :].rearrange("p g t -> p (g t)"), op=ALU.add),
                             waits=[t])
                    bank_free[6] = t
                    t = P.op("dve", lambda e, tb=tb: e.tensor_tensor(
                        out=sguT[:, :, tb * 128:(tb + 1) * 128], in0=mixs[:].rearrange("p (g t) -> p g t", g=4),
                        in1=uT[:, :, tb * 128:(tb + 1) * 128], op=ALU.mult), waits=[t, t_uT, sgu_free])
                    mixs_free = t
                t_sgu = t
                uT_free = t
                for m in range(8):
                    b = acc_bank()
                    tk = None
                    for kc in range(4):
                        tk = P.op("pe", lambda e, kc=kc, b=b, m=m: e.matmul(
                            bank[b][:], wba[:, kc, m * 128:(m + 1) * 128], sguT[:, kc, :], start=(kc == 0), stop=(kc == 3)),
                            waits=[t_sgu, t_wba, bank_free[b]] if kc == 0 else [], sig=(kc == 3))
                    r = m % 2
                    t = P.op("dve", lambda e, b=b, m=m, r=r: e.tensor_tensor(out=ma_st[r][:], in0=bank[b][:], in1=ga[:, m, :],
                                                                            op=ALU.mult), waits=[tk, t_ga, ma_free[r]])
                    bank_free[b] = t
                    ma_free[r] = P.dma("sp", lambda e, m=m, r=r, tok0=tok0: e.dma_start(
                        out=maT_s[m * 128:(m + 1) * 128, tok0:tok0 + 512], in_=ma_st[r][:]), s_ma[r], waits=[t])
                    if m == 7:
                        sgu_free = tk
                ga_free = t
            t_endA = [ma_free[0], ma_free[1], t]
            for nm, tl in (("uT", uT), ("vg0", vg0[3]), ("vg1", vg1), ("vg2", vg2), ("mv", mv), ("rs", rs), ("vln1", vln[1]),
                           ("mixs", mixs), ("sguT", sguT), ("ga", ga), ("wsT", wsT), ("bsT", bsT), ("bias_fm", bias_fm), ("bV", bV), ("lng", lng), ("xTA1", xT[1])):
                dbg(nm, tl, t)
            if s_scr.v:
                t_endA.append((s_scr, s_scr.v))
            P.barrier()
            P.replay()
        with ExitStack() as pb_:
            def sbb(name, shape, dt):
                return pb_.enter_context(nc.sbuf_tensor("sb_" + name, shape, dt))
            t_wB = (s_wB, s_wB.v)
            xt = [sbb(f"xtB{i}", [128, 4, D], F32) for i in range(2)]
            xT = [sbb(f"xTB{i}", [128, 8, 512], BF16) for i in range(2)]
            s_xt = [P.sem(f"xtB{i}") for i in range(2)]
            qk_st = [sbb(f"qk_st{i}", [128, 512], BF16) for i in range(3)]
            s_qk = [P.sem(f"qk{i}") for i in range(3)]
            gb_st = [sbb(f"gb_st{i}", [128, 512], F32) for i in range(2)]
            s_gb = [P.sem(f"gb{i}") for i in range(2)]
            val_st = [sbb(f"val_st{i}", [128, 1024], BF16) for i in range(2)]
            s_val = [P.sem(f"val{i}") for i in range(2)]
            xt_free = [None, None]
            xT_free = [None, None]
            qk_free = [None] * 3
            gb_free = [None] * 2
            val_free = [None] * 2
            acc_rr = [0]
            qk_rr = 0

            def acc_bank():
                b = 2 + acc_rr[0] % 4
                acc_rr[0] += 1
                return b

            for tt_i in range(NT):
                tok0 = tt_i * 512
                xb = tt_i % 2
                if tt_i == 0:
                    t_ld_nextB = P.dma("sp", lambda e: e.dma_start(
                        out=xt[0][:], in_=A["x"][0:512, :].rearrange("(b p) d -> p b d", p=128)), s_xt[0], waits=t_endA)
                t_ld = t_ld_nextB
                if tt_i + 1 < NT:
                    xn = (tt_i + 1) % 2
                    t_ld_nextB = P.dma("sp", lambda e, xn=xn, tok1=tok0 + 512: e.dma_start(
                        out=xt[xn][:], in_=A["x"][tok1:tok1 + 512, :].rearrange("(b p) d -> p b d", p=128)),
                        s_xt[xn], waits=[xt_free[xn]] + t_endA)
                t_cp = None
                for kc in range(8):
                    pb = kc % 2
                    for tb in range(4):
                        t = P.op("pe", lambda e, pb=pb, tb=tb, kc=kc, xb=xb: e.transpose(
                            out=bank[pb][:, tb * 128:(tb + 1) * 128], in_=xt[xb][:, tb, kc * 128:(kc + 1) * 128],
                            identity=ident_f[:]), waits=[t_ld, bank_free[pb]] if tb == 0 else [])
                    if kc % 2 == 0:
                        t_cp = P.op("act", lambda e, pb=pb, kc=kc, xb=xb: e.copy(out=xT[xb][:, kc, :], in_=bank[pb][:]),
                                    waits=[t, xT_free[xb]])
                    else:
                        t_cp = P.op("dve", lambda e, pb=pb, kc=kc, xb=xb: e.tensor_copy(out=xT[xb][:, kc, :], in_=bank[pb][:]),
                                    waits=[t, xT_free[xb]])
                    bank_free[pb] = t_cp
                    if kc == 6:
                        t_cp6 = t_cp
                xt_free[xb] = t
                t_xT = [t_cp6, t_cp]

                def fm_group(col0, b, first_waits):
                    tk = None
                    for kc in range(8):
                        tk = P.op("pe", lambda e, kc=kc, col0=col0, b=b, xb=xb: e.matmul(
                            bank[b][:], wB[:, kc, col0:col0 + 128], xT[xb][:, kc, :], start=(kc == 0), stop=(kc == 7)),
                            waits=first_waits if kc == 0 else [], sig=(kc == 7))
                    return tk

                for ch in range(16):
                    isq = ch < 8
                    h = ch % 8
                    b = acc_bank()
                    t = fm_group(ch * 128, b, [t_wB, bank_free[b]] + t_xT)
                    r = qk_rr % 3
                    qk_rr += 1
                    if isq:
                        t = P.op("dve", lambda e, b=b, h=h, r=r: e.tensor_scalar(
                            out=qk_st[r][:], in0=bank[b][:], scalar1=0.125, scalar2=bq8[:, h:h + 1],
                            op0=ALU.mult, op1=ALU.add), waits=[t, t_bias, qk_free[r]])
                    else:
                        t = P.op("dve", lambda e, b=b, h=h, r=r: e.tensor_scalar(
                            out=qk_st[r][:], in0=bank[b][:], scalar1=bias_fm[:, 16 + h:17 + h], scalar2=None,
                            op0=ALU.add), waits=[t, t_bias, qk_free[r]])
                    bank_free[b] = t
                    dst = qT_s if isq else kT_s
                    for c in range(2):
                        qk_free[r] = P.dma("sp", lambda e, dst=dst, h=h, c=c, r=r, tok0=tok0: e.dma_start(
                            out=dst[h, c, :, tok0:tok0 + 512], in_=qk_st[r][c * 64:(c + 1) * 64, :]), s_qk[r], waits=[t])
                for tb in range(4):
                    r = tb % 2
                    for half in range(2):
                        b = acc_bank()
                        tk = None
                        for kc in range(8):
                            tk = P.op("pe", lambda e, kc=kc, b=b, tb=tb, half=half, xb=xb: e.matmul(
                                bank[b][:], xT[xb][:, kc, tb * 128:(tb + 1) * 128],
                                wB[:, kc, 2048 + half * 512:2048 + (half + 1) * 512], start=(kc == 0), stop=(kc == 7)),
                                waits=[t_wB, bank_free[b]] + t_xT if kc == 0 else [], sig=(kc == 7))
                        t = P.op("dve", lambda e, b=b, r=r, half=half: e.tensor_tensor(
                            out=val_st[r][:, half * 512:(half + 1) * 512], in0=bank[b][:],
                            in1=bVAL[:, half * 512:(half + 1) * 512], op=ALU.add), waits=[tk, t_w, val_free[r]])
                        bank_free[b] = t
                    val_free[r] = P.dma("sp", lambda e, r=r, tb=tb, tok0=tok0: e.dma_start(
                        out=v_s[tok0 + tb * 128:tok0 + (tb + 1) * 128, :], in_=val_st[r][:]), s_val[r], waits=[t])
                for m in range(8):
                    b = acc_bank()
                    t = fm_group(3072 + m * 128, b, [t_wB, bank_free[b]] + t_xT)
                    if m == 7:
                        xT_free[xb] = t
                    r = m % 2
                    t = P.op("act", lambda e, b=b, m=m, r=r: e.activation(out=gb_st[r][:], in_=bank[b][:], func=AF.Sigmoid,
                                                                         bias=bias_fm[:, 40 + m:41 + m]),
                             waits=[t, t_bias, gb_free[r]])
                    bank_free[b] = t
                    gb_free[r] = P.dma("sp", lambda e, m=m, r=r, tok0=tok0: e.dma_start(
                        out=gbT_s[m * 128:(m + 1) * 128, tok0:tok0 + 512], in_=gb_st[r][:]), s_gb[r], waits=[t])
            t_endB = [x for x in (qk_free + gb_free + val_free) if x is not None] + [t]
            P.barrier()
            P.replay()
    return t_endA + t_endB


def phase2(nc, P, A, S, bank, bank_free, ident_f, ident_b, t_const, s_scr, qT_s, kT_s, v_s, aT_s, dbg, prep_fns=()):
    NB = S // 128
    NQP = S // 256
    t_prev = (s_scr, s_scr.v)
    with ExitStack() as ps:
        def sb(name, shape, dt):
            return ps.enter_context(nc.sbuf_tensor("sb_" + name, shape, dt))
        qa2 = [[sb(f"qa{p}{c}", [68, S], BF16) for c in range(2)] for p in range(2)]
        ka2 = [[sb(f"ka{p}{c}", [68, S], BF16) for c in range(2)] for p in range(2)]
        Vh2 = [sb(f"Vh{p}", [128, NB, 129], BF16) for p in range(2)]
        Osb = sb("Osb", [128, 2, 2, 129], F32)
        rc4 = sb("rc4", [128, 2, 2], F32)
        r1l = sb("r1l", [128, 2], F32)
        ssq = sb("ssq", [128, 2], F32)
        rstd2 = sb("rstd2", [128, 2], F32)
        corrT = sb("corrT", [128, H, 128], BF16)
        lamt = sb("lamt", [128, 4, 64], F32)
        lamw = sb("lamw", [128, 2, 64], F32)
        lams = sb("lams", [128, 4], F32)
        neglam = sb("neglam", [128, 1], F32)
        neghalf = sb("neghalf", [128, 1], F32)
        neghalf2 = sb("neghalf2", [128, 2], F32)
        PT = [sb(f"PT{i}", [128, 512], BF16) for i in range(2)]
        rcp = [sb(f"rcp{j}", [128, 4], F32) for j in range(2)]
        Abuf = [sb(f"Abuf{j}", [128, 128], F32) for j in range(2)]
        Dbuf = [sb(f"Dbuf{j}", [128, 128], F32) for j in range(2)]
        sq = sb("sqjunk", [128, 128], F32)
        On = [[sb(f"On{j}{p}", [128, 128], BF16) for p in range(2)] for j in range(2)]
        aT_st = [sb(f"aT_st{i}", [128, 256], BF16) for i in range(2)]
        s_c2 = P.sem("p2c")
        s_ld2 = [P.sem(f"p2ld{p}") for p in range(2)]
        s_ast = [P.sem(f"p2a{i}") for i in range(2)]

        P.dma("sp", lambda e: e.dma_start(out=corrT[:], in_=A["corrT"]), s_c2, waits=[t_prev])
        for i, nm in enumerate(("lam_q1", "lam_k1", "lam_q2", "lam_k2")):
            P.dma("sp", lambda e, i=i, nm=nm: e.dma_start(out=lamt[:, i, :], in_=A[nm][0].partition_broadcast(128)), s_c2)
        t_c2 = (s_c2, s_c2.v)
        P.op("dve", lambda e: e.memset(Vh2[0][:, :, 128:129], 1.0), waits=[t_prev])
        t = P.op("dve", lambda e: e.memset(Vh2[1][:, :, 128:129], 1.0))
        t_ones = t
        P.op("dve", lambda e: e.memset(neghalf[:], -0.5), sig=False)
        P.op("dve", lambda e: e.memset(neghalf2[:], -0.5), sig=False)
        P.op("dve", lambda e: e.tensor_tensor(out=lamw[:, 0, :], in0=lamt[:, 0, :], in1=lamt[:, 1, :], op=ALU.mult),
             waits=[t_c2], sig=False)
        t = P.op("dve", lambda e: e.tensor_tensor(out=lamw[:, 1, :], in0=lamt[:, 2, :], in1=lamt[:, 3, :], op=ALU.mult))
        t = P.op("dve", lambda e: e.tensor_reduce(out=lams[:, 0:2], in_=lamw[:], axis=AX.X, op=ALU.add), waits=[t])
        t = P.op("act", lambda e: e.activation(out=lams[:, 2:4], in_=lams[:, 0:2], func=AF.Exp), waits=[t])
        t = P.op("dve", lambda e: e.tensor_tensor(out=lams[:, 0:1], in0=lams[:, 3:4], in1=lams[:, 2:3], op=ALU.subtract),
                 waits=[t])
        t_lam = P.op("dve", lambda e: e.tensor_scalar(out=neglam[:], in0=lams[:, 0:1], scalar1=-LAMBDA_INIT, scalar2=None,
                                                      op0=ALU.add), waits=[t])

        ST = [bank[0], bank[1]]
        OB = [[bank[2], bank[3]], [bank[4], bank[5]]]
        TB = bank[7]
        st_free = [bank_free[0], bank_free[1]]
        pt_free = [None, None]
        o_free = [[bank_free[2], bank_free[3]], [bank_free[4], bank_free[5]]]
        tb_free = bank_free[7]
        ast_free = [None, None]
        on_free = [[None, None], [None, None]]
        n_ast = 0

        head_done = [None] * H
        t_hlds = [None] * H

        def issue_loads(h):
            p = h % 2
            w = [t_prev] + (head_done[h - 2] if h >= 2 else [])
            sl = s_ld2[p]
            for c in range(2):
                P.dma("sp", lambda e, c=c, h=h, p=p: e.dma_start(out=qa2[p][c][0:64, :], in_=qT_s[h, c]), sl, waits=w)
                P.dma("sp", lambda e, c=c, h=h, p=p: e.dma_start(out=qa2[p][c][64:68, :], in_=A["qaug"][h]), sl)
                P.dma("sp", lambda e, c=c, h=h, p=p: e.dma_start(out=ka2[p][c][0:64, :], in_=kT_s[h, c]), sl)
                P.dma("sp", lambda e, c=c, h=h, p=p: e.dma_start(out=ka2[p][c][64:68, :], in_=A["kaug"][h]), sl)
            P.dma("sp", lambda e, h=h, p=p: e.dma_start(
                out=Vh2[p][:, :, 0:128], in_=v_s.rearrange("(kb p) d -> p kb d", p=128)[:, :, h * 128:(h + 1) * 128]), sl)
            t_hlds[h] = (sl, sl.v)

        issue_loads(0)
        slopes = _slopes()
        for h in range(H):
            if h + 1 < H:
                issue_loads(h + 1)
            npf = (len(prep_fns) + H - 1) // H
            for f in prep_fns[h * npf:(h + 1) * npf]:
                f()
            t_hld = t_hlds[h]
            qa, ka, Vh = qa2[h % 2], ka2[h % 2], Vh2[h % 2]
            def kb_first(qp, h=h):
                for kb in range(2 * qp + 2):
                    if slopes[h] * (256 * qp - (128 * kb + 127)) < 64.0:
                        return kb
                return 2 * qp
            kb0 = [kb_first(qp) for qp in range(NQP)]
            units = [(qp, kb) for qp in range(NQP) for kb in range(kb0[qp], 2 * qp + 2)]
            chain_q = []
            deferred = []
            qk_tok = {}

            def emit_qk(i):
                qp, kb = units[i]
                b = i % 2
                q0 = qp * 256
                last = (kb == 2 * qp + 1)
                diag0 = (kb == 2 * qp)
                tk = None
                for c in range(2):
                    w = ([st_free[b]] + ([t_hld, t_c2] if i < 2 else [])) if c == 0 else []
                    if last:
                        P.op("pe", lambda e, c=c, b=b, kb=kb, q0=q0, ka=ka, qa=qa: e.matmul(
                            ST[b][:, c * 256 + 128:c * 256 + 256], ka[c][0:68, kb * 128:(kb + 1) * 128],
                            qa[c][0:68, q0 + 128:q0 + 256], start=True, stop=False), waits=w, sig=False)
                        tk = P.op("pe", lambda e, c=c, b=b, h=h: e.matmul(
                            ST[b][:, c * 256 + 128:c * 256 + 256], ident_b[:], corrT[:, h, :], start=False, stop=True), sig=(c == 1))
                    else:
                        tk = P.op("pe", lambda e, c=c, b=b, kb=kb, q0=q0, diag0=diag0, ka=ka, qa=qa: e.matmul(
                            ST[b][:, c * 256:c * 256 + 256], ka[c][0:68, kb * 128:(kb + 1) * 128],
                            qa[c][0:68, q0:q0 + 256], start=True, stop=not diag0), waits=w, sig=(not diag0 and c == 1))
                        if diag0:
                            tk = P.op("pe", lambda e, c=c, b=b, h=h: e.matmul(
                                ST[b][:, c * 256:c * 256 + 128], ident_b[:], corrT[:, h, :], start=False, stop=True), sig=(c == 1))
                qk_tok[i] = tk

            emit_qk(0)
            for i, (qp, kb) in enumerate(units):
                b = i % 2
                last = (kb == 2 * qp + 1)
                if last:
                    src = ST[b][:].rearrange("p (c j q) -> p c j q", c=2, j=2)[:, :, 1, :]
                    dst = PT[b][:].rearrange("p (c j q) -> p c j q", c=2, j=2)[:, :, 1, :]
                else:
                    src = ST[b][:]
                    dst = PT[b][:]
                t_exp = P.op("act", lambda e, src=src, dst=dst: e.activation(out=dst, in_=src, func=AF.Exp),
                             waits=[qk_tok[i], pt_free[b]])
                st_free[b] = t_exp
                if i + 1 < len(units):
                    emit_qk(i + 1)
                while deferred and deferred[0][0] <= i and deferred[0][2] <= qp - 1:
                    deferred.pop(0)[1]()
                tk = None
                pv_list = [(j, c) for j in range(2) for c in range(2) if not (last and j == 0)]
                for n_, (j, c) in enumerate(pv_list):
                    stop = (kb == 2 * qp + j)
                    w = ([t_exp] + ([t_ones] if i == 0 else [])) if n_ == 0 else []
                    if kb == kb0[qp]:
                        w = w + [o_free[j][c]]
                    tk = P.op("pe", lambda e, j=j, c=c, b=b, kb=kb, stop=stop, Vh=Vh, st_=(kb == kb0[qp]): e.matmul(
                        OB[j][c][:, 0:129], PT[b][:, c * 256 + j * 128:c * 256 + (j + 1) * 128], Vh[:, kb, :],
                        start=st_, stop=stop), waits=w, sig=(n_ == len(pv_list) - 1))
                pt_free[b] = tk
                for j in range(2):
                    if kb != 2 * qp + j:
                        continue
                    t = P.op("dve", lambda e, j=j: e.tensor_copy(out=Osb[:, j, 0, :], in_=OB[j][0][:, 0:129]), waits=[tk])
                    o_free[j][0] = t
                    t = P.op("dve", lambda e, j=j: e.tensor_copy(out=Osb[:, j, 1, :], in_=OB[j][1][:, 0:129]), waits=[t])
                    o_free[j][1] = t
                    chain_q.append((j, qp, t))
                if last:
                  while deferred and deferred[0][2] <= qp - 2:
                      deferred.pop(0)[1]()
                  t = chain_q[-1][2]
                  t = P.op("dve", lambda e: e.reciprocal(out=rc4[:], in_=Osb[:, :, :, 128]), waits=[t])
                  t = P.op("dve", lambda e: e.tensor_scalar(out=r1l[:], in0=rc4[:, :, 1], scalar1=neglam[:, 0:1], scalar2=None,
                                                           op0=ALU.mult), waits=[t, t_lam])
                  for j in range(2):
                      t = P.op("dve", lambda e, j=j: e.tensor_scalar(out=Abuf[j][:], in0=Osb[:, j, 0, 0:128],
                                                                    scalar1=rc4[:, j, 0:1], scalar2=None, op0=ALU.mult), waits=[t])
                      t = P.op("dve", lambda e, j=j: e.scalar_tensor_tensor(out=Dbuf[j][:], in0=Osb[:, j, 1, 0:128],
                                                                           scalar=r1l[:, j:j + 1], in1=Abuf[j][:],
                                                                           op0=ALU.mult, op1=ALU.add), waits=[t])
                      t = P.op("dve", lambda e, j=j: e.scalar_tensor_tensor(out=sq[:], in0=Dbuf[j][:], scalar=1.0, in1=Dbuf[j][:],
                                                                           op0=ALU.mult, op1=ALU.mult,
                                                                           accum_out=ssq[:, j:j + 1]), waits=[t])
                  t = P.op("dve", lambda e: e.tensor_scalar(out=ssq[:], in0=ssq[:], scalar1=1.0 / 128.0, scalar2=RMS_EPS,
                                                           op0=ALU.mult, op1=ALU.add), waits=[t])
                  t = P.op("pool", lambda e: e.tensor_tensor(out=rstd2[:], in0=ssq[:], in1=neghalf2[:], op=ALU.pow), waits=[t])
                  for (j, qp_, _t) in chain_q:
                    t_on = P.op("dve", lambda e, j=j, qpar=qp_ % 2: e.tensor_scalar(out=On[j][qpar][:], in0=Dbuf[j][:],
                                                                                   scalar1=rstd2[:, j:j + 1], scalar2=None,
                                                                                   op0=ALU.mult), waits=[t, on_free[j][qp_ % 2]])

                    def fin(j=j, t_on=t_on, qp=qp_, h=h):
                        nonlocal tb_free, n_ast
                        tt = P.op("pe", lambda e, j=j, qpar=qp % 2: e.transpose(out=TB[:, j * 128:(j + 1) * 128], in_=On[j][qpar][:],
                                                                               identity=ident_b[:]),
                                  waits=[t_on] + ([tb_free] if j == 0 else []))
                        on_free[j][qp % 2] = tt
                        if j == 1:
                            r = n_ast % 2
                            n_ast += 1
                            tc = P.op("dve", lambda e, r=r: e.tensor_copy(out=aT_st[r][:], in_=TB[:, 0:256]),
                                      waits=[tt, ast_free[r]])
                            tb_free = tc
                            ast_free[r] = P.dma("sp", lambda e, r=r, qp=qp, h=h: e.dma_start(
                                out=aT_s[h * 128:(h + 1) * 128, qp * 256:(qp + 1) * 256], in_=aT_st[r][:]), s_ast[r],
                                waits=[tc])
                    deferred.append((i + 5, fin, qp_))
                  chain_q = []
            while deferred:
                deferred.pop(0)[1]()
            head_done[h] = [tk, (P.esem["pe"], P.esem["pe"].v)]
        for bi, tkn in ((0, st_free[0]), (1, st_free[1]), (2, o_free[0][0]), (3, o_free[0][1]), (4, o_free[1][0]),
                        (5, o_free[1][1]), (7, tb_free)):
            bank_free[bi] = tkn
        t_end = [x for x in ast_free if x is not None] + [(P.esem[k], P.esem[k].v) for k in ("pe", "act", "dve", "pool")]
        P.barrier()
        P.replay()
    return t_end


def phase3(nc, P, A, S, bank, bank_free, ident_f, t_const, s_scr, gbT_s, maT_s, aT_s, h1_s, h1b_s, logits_all, dbg):
    NT = S // 512
    t_prev = (s_scr, s_scr.v)
    with ExitStack() as ps:
        def sb(name, shape, dt):
            return ps.enter_context(nc.sbuf_tensor("sb_" + name, shape, dt))
        wbb = sb("wbb", [128, 8, D], BF16)
        wst = [sb(f"wst{i}", [128, D], F32) for i in range(2)]
        wout = sb("wout", [128, 8, D], BF16)
        sublg = sb("sublg", [128, 1], F32)
        ln1g = sb("ln1g", [128, D], F32)
        ln1b = sb("ln1b", [128, D], F32)
        wr = sb("wr", [128, 8, 36], F32)
        rbias = sb("rbias", [128, 36], F32)
        aT = [sb(f"aT{i}", [128, 8, 512], BF16) for i in range(2)]
        NGC = 6
        gbc = [sb(f"gbc{i}", [128, 512], F32) for i in range(NGC)]
        mac = [sb(f"mac{i}", [128, 512], F32) for i in range(NGC)]
        s_gc = [P.sem(f"p3gc{i}") for i in range(NGC)]
        gc_free = [None] * NGC
        t_gc = {}

        def issue_gc(ci):
            g = ci % NGC
            tt_, m_ = divmod(ci, 8)
            P.dma("sp", lambda e, g=g, tt_=tt_, m_=m_: e.dma_start(
                out=gbc[g][:], in_=gbT_s[m_ * 128:(m_ + 1) * 128, tt_ * 512:(tt_ + 1) * 512]), s_gc[g], waits=[t_prev, gc_free[g]])
            t_gc[ci] = P.dma("sp", lambda e, g=g, tt_=tt_, m_=m_: e.dma_start(
                out=mac[g][:], in_=maT_s[m_ * 128:(m_ + 1) * 128, tt_ * 512:(tt_ + 1) * 512]), s_gc[g])
        xt = [sb(f"xt3{i}", [128, 4, D], F32) for i in range(2)]
        tmpg = [sb(f"tmpg{i}", [128, 512], F32) for i in range(2)]
        mgT = sb("mgT", [128, 8, 512], BF16)
        z = [sb(f"z{i}", [128, D], F32) for i in range(2)]
        zn = [sb(f"zn{i}", [128, D], F32) for i in range(2)]
        h1 = [sb(f"h1t{i}", [128, D], F32) for i in range(2)]
        h1b = [sb(f"h1bt{i}", [128, D], BF16) for i in range(2)]
        h1T = sb("h1T", [128, 8, 128], F32)
        st12 = sb("st12", [128, 2, 6], F32)
        mv = sb("mv3", [128, 2], F32)
        rr = sb("rr3", [128, 2], F32)
        neghalf = sb("neghalf3", [128, 1], F32)
        s_w = P.sem("p3w")
        s_wst = [P.sem(f"p3wst{i}") for i in range(2)]
        s_in = [P.sem(f"p3in{i}") for i in range(2)]
        s_xin = [P.sem(f"p3xin{i}") for i in range(2)]
        s_h1 = [P.sem(f"p3h1{i}") for i in range(2)]

        P.dma("sp", lambda e: e.dma_start(out=sublg[:], in_=A["subln_g"][0].rearrange("(p o) -> p o", o=1)), s_w, waits=[t_prev])
        P.dma("sp", lambda e: e.dma_start(out=ln1g[:], in_=A["ln1_g"][0].partition_broadcast(128)), s_w)
        P.dma("sp", lambda e: e.dma_start(out=ln1b[:], in_=A["ln1_b"][0].partition_broadcast(128)), s_w)
        P.dma("sp", lambda e: e.dma_start(out=wr[:, :, 0:4], in_=A["w_group"][0].rearrange("(kc p) n -> p kc n", p=128)), s_w)
        P.dma("sp", lambda e: e.dma_start(out=wr[:, :, 4:36], in_=A["w_expert"][0].rearrange("(kc p) n -> p kc n", p=128)), s_w)
        P.dma("sp", lambda e: e.dma_start(out=rbias[:, 0:4], in_=A["b_group"][0].partition_broadcast(128)), s_w)
        P.dma("sp", lambda e: e.dma_start(out=rbias[:, 4:36], in_=A["b_expert"][0].partition_broadcast(128)), s_w)
        t_w = (s_w, s_w.v)
        s_wo = P.sem("p3wo")
        for kc in range(8):
            P.dma("pool", lambda e, kc=kc: e.dma_start(out=wout[:, kc, :], in_=A["w_out"][0, kc * 128:(kc + 1) * 128, :]), s_wo,
                  waits=[t_prev])
        t_wo = (s_wo, s_wo.v)
        P.op("dve", lambda e: e.memset(neghalf[:], -0.5), waits=[t_prev], sig=False)
        wst_free = [None, None]
        t_wbb = None
        for hh in range(8):
            r = hh % 2
            tl = P.dma("sp", lambda e, hh=hh, r=r: e.dma_start(out=wst[r][:], in_=A["w_branch_b"][0, hh * 128:(hh + 1) * 128, :]),
                       s_wst[r], waits=[wst_free[r], t_prev])
            t_wbb = P.op("dve", lambda e, hh=hh, r=r: e.tensor_scalar(out=wbb[:, hh, :], in0=wst[r][:], scalar1=sublg[:, 0:1],
                                                                      scalar2=1.0 - LAMBDA_INIT, op0=ALU.mult, op1=ALU.mult),
                         waits=[tl, t_w])
            wst_free[r] = t_wbb

        aT_free = [None, None]
        xt_free = [None, None]
        tmp_free = [None, None]
        mg_free = [None, None]
        z_free = [None, None]
        zn_free = [None, None]
        h1_free = [[], []]
        h1b_free = [None, None]
        h1T_free = None
        rr_acc = [0]
        mgT2 = [mgT, sb("mgT1", [128, 8, 512], BF16)]
        t_aT = {}
        t_xt = {}
        t_mg_done = {}

        def acc_bank():
            b = rr_acc[0] % 4
            rr_acc[0] += 1
            return b

        def issue_in(tt_i):
            ib = tt_i % 2
            tok0 = tt_i * 512
            t_aT[tt_i] = P.dma("sp", lambda e, ib=ib, tok0=tok0: e.dma_start(
                out=aT[ib][:], in_=aT_s.rearrange("(h p) s -> p h s", p=128)[:, :, tok0:tok0 + 512]), s_in[ib],
                waits=[t_prev, aT_free[ib]])
            t_xt[tt_i] = P.dma("sp", lambda e, ib=ib, tok0=tok0: e.dma_start(
                out=xt[ib][:], in_=A["x"][tok0:tok0 + 512, :].rearrange("(b p) d -> p b d", p=128)), s_xin[ib],
                waits=[xt_free[ib]])

        def emit_yb(tt_i, m):
            ib = tt_i % 2
            b = acc_bank()
            tk = None
            for kc in range(8):
                tk = P.op("pe", lambda e, kc=kc, b=b, m=m, ib=ib: e.matmul(
                    bank[b][:], wbb[:, kc, m * 128:(m + 1) * 128], aT[ib][:, kc, :], start=(kc == 0), stop=(kc == 7)),
                    waits=[t_aT[tt_i], t_wbb, bank_free[b]] if kc == 0 else [], sig=(kc == 7))
            if m == 7:
                aT_free[ib] = tk
            r = m % 2
            ci = tt_i * 8 + m
            g = ci % NGC
            if ci + NGC - 1 < NT * 8:
                issue_gc(ci + NGC - 1)
            t = P.op("dve", lambda e, b=b, g=g, r=r: e.tensor_tensor(out=tmpg[r][:], in0=bank[b][:], in1=gbc[g][:],
                                                                    op=ALU.mult), waits=[tk, tmp_free[r], t_gc[ci]])
            bank_free[b] = t
            t_mg = P.op("pool", lambda e, m=m, g=g, r=r, ib=ib: e.tensor_tensor(out=mgT2[ib][:, m, :], in0=tmpg[r][:], in1=mac[g][:],
                                                                               op=ALU.add), waits=[t, mg_free[ib] if m == 0 else None])
            tmp_free[r] = t_mg
            gc_free[g] = t_mg
            if m == 7:
                t_mg_done[tt_i] = t_mg

        nblk = 0
        for ci in range(NGC - 1):
            issue_gc(ci)
        issue_in(0)
        for m in range(8):
            emit_yb(0, m)
        for tt_i in range(NT):
            tok0 = tt_i * 512
            ib = tt_i % 2
            if tt_i + 1 < NT:
                issue_in(tt_i + 1)
            t_mg = t_mg_done[tt_i]
            for tb in range(4):
                zb = nblk % 2
                nblk += 1
                tz = None
                for half in range(2):
                    b = acc_bank()
                    tk = None
                    for kc in range(8):
                        tk = P.op("pe", lambda e, kc=kc, b=b, tb=tb, half=half, ib=ib: e.matmul(
                            bank[b][:], mgT2[ib][:, kc, tb * 128:(tb + 1) * 128], wout[:, kc, half * 512:(half + 1) * 512],
                            start=(kc == 0), stop=(kc == 7)), waits=[t_mg, t_wo, bank_free[b]] if kc == 0 else [], sig=(kc == 7))
                    tz = P.op("dve", lambda e, b=b, tb=tb, half=half, ib=ib, zb=zb: e.scalar_tensor_tensor(
                        out=z[zb][:, half * 512:(half + 1) * 512], in0=xt[ib][:, tb, half * 512:(half + 1) * 512], scalar=ALPHA,
                        in1=bank[b][:], op0=ALU.mult, op1=ALU.add), waits=[tk, z_free[zb], t_xt[tt_i]])
                    bank_free[b] = tz
                    tz = P.op("dve", lambda e, half=half, zb=zb: e.bn_stats(out=st12[:, half, :],
                                                                           in_=z[zb][:, half * 512:(half + 1) * 512]), waits=[tz])
                if tb == 3:
                    mg_free[ib] = tk
                    xt_free[ib] = tz
                if tt_i + 1 < NT:
                    emit_yb(tt_i + 1, 2 * tb)
                    emit_yb(tt_i + 1, 2 * tb + 1)

                t = P.op("dve", lambda e: e.bn_aggr(out=mv[:], in_=st12[:].rearrange("p a b -> p (a b)")), waits=[tz])
                t = P.op("dve", lambda e: e.tensor_scalar(out=rr[:, 0:1], in0=mv[:, 1:2], scalar1=LN_EPS, scalar2=None, op0=ALU.add),
                         waits=[t])
                t = P.op("act", lambda e: e.activation(out=rr[:, 0:1], in_=rr[:, 0:1], func=AF.Sqrt), waits=[t])
                t = P.op("dve", lambda e: e.reciprocal(out=rr[:, 0:1], in_=rr[:, 0:1]), waits=[t])
                t = P.op("dve", lambda e: e.scalar_tensor_tensor(out=rr[:, 1:2], in0=mv[:, 0:1], scalar=-1.0, in1=rr[:, 0:1],
                                                                op0=ALU.mult, op1=ALU.mult), waits=[t])
                t = P.op("act", lambda e, zb=zb: e.activation(out=zn[zb][:], in_=z[zb][:], func=AF.Identity, bias=rr[:, 1:2],
                                                             scale=rr[:, 0:1]), waits=[t, zn_free[zb]])
                z_free[zb] = t
                t = P.op("dve", lambda e, zb=zb: e.tensor_tensor(out=zn[zb][:], in0=zn[zb][:], in1=ln1g[:], op=ALU.mult),
                         waits=[t, t_w])
                t_h1 = P.op("pool", lambda e, zb=zb: e.tensor_tensor(out=h1[zb][:], in0=zn[zb][:], in1=ln1b[:], op=ALU.add),
                            waits=[t] + h1_free[zb])
                zn_free[zb] = t_h1
                t_b = P.op("act", lambda e, zb=zb: e.copy(out=h1b[zb][:], in_=h1[zb][:]), waits=[t_h1, h1b_free[zb]])
                r0 = tok0 + tb * 128
                td1 = P.dma("sp", lambda e, zb=zb, r0=r0: e.dma_start(out=h1_s[r0:r0 + 128, :], in_=h1[zb][:]), s_h1[zb], waits=[t_h1])
                h1b_free[zb] = P.dma("sp", lambda e, zb=zb, r0=r0: e.dma_start(out=h1b_s[r0:r0 + 128, :], in_=h1b[zb][:]), s_h1[zb],
                                     waits=[t_b])
                h1_free[zb] = [h1b_free[zb]]
        t_h1done = [(s_h1[0], s_h1[0].v), (s_h1[1], s_h1[1].v)]
        NRB = 4
        hr = [xt[0][:, i, :] for i in range(4)]
        s_hr = [P.sem(f"p3hr{i}") for i in range(NRB)]
        hr_free = [None] * NRB
        t_hr = {}
        t_main_done = [(P.esem[k], P.esem[k].v) for k in ("pe", "act", "dve", "pool")]

        def issue_hr(bi):
            g = bi % NRB
            t_hr[bi] = P.dma("sp", lambda e, bi=bi, g=g: e.dma_start(out=hr[g], in_=h1_s[bi * 128:(bi + 1) * 128, :]), s_hr[g],
                             waits=t_h1done + t_main_done + [hr_free[g]])
        NBk = S // 128
        for bi in range(min(NRB - 1, NBk)):
            issue_hr(bi)
        for bi in range(NBk):
            g = bi % NRB
            if bi + NRB - 1 < NBk:
                issue_hr(bi + NRB - 1)
            tks = []
            for kc in range(8):
                bq = 4 + kc // 4
                tks.append(P.op("pe", lambda e, kc=kc, bq=bq, g=g: e.transpose(
                    out=bank[bq][:, (kc % 4) * 128:(kc % 4 + 1) * 128], in_=hr[g][:, kc * 128:(kc + 1) * 128],
                    identity=ident_f[:]), waits=[t_hr[bi], bank_free[bq], t_const] if kc % 4 == 0 else []))
            hr_free[g] = tks[7]
            t = P.op("act", lambda e: e.copy(out=h1T[:, 0:4, :], in_=bank[4][:].rearrange("p (k t) -> p k t", k=4)),
                     waits=[tks[3], h1T_free])
            bank_free[4] = t
            t = P.op("dve", lambda e: e.tensor_copy(out=h1T[:, 4:8, :], in_=bank[5][:].rearrange("p (k t) -> p k t", k=4)),
                     waits=[tks[7], h1T_free])
            bank_free[5] = t
            tk = None
            for kc in range(8):
                tk = P.op("pe", lambda e, kc=kc: e.matmul(bank[6][:, 0:36], h1T[:, kc, :], wr[:, kc, :], start=(kc == 0),
                                                         stop=(kc == 7)),
                          waits=[t, bank_free[4], t_w, bank_free[6]] if kc == 0 else [], sig=(kc == 7))
            h1T_free = tk
            t = P.op("dve", lambda e, bi=bi: e.tensor_tensor(out=logits_all[:, bi, :], in0=bank[6][:, 0:36], in1=rbias[:],
                                                            op=ALU.add), waits=[tk])
            bank_free[6] = t
        t_end = [(s_h1[0], s_h1[0].v), (s_h1[1], s_h1[1].v)] + [(P.esem[k], P.esem[k].v) for k in ("pe", "act", "dve", "pool")]
        dbg("logits", logits_all, t_end[-2])
        P.barrier()
        P.replay()
    return t_end


def phase4(nc, P, A, S, bank, bank_free, ident_b, t_const, s_scr, h1_s, h1b_s, slot_s, out_s, out, logits_all, dbg,
           wgu_r, wd_r, t_prep):
    NB = S // 128
    NBLK = 2 * S // MBLK + NE
    NCH = NBLK * 2
    t_prev = (s_scr, s_scr.v)
    with ExitStack() as ps:
        def sb(name, shape, dt):
            return ps.enter_context(nc.sbuf_tensor("sb_" + name, shape, dt))
        L = logits_all
        ones_b = sb("ones_b", [128, 128], BF16)
        triu_b = sb("triu_b", [128, 128], BF16)
        tokidx = sb("tokidx", [128, NB], I32)
        blkstart = sb("blkstart", [128, NBLK], F32)
        w1 = sb("w1", [128, NB], F32)
        w2 = sb("w2", [128, NB], F32)
        dest_i = [sb(f"dest_i{k}", [128, NB], I32) for k in range(2)]
        sidx = sb("sidx", [128, NCH], I32)
        be_i = sb("be_i", [128, NBLK], I32)
        widx_i = sb("widx_i", [128, NBLK], I32)
        pk = sb("pk", [128, 8], F32)
        rs = ExitStack()

        def sbt(name, shape, dt):
            return rs.enter_context(nc.sbuf_tensor("sb_" + name, shape, dt))
        gsh = sbt("gsh", [128, NB, 4], F32)
        gex = sbt("gex", [128, NB, 4], F32)
        gsum = sbt("gsum", [128, NB], F32)
        gw = sbt("gw", [128, NB], F32)
        dm = sbt("dmat", [128, NB], F32)
        oh = [sbt(f"oh{k}", [128, NB, NE], F32) for k in range(2)]
        Call = sbt("Call", [128, NB, NE], BF16)
        sm = sbt("rt_small", [128, 16], F32)
        gmask = sbt("gmask", [128, 4], F32)
        pen = sbt("pen", [128, 4], F32)
        msk = sbt("msk", [128, NE], F32)
        top8 = sbt("top8", [128, 8], F32)
        colsum = sbt("colsum", [128, NB, NE], F32)
        cum = sbt("cum", [128, NB + 1, NE], F32)
        dest_all = sbt("dest_all", [128, NB, NE], F32)
        prod = sbt("prod", [128, NB, NE], F32)
        cnt = sbt("cnt", [128, NE], F32)
        cnt_i = sbt("cnt_i", [128, NE], I32)
        padded = sbt("padded", [128, NE], F32)
        pe_a = sbt("pe_a", [128, NE], F32)
        pe_b = sbt("pe_b", [128, NE], F32)
        pstart = sbt("pstart", [128, NE], F32)
        dest_f = [sbt(f"dest_f{k}", [128, NB], F32) for k in range(2)]
        tmp_i = [sbt(f"tmp_i{k}", [128, NB], I32) for k in range(2)]
        tmp_f = [sbt(f"tmp_f{k}", [128, NB], F32) for k in range(2)]
        addr_i = [sbt(f"addr_i{k}", [128, NB], I32) for k in range(2)]
        zero_i = sbt("zero_i", [128, NCH], I32)
        be_f = sbt("be_f", [128, NBLK], F32)
        s_c = P.sem("p4c")
        P.dma("sp", lambda e: e.dma_start(out=ones_b[:], in_=A["ones_b"]), s_c, waits=[t_prev])
        P.dma("sp", lambda e: e.dma_start(out=triu_b[:], in_=A["triu_b"]), s_c)
        P.dma("sp", lambda e: e.dma_start(out=tokidx[:], in_=A["tokidx"]), s_c)
        P.dma("sp", lambda e: e.dma_start(out=blkstart[:], in_=A["blkstart"]), s_c)
        t_c = (s_c, s_c.v)

        t = None
        for b in range(NB):
            t = P.op("dve", lambda e, b=b: e.tensor_reduce(out=sm[:, 0:1], in_=L[:, b, 0:4], axis=AX.X, op=ALU.max),
                     waits=[t_prev] if b == 0 else [t])
            t = P.op("dve", lambda e, b=b: e.tensor_scalar(out=gmask[:], in0=L[:, b, 0:4], scalar1=sm[:, 0:1], scalar2=None,
                                                          op0=ALU.is_equal), waits=[t])
            t = P.op("dve", lambda e, b=b: e.tensor_scalar(out=gsh[:, b, :], in0=L[:, b, 0:4], scalar1=sm[:, 0:1], scalar2=None,
                                                          op0=ALU.subtract), waits=[t])
            t = P.op("dve", lambda e: e.tensor_scalar(out=pen[:], in0=gmask[:], scalar1=1e30, scalar2=-1e30, op0=ALU.mult,
                                                     op1=ALU.add), waits=[t])
            for g in range(4):
                t = P.op("dve", lambda e, b=b, g=g: e.tensor_scalar(out=msk[:, g * 8:(g + 1) * 8], in0=L[:, b, 4 + g * 8:12 + g * 8],
                                                                   scalar1=pen[:, g:g + 1], scalar2=None, op0=ALU.add), waits=[t])
            t = P.op("dve", lambda e: e.max(out=top8[:], in_=msk[:]), waits=[t])
            t = P.op("dve", lambda e, b=b: e.tensor_scalar(out=oh[0][:, b, :], in0=msk[:], scalar1=top8[:, 0:1], scalar2=None,
                                                          op0=ALU.is_equal), waits=[t])
            t = P.op("dve", lambda e, b=b: e.tensor_scalar(out=oh[1][:, b, :], in0=msk[:], scalar1=top8[:, 1:2], scalar2=None,
                                                          op0=ALU.is_equal), waits=[t])
            t = P.op("dve", lambda e, b=b: e.tensor_tensor(out=dm[:, b:b + 1], in0=top8[:, 0:1], in1=top8[:, 1:2],
                                                          op=ALU.subtract), waits=[t])
            t = P.op("dve", lambda e, b=b: e.tensor_tensor(out=Call[:, b, :], in0=oh[0][:, b, :], in1=oh[1][:, b, :], op=ALU.add),
                     waits=[t])
        t_route = t
        ta = P.op("act", lambda e: e.activation(out=gex[:].rearrange("p b g -> p (b g)"), in_=gsh[:].rearrange("p b g -> p (b g)"),
                                                func=AF.Exp), waits=[t_route])
        ta2 = P.op("act", lambda e: e.activation(out=dm[:], in_=dm[:], func=AF.Sigmoid), waits=[ta])
        t = P.op("dve", lambda e: e.tensor_reduce(out=gsum[:], in_=gex[:], axis=AX.X, op=ALU.add), waits=[ta])
        t = P.op("dve", lambda e: e.reciprocal(out=gw[:], in_=gsum[:]), waits=[t])
        t = P.op("dve", lambda e: e.tensor_tensor(out=w1[:], in0=gw[:], in1=dm[:], op=ALU.mult), waits=[t, ta2])
        t_w12 = P.op("dve", lambda e: e.tensor_tensor(out=w2[:], in0=gw[:], in1=w1[:], op=ALU.subtract), waits=[t])

        GB_ = 16
        for g0 in range(0, NB, GB_):
            n = min(GB_, NB - g0)
            tk = P.op("pe", lambda e, g0=g0, n=n: e.matmul(bank[0][:, 0:n * NE], ones_b[:],
                                                          Call[:, g0:g0 + n, :].rearrange("p b e -> p (b e)"), start=True, stop=True),
                      waits=[t_route, t_c, bank_free[0]])
            t = P.op("dve", lambda e, g0=g0, n=n: e.tensor_copy(out=colsum[:, g0:g0 + n, :].rearrange("p b e -> p (b e)"),
                                                               in_=bank[0][:, 0:n * NE]), waits=[tk])
            bank_free[0] = t
        t = P.op("dve", lambda e: e.tensor_reduce(out=cnt[:], in_=colsum[:].rearrange("p b e -> p e b"), axis=AX.X, op=ALU.add),
                 waits=[t])
        t = P.op("dve", lambda e: e.tensor_scalar(out=padded[:], in0=cnt[:], scalar1=float(MBLK - 1), scalar2=None, op0=ALU.add),
                 waits=[t])
        t = P.op("dve", lambda e: e.tensor_copy(out=cnt_i[:], in_=padded[:]), waits=[t])
        t = P.op("dve", lambda e: e.tensor_single_scalar(out=cnt_i[:], in_=cnt_i[:], scalar=8, op=ALU.arith_shift_right), waits=[t])
        t = P.op("dve", lambda e: e.tensor_single_scalar(out=cnt_i[:], in_=cnt_i[:], scalar=8, op=ALU.logical_shift_left), waits=[t])
        t = P.op("dve", lambda e: e.tensor_copy(out=padded[:], in_=cnt_i[:]), waits=[t])
        t = P.op("dve", lambda e: e.tensor_copy(out=pe_a[:], in_=padded[:]), waits=[t])
        src, dst = pe_a, pe_b
        sft = 1
        while sft < NE:
            t = P.op("dve", lambda e, src=src, dst=dst, sft=sft: e.tensor_copy(out=dst[:, 0:sft], in_=src[:, 0:sft]), waits=[t])
            t = P.op("dve", lambda e, src=src, dst=dst, sft=sft: e.tensor_tensor(out=dst[:, sft:NE], in0=src[:, sft:NE],
                                                                                in1=src[:, 0:NE - sft], op=ALU.add), waits=[t])
            src, dst = dst, src
            sft *= 2
        pend = src
        t = P.op("dve", lambda e: e.tensor_tensor(out=pstart[:], in0=pend[:], in1=padded[:], op=ALU.subtract), waits=[t])
        t = P.op("dve", lambda e: e.tensor_copy(out=cum[:, 0, :], in_=pstart[:]), waits=[t])
        for b in range(NB):
            t = P.op("dve", lambda e, b=b: e.tensor_tensor(out=cum[:, b + 1, :], in0=cum[:, b, :], in1=colsum[:, b, :], op=ALU.add),
                     waits=[t])
        for g0 in range(0, NB, GB_):
            n = min(GB_, NB - g0)
            tk = None
            for bb in range(n):
                tk = P.op("pe", lambda e, g0=g0, bb=bb: e.matmul(bank[0][:, bb * NE:(bb + 1) * NE], triu_b[:], Call[:, g0 + bb, :],
                                                                start=True, stop=True), waits=[bank_free[0]] if bb == 0 else [],
                          sig=(bb == n - 1))
            t = P.op("dve", lambda e, g0=g0, n=n: e.tensor_tensor(out=dest_all[:, g0:g0 + n, :].rearrange("p b e -> p (b e)"),
                                                                 in0=bank[0][:, 0:n * NE],
                                                                 in1=cum[:, g0:g0 + n, :].rearrange("p b e -> p (b e)"), op=ALU.add),
                     waits=[tk, t])
            bank_free[0] = t
        for k in range(2):
            t = P.op("dve", lambda e, k=k: e.tensor_tensor(out=prod[:].rearrange("p b e -> p (b e)"),
                                                          in0=oh[k][:].rearrange("p b e -> p (b e)"),
                                                          in1=dest_all[:].rearrange("p b e -> p (b e)"), op=ALU.mult), waits=[t])
            t = P.op("dve", lambda e, k=k: e.tensor_reduce(out=dest_f[k][:], in_=prod[:], axis=AX.X, op=ALU.add), waits=[t])
            t = P.op("dve", lambda e, k=k: e.tensor_copy(out=dest_i[k][:], in_=dest_f[k][:]), waits=[t])
            t = P.op("dve", lambda e, k=k: e.tensor_single_scalar(out=tmp_i[0][:], in_=dest_i[k][:], scalar=127, op=ALU.bitwise_and),
                     waits=[t])
            t = P.op("dve", lambda e, k=k: e.tensor_single_scalar(out=tmp_i[1][:], in_=dest_i[k][:], scalar=7,
                                                                 op=ALU.arith_shift_right), waits=[t])
            t = P.op("dve", lambda e: e.tensor_copy(out=tmp_f[0][:], in_=tmp_i[0][:]), waits=[t])
            t = P.op("dve", lambda e: e.tensor_copy(out=tmp_f[1][:], in_=tmp_i[1][:]), waits=[t])
            t = P.op("dve", lambda e: e.scalar_tensor_tensor(out=tmp_f[0][:], in0=tmp_f[0][:], scalar=float(NCH), in1=tmp_f[1][:],
                                                            op0=ALU.mult, op1=ALU.add), waits=[t])
            t = P.op("dve", lambda e, k=k: e.tensor_copy(out=addr_i[k][:], in_=tmp_f[0][:]), waits=[t])
        t_addr = t
        t = P.op("dve", lambda e: e.memset(be_f[:], 0.0), waits=[t])
        for ex in range(NE):
            t = P.op("dve", lambda e, ex=ex: e.scalar_tensor_tensor(out=be_f[:], in0=blkstart[:], scalar=pend[:, ex:ex + 1],
                                                                   in1=be_f[:], op0=ALU.is_ge, op1=ALU.add), waits=[t, t_c])
        t = P.op("dve", lambda e: e.tensor_scalar(out=be_f[:], in0=be_f[:], scalar1=float(NE - 1), scalar2=None, op0=ALU.min),
                 waits=[t])
        t = P.op("dve", lambda e: e.tensor_copy(out=be_i[:], in_=be_f[:]), waits=[t])
        widx_f = sbt("widx_f", [128, NBLK], F32)
        t = P.op("dve", lambda e: e.tensor_copy(out=pk[:, 0:1], in_=tokidx[:, 0:1]), waits=[t, t_c])
        t = P.op("dve", lambda e: e.tensor_scalar(out=widx_f[:], in0=be_f[:], scalar1=128.0, scalar2=pk[:, 0:1],
                                                 op0=ALU.mult, op1=ALU.add), waits=[t])
        t_be = P.op("dve", lambda e: e.tensor_copy(out=widx_i[:], in_=widx_f[:]), waits=[t])
        s_sl = P.sem("p4sl")
        s_sz = P.sem("p4sz")
        s_sr = P.sem("p4sr")
        t = P.op("dve", lambda e: e.memset(zero_i[:], 0), waits=[t_be])
        t = P.dma("sp", lambda e: e.dma_start(out=slot_s, in_=zero_i[:]), s_sz, waits=[t, t_prev])
        slot_flat = slot_s.rearrange("p (c o) -> (p c) o", o=1)
        for b in range(NB):
            for k in range(2):
                P.dma("pool", lambda e, b=b, k=k: e.indirect_dma_start(
                    out=slot_flat, out_offset=bass.IndirectOffsetOnAxis(ap=addr_i[k][:, b:b + 1], axis=0),
                    in_=tokidx[:, b:b + 1], in_offset=None), s_sl, waits=[t, t_addr, t_c])
        t_sc = (s_sl, s_sl.v)
        t_sidx = P.dma("sp", lambda e: e.dma_start(out=sidx[:], in_=slot_s), s_sr, waits=[t_sc])
        dbg("be_i", be_i, t_be)
        dbg("dest_i0", dest_i[0], t_addr)
        dbg("dest_i1", dest_i[1], t_addr)
        dbg("w1", w1, t_w12)
        dbg("w2", w2, t_w12)
        dbg("sidx", sidx, t_sidx)

        t_rs_end = [t_sidx, (s_scr, s_scr.v)] + [(P.esem[k], P.esem[k].v) for k in ("pe", "act", "dve", "pool")]
        P.barrier()
        P.replay()
        rs.close()
        xs = ExitStack()

        def sbx(name, shape, dt):
            return xs.enter_context(nc.sbuf_tensor("sb_" + name, shape, dt))
        wgu = [sbx(f"wgu{i}", [128, 2, 8, EH], BF16) for i in range(2)]
        wd = [sbx(f"wd{i}", [128, 4, D], BF16) for i in range(2)]
        xg = [sbx(f"xg{i}", [128, D], BF16) for i in range(6)]
        xTm = [sbx(f"xTm{i}", [128, 8, 256], BF16) for i in range(2)]
        sgt = [sbx(f"sgt{i}", [128, 256], F32) for i in range(2)]
        hT = [sbx(f"hT{i}", [128, 4, 256], BF16) for i in range(2)]
        ost = [sbx(f"ost{i}", [128, D], F32) for i in range(2)]
        s_wt = [P.sem(f"p4w{i}") for i in range(2)]
        s_xg = [P.sem(f"p4x{i}") for i in range(6)]
        s_os = [P.sem(f"p4o{i}") for i in range(2)]
        w_free = [None, None]
        xg_free = [None] * 6
        xTm_free = [None, None]
        sgt_free = [None, None]
        hT_free = [None, None]
        ost_free = [None, None]
        n_os = 0
        n_sg = 0
        reg_holder = []
        acc = [0]

        def acc_bank():
            b = 1 + acc[0] % 4
            acc[0] += 1
            return b

        def load_w(j):
            wb = j % 2
            P.dma("pool", lambda e, wb=wb, j=j: e.indirect_dma_start(
                out=wgu[wb][:].rearrange("p g k f -> p (g k f)"), out_offset=None, in_=wgu_r,
                in_offset=bass.IndirectOffsetOnAxis(ap=widx_i[:, j:j + 1], axis=0)), s_wt[wb],
                waits=[t_be, w_free[wb], t_prep] + (t_rs_end if j < 2 else []))
            tok = P.dma("pool", lambda e, wb=wb, j=j: e.indirect_dma_start(
                out=wd[wb][:].rearrange("p k f -> p (k f)"), out_offset=None, in_=wd_r,
                in_offset=bass.IndirectOffsetOnAxis(ap=widx_i[:, j:j + 1], axis=0)), s_wt[wb])
            return [tok]

        def load_x(j):
            toks = []
            for i in range(2):
                xi = (2 * j + i) % 6
                c = 2 * j + i
                toks.append(P.dma("pool", lambda e, xi=xi, c=c: e.indirect_dma_start(
                    out=xg[xi][:], out_offset=None, in_=h1b_s,
                    in_offset=bass.IndirectOffsetOnAxis(ap=sidx[:, c:c + 1], axis=0)), s_xg[xi],
                    waits=[t_sidx, xg_free[xi]] + (t_rs_end if j < 2 else [])))
            return toks

        t_wl = {0: load_w(0)}
        t_xl = {0: load_x(0)}
        if NBLK > 1:
            t_xl[1] = load_x(1)
        t_xTs = {}

        def emit_T(j):
            xb = j % 2
            t = None
            for i in range(2):
                xi = (2 * j + i) % 6
                tk = None
                for kc in range(8):
                    tk = P.op("pe", lambda e, kc=kc, xi=xi: e.transpose(out=bank[7][:, kc * 128:(kc + 1) * 128],
                                                                       in_=xg[xi][:, kc * 128:(kc + 1) * 128], identity=ident_b[:]),
                              waits=[t_xl[j][i], bank_free[7], t_const] if kc == 0 else [], sig=(kc == 7))
                xg_free[xi] = tk
                t = P.op("act", lambda e, i=i, xb=xb: e.copy(out=xTm[xb][:, :, i * 128:(i + 1) * 128],
                                                            in_=bank[7][:].rearrange("p (k t) -> p k t", k=8)),
                         waits=[tk, xTm_free[xb]] if i == 0 else [tk])
                bank_free[7] = t
            t_xTs[j] = t

        emit_T(0)
        for j in range(NBLK):
            wb = j % 2
            if j + 1 < NBLK:
                t_wl[j + 1] = load_w(j + 1)
            if j + 2 < NBLK:
                t_xl[j + 2] = load_x(j + 2)
            xb = j % 2
            t_xT = t_xTs[j]
            hb = j % 2
            for hc in range(4):
                b = acc_bank()
                tk = None
                for gu in range(2):
                    for kc in range(8):
                        tk = P.op("pe", lambda e, kc=kc, b=b, gu=gu, hc=hc, wb=wb, xb=xb: e.matmul(
                            bank[b][:, gu * 256:(gu + 1) * 256], wgu[wb][:, gu, kc, hc * 128:(hc + 1) * 128], xTm[xb][:, kc, :],
                            start=(kc == 0), stop=(kc == 7)),
                            waits=[t_xT, bank_free[b]] + t_wl[j] if (kc == 0 and gu == 0) else [], sig=(kc == 7 and gu == 1))
                r = n_sg % 2
                n_sg += 1
                t = P.op("act", lambda e, b=b, r=r: e.activation(out=sgt[r][:], in_=bank[b][:, 0:256], func=AF.Silu),
                         waits=[tk, sgt_free[r]])
                t = P.op("dve", lambda e, b=b, r=r, hc=hc, hb=hb: e.tensor_tensor(out=hT[hb][:, hc, :], in0=sgt[r][:],
                                                                                in1=bank[b][:, 256:512], op=ALU.mult),
                         waits=[t, hT_free[hb]] if hc == 0 else [t])
                sgt_free[r] = t
                bank_free[b] = t
            xTm_free[xb] = tk
            t_hT = t
            if j + 1 < NBLK:
                emit_T(j + 1)
            for i in range(2):
                r = n_os % 2
                n_os += 1
                t = None
                for oh_ in range(2):
                    b = acc_bank()
                    tk = None
                    for hc in range(4):
                        tk = P.op("pe", lambda e, hc=hc, b=b, i=i, oh_=oh_, hb=hb, wb=wb: e.matmul(
                            bank[b][:], hT[hb][:, hc, i * 128:(i + 1) * 128], wd[wb][:, hc, oh_ * 512:(oh_ + 1) * 512],
                            start=(hc == 0), stop=(hc == 3)), waits=[t_hT, bank_free[b]] if hc == 0 else [], sig=(hc == 3))
                    if oh_ == 0:
                        t = P.op("act", lambda e, b=b, r=r: e.copy(out=ost[r][:, 0:512], in_=bank[b][:]), waits=[tk, ost_free[r]])
                    else:
                        t = P.op("dve", lambda e, b=b, r=r: e.tensor_copy(out=ost[r][:, 512:1024], in_=bank[b][:]),
                                 waits=[tk, t, ost_free[r]])
                    bank_free[b] = t
                c = 2 * j + i
                ost_free[r] = P.dma("sp", lambda e, r=r, c=c: e.dma_start(out=out_s[c * 128:(c + 1) * 128, :], in_=ost[r][:]), s_os[r],
                                    waits=[t, (P.esem["act"], P.esem["act"].v)])
            hT_free[hb] = tk
            w_free[wb] = tk
        t_exp_done = [x for x in ost_free if x is not None] + [(P.esem[k], P.esem[k].v) for k in ("pe", "act", "dve", "pool")]
        P.barrier()
        P.replay()
        xs.close()

        ln2g = sb("ln2g", [128, D], F32)
        ln2b = sb("ln2b", [128, D], F32)
        NG = 4
        o1 = [sb(f"o1_{i}", [128, D], F32) for i in range(NG)]
        o2 = [sb(f"o2_{i}", [128, D], F32) for i in range(NG)]
        h1t = [sb(f"h1c{i}", [128, D], F32) for i in range(NG)]
        yb = [sb(f"yb{i}", [128, D], F32) for i in range(2)]
        zo = [sb(f"zo{i}", [128, D], F32) for i in range(2)]
        st12 = sb("st12_4", [128, 2, 6], F32)
        mv = sb("mv4", [128, 2], F32)
        rr = sb("rr4", [128, 2], F32)
        neghalf = sb("neghalf4", [128, 1], F32)
        s_l2 = P.sem("p4l2")
        s_cin = [P.sem(f"p4ci{i}") for i in range(4)]
        s_co = [P.sem(f"p4co{i}") for i in range(2)]
        s_ch = [P.sem(f"p4ch{i}") for i in range(4)]
        P.dma("sp", lambda e: e.dma_start(out=ln2g[:], in_=A["ln2_g"][0].partition_broadcast(128)), s_l2, waits=t_exp_done)
        P.dma("sp", lambda e: e.dma_start(out=ln2b[:], in_=A["ln2_b"][0].partition_broadcast(128)), s_l2)
        t_l2 = (s_l2, s_l2.v)
        P.op("dve", lambda e: e.memset(neghalf[:], -0.5), waits=t_exp_done)
        cin_free = [None] * NG
        zo_free = [None, None]
        yb_free = [None, None]
        t_ins = {}

        def issue_gather(b):
            g = b % NG
            P.dma("pool", lambda e, b=b, g=g: e.indirect_dma_start(
                out=o1[g][:], out_offset=None, in_=out_s, in_offset=bass.IndirectOffsetOnAxis(ap=dest_i[0][:, b:b + 1], axis=0)),
                s_cin[g], waits=t_exp_done + [cin_free[g]])
            ta = P.dma("pool", lambda e, b=b, g=g: e.indirect_dma_start(
                out=o2[g][:], out_offset=None, in_=out_s, in_offset=bass.IndirectOffsetOnAxis(ap=dest_i[1][:, b:b + 1], axis=0)),
                s_cin[g])
            tb_ = P.dma("sp", lambda e, b=b, g=g: e.dma_start(out=h1t[g][:], in_=h1_s[b * 128:(b + 1) * 128, :]), s_ch[g],
                        waits=[cin_free[g], t_prev] + t_exp_done)
            t_ins[b] = (ta, tb_)

        for b in range(min(NG - 1, NB)):
            issue_gather(b)
        for b in range(NB):
            r = b % 2
            g = b % NG
            if b + NG - 1 < NB:
                issue_gather(b + NG - 1)
            t_in1, t_in2 = t_ins[b]
            t = P.op("act", lambda e, b=b, r=r, g=g: e.activation(out=yb[r][:], in_=o1[g][:], func=AF.Identity, scale=w1[:, b:b + 1]),
                     waits=[t_in1, t_in2, t_w12, zo_free[r], yb_free[r]])
            t = P.op("dve", lambda e, b=b, r=r, g=g: e.scalar_tensor_tensor(out=yb[r][:], in0=o2[g][:], scalar=w2[:, b:b + 1],
                                                                           in1=yb[r][:], op0=ALU.mult, op1=ALU.add), waits=[t])
            t = P.op("dve", lambda e, r=r, g=g: e.scalar_tensor_tensor(out=yb[r][:], in0=h1t[g][:], scalar=ALPHA, in1=yb[r][:],
                                                                      op0=ALU.mult, op1=ALU.add), waits=[t])
            cin_free[g] = t
            for half in range(2):
                t = P.op("dve", lambda e, half=half, r=r: e.bn_stats(out=st12[:, half, :], in_=yb[r][:, half * 512:(half + 1) * 512]),
                         waits=[t])
            t = P.op("dve", lambda e: e.bn_aggr(out=mv[:], in_=st12[:].rearrange("p a b -> p (a b)")), waits=[t])
            t = P.op("dve", lambda e: e.tensor_scalar(out=rr[:, 0:1], in0=mv[:, 1:2], scalar1=LN_EPS, scalar2=None, op0=ALU.add),
                     waits=[t])
            t = P.op("act", lambda e: e.activation(out=rr[:, 0:1], in_=rr[:, 0:1], func=AF.Sqrt), waits=[t])
            t = P.op("dve", lambda e: e.reciprocal(out=rr[:, 0:1], in_=rr[:, 0:1]), waits=[t])
            t = P.op("dve", lambda e: e.scalar_tensor_tensor(out=rr[:, 1:2], in0=mv[:, 0:1], scalar=-1.0, in1=rr[:, 0:1],
                                                            op0=ALU.mult, op1=ALU.mult), waits=[t])
            t = P.op("act", lambda e, r=r: e.activation(out=yb[r][:], in_=yb[r][:], func=AF.Identity, bias=rr[:, 1:2],
                                                       scale=rr[:, 0:1]), waits=[t])
            t = P.op("dve", lambda e, r=r: e.tensor_tensor(out=yb[r][:], in0=yb[r][:], in1=ln2g[:], op=ALU.mult), waits=[t, t_l2])
            t = P.op("pool", lambda e, r=r: e.tensor_tensor(out=zo[r][:], in0=yb[r][:], in1=ln2b[:], op=ALU.add), waits=[t, zo_free[r]])
            yb_free[r] = t
            zo_free[r] = P.dma("sp", lambda e, b=b, r=r: e.dma_start(out=out[b * 128:(b + 1) * 128, :], in_=zo[r][:]), s_co[r], waits=[t])
        t_end = [x for x in zo_free if x is not None]
        P.op("sp", lambda e: e.nop(), waits=t_end, sig=False)
        P.replay()
    return t_end

def kernel(**inputs):
    S = 8192
    n = 8
    x = np.ascontiguousarray(np.asarray(inputs["x"], dtype=np.float32))
    nc = build(S)
    consts = make_consts(S)
    shared = {k: np.ascontiguousarray(np.asarray(inputs[k], dtype=np.float32)) for k in IN_SHAPES}
    shared.update(consts)
    in_maps = []
    for i in range(n):
        m = dict(shared)
        m["x"] = x[i]
        in_maps.append(m)
    res = run_bass_kernel_spmd(nc, in_maps, core_ids=list(range(n)))
    return np.stack([np.asarray(r["out"], dtype=np.float32) for r in res.results], axis=0)
```

```python
import math
from contextlib import ExitStack

import ml_dtypes
import numpy as np

import concourse.bass as bass
import concourse.mybir as mybir
from concourse.bass_utils import run_bass_kernel_spmd

F32 = mybir.dt.float32
BF16 = mybir.dt.bfloat16
I32 = mybir.dt.int32
AF = mybir.ActivationFunctionType
ALU = mybir.AluOpType
AX = mybir.AxisListType

D = 1024
H = 8
NE = 32
EH = 512
IN_W = 6144
OFF_U, OFF_V, OFF_Q, OFF_K, OFF_VAL, OFF_GA, OFF_GB = 0, 512, 1024, 2048, 3072, 4096, 5120
ALPHA = 2.0 ** 0.25
LN_EPS = 1e-5
RMS_EPS = 1e-5
LAMBDA_INIT = 0.8 - 0.6 * math.exp(-0.3 * 0)
MBLK = 256


class Sem:
    def __init__(self, h):
        self.h = h
        self.v = 0


class Prog:
    ENG = {"pe": "tensor", "act": "scalar", "dve": "vector", "pool": "gpsimd", "sp": "sync"}

    def __init__(self, nc, es):
        self.nc = nc
        self.es = es
        self.q = {k: [] for k in self.ENG}
        self.esem = {k: self.sem("e_" + k) for k in self.ENG}
        self.nsem = 0

    def sem(self, name):
        sm = Sem(self.es.enter_context(self.nc.semaphore(name)))
        if not hasattr(self, "all_sems"):
            self.all_sems = []
        self.all_sems.append(sm)
        return sm

    def barrier(self):
        toks = [(sm, sm.v) for sm in self.all_sems if sm.v > 0]
        for k in self.ENG:
            self.q[k].append((lambda e: e.nop(), self._w(toks), None, 1))

    def op(self, eng, fn, waits=(), sig=True):
        if eng in ("dve", "pool"):
            sig = True
            if self.esem[eng].v:
                waits = list(waits) + [(self.esem[eng], self.esem[eng].v)]
        s = self.esem[eng] if sig else None
        tok = None
        if s is not None:
            s.v += 1
            tok = (s, s.v)
        self.q[eng].append((fn, self._w(waits), s, 1))
        return tok

    def dma(self, eng, fn, sem, waits=()):
        sem.v += 16
        self.q[eng].append((fn, self._w(waits), sem, 16))
        return (sem, sem.v)

    @staticmethod
    def _w(waits):
        best = {}
        for t in waits:
            if t is None:
                continue
            s, v = t
            if id(s) not in best or best[id(s)][1] < v:
                best[id(s)] = (s, v)
        return tuple(best.values())

    def replay(self):
        nc = self.nc
        with nc.Block() as block:
            for k, bn in self.ENG.items():
                items = self.q[k]
                own = self.esem[k]

                def body(eng, items=items, own=own):
                    for fn, waits, s, amt in items:
                        for (ws, wv) in waits:
                            eng.wait_ge(ws.h, wv)
                        ins = fn(eng)
                        if s is not None:
                            ins.then_inc(s.h, amt)

                getattr(block, bn)(body)
        self.q = {k: [] for k in self.ENG}


def _slopes():
    return [2.0 ** (-8.0 * (h + 1) / H) for h in range(H)]


def make_consts(S):
    bf = ml_dtypes.bfloat16
    c = {}
    c["ident_f"] = np.eye(128, dtype=np.float32)
    c["ident_b"] = np.eye(128, dtype=np.float32).astype(bf)
    tri = (np.arange(128)[:, None] < np.arange(128)[None, :]).astype(np.float32)
    c["triu_b"] = tri.astype(bf)
    c["ones_b"] = np.ones((128, 128), np.float32).astype(bf)
    pos = np.arange(S)
    blk = (pos // 128).astype(np.float32)
    r = (pos % 128).astype(np.float32)
    qa = np.zeros((H, 4, S), np.float32)
    ka = np.zeros((H, 4, S), np.float32)
    corr = np.zeros((H, 128, 128), np.float32)
    kr = np.arange(128)[:, None]
    qr = np.arange(128)[None, :]
    for h, sl in enumerate(_slopes()):
        qa[h, 0] = 1.0
        qa[h, 1] = 1.0
        qa[h, 2] = -sl * 128.0 * blk
        qa[h, 3] = -sl * r
        ka[h, 0] = sl * 128.0 * blk
        ka[h, 1] = sl * r
        ka[h, 2] = 1.0
        ka[h, 3] = 1.0
        cm = np.where(kr > qr, -2.0 * sl * (kr - qr), 0.0)
        cm = np.where((kr // 64) > (qr // 64), -30000.0, cm)
        corr[h] = cm
    c["qaug"] = qa.astype(bf)
    c["kaug"] = ka.astype(bf)
    c["corrT"] = np.ascontiguousarray(corr.transpose(1, 0, 2)).astype(bf)
    nb = S // 128
    c["tokidx"] = (np.arange(nb)[None, :] * 128 + np.arange(128)[:, None]).astype(np.int32)
    nblk = 2 * S // MBLK + NE
    c["blkstart"] = np.broadcast_to((np.arange(nblk) * float(MBLK))[None, :], (128, nblk)).astype(np.float32).copy()
    return c


CONST_DT = {"ident_f": F32, "ident_b": BF16, "triu_b": BF16, "ones_b": BF16, "qaug": BF16, "kaug": BF16,
            "corrT": BF16, "tokidx": I32, "blkstart": F32}

IN_SHAPES = {
    "w_in": [1, D, IN_W], "b_in": [1, IN_W], "sg_ln_g": [1, 512], "sg_ln_b": [1, 512],
    "sg_w": [1, 8, 128, 128], "sg_b": [1, 8, 128], "w_branch_a": [1, 512, D],
    "lam_q1": [1, 64], "lam_k1": [1, 64], "lam_q2": [1, 64], "lam_k2": [1, 64], "subln_g": [1, 128],
    "w_branch_b": [1, D, D], "w_out": [1, D, D], "ln1_g": [1, D], "ln1_b": [1, D],
    "w_group": [1, D, 4], "b_group": [1, 4], "w_expert": [1, D, NE], "b_expert": [1, NE],
    "w_gate": [1, NE, D, EH], "w_up": [1, NE, D, EH], "w_down": [1, NE, EH, D],
    "ln2_g": [1, D], "ln2_b": [1, D],
}


def build(S, debug=False, phases=(1, 2, 3, 4)):
    nc = bass.Bass("TRN2", target_bir_lowering=False)
    NT = S // 512
    NB = S // 128
    NBLK = 2 * S // MBLK + NE
    NSLOT = NBLK * MBLK
    NCH = NSLOT // 128
    consts = make_consts(S)

    A = {}
    A["x"] = nc.dram_tensor("x", [S, D], F32, kind="ExternalInput").ap()
    for k, shp in IN_SHAPES.items():
        A[k] = nc.dram_tensor(k, shp, F32, kind="ExternalInput").ap()
    for k, v in consts.items():
        A[k] = nc.dram_tensor(k, list(v.shape), CONST_DT[k], kind="ExternalInput").ap()
    out = nc.dram_tensor("out", [S, D], F32, kind="ExternalOutput").ap()
    skind = "ExternalOutput" if debug else "Internal"

    def scratch(name, shape, dt):
        return nc.dram_tensor(name, shape, dt, kind=skind).ap()

    qT_s = scratch("qT_s", [H, 2, 64, S], BF16)
    kT_s = scratch("kT_s", [H, 2, 64, S], BF16)
    v_s = scratch("v_s", [S, D], BF16)
    gbT_s = scratch("gbT_s", [D, S], F32)
    maT_s = scratch("maT_s", [D, S], F32)
    aT_s = scratch("aT_s", [D, S], BF16)
    h1_s = scratch("h1_s", [S, D], F32)
    h1b_s = scratch("h1b_s", [S, D], BF16)
    slot_s = scratch("slot_s", [128, NCH], I32)
    out_s = scratch("out_s", [NSLOT, D], F32)
    wgu_r = nc.dram_tensor("wgu_r", [NE * 128, 2 * 8 * EH], BF16).ap()
    wd_r = nc.dram_tensor("wd_r", [NE * 128, 4 * D], BF16).ap()
    dbg_s = scratch("dbg_s", [128, 4096], F32) if debug else None

    with ExitStack() as es:
        P = Prog(nc, es)

        def sb(name, shape, dt, stack=es):
            return stack.enter_context(nc.sbuf_tensor("sb_" + name, shape, dt))

        bank = [es.enter_context(nc.psum_tensor(f"bank{i}", [128, 512], F32)) for i in range(7)]
        bank.append(es.enter_context(nc.psum_tensor("bank7b", [128, 1024], BF16)))
        bank_free = [None] * 8

        s_const = P.sem("const")
        s_scr = P.sem("scr")

        ident_f = sb("ident_f", [128, 128], F32)
        ident_b = sb("ident_b", [128, 128], BF16)
        P.dma("sp", lambda e: e.dma_start(out=ident_f[:], in_=A["ident_f"]), s_const)
        t_const = P.dma("sp", lambda e: e.dma_start(out=ident_b[:], in_=A["ident_b"]), s_const)

        def dbg(name, tile, tok):
            if not debug:
                return
            shp = list(tile.shape)
            d = nc.dram_tensor("dbg_" + name, shp, tile.dtype, kind="ExternalOutput").ap()
            P.dma("sp", lambda e: e.dma_start(out=d, in_=tile[:]), s_scr, waits=[tok])

        if 1 in phases:
            t_ph1 = phase1(nc, P, A, S, NT, sb, bank, bank_free, ident_f, t_const, s_scr,
                           qT_s, kT_s, v_s, gbT_s, maT_s, dbg)
        NBk = S // 128
        logits_all = sb("logits_all", [128, NBk, 36], F32)
        s_prep = P.sem("wprep")
        prep_fns = []
        if 4 in phases:
            for ex in range(NE):
                for gu, nm in enumerate(("w_gate", "w_up")):
                    prep_fns.append(lambda ex=ex, gu=gu, nm=nm: P.dma("pool", lambda e: e.dma_start(
                        out=wgu_r[ex * 128:(ex + 1) * 128, gu * 8 * EH:(gu + 1) * 8 * EH].rearrange("p (kc f) -> p kc f", kc=8),
                        in_=A[nm][0, ex].rearrange("(kc p) f -> p kc f", p=128)), s_prep))
                prep_fns.append(lambda ex=ex: P.dma("pool", lambda e: e.dma_start(
                    out=wd_r[ex * 128:(ex + 1) * 128, :].rearrange("p (kc f) -> p kc f", kc=4),
                    in_=A["w_down"][0, ex].rearrange("(kc p) f -> p kc f", p=128)), s_prep))
        if 2 not in phases:
            for f in prep_fns:
                f()
            prep_fns = []
        if 2 in phases:
            phase2(nc, P, A, S, bank, bank_free, ident_f, ident_b, t_const, s_scr, qT_s, kT_s, v_s, aT_s, dbg, prep_fns)
        t_prep = (s_prep, s_prep.v)
        if 3 in phases:
            phase3(nc, P, A, S, bank, bank_free, ident_f, t_const, s_scr, gbT_s, maT_s, aT_s, h1_s, h1b_s, logits_all, dbg)
        if 4 in phases:
            phase4(nc, P, A, S, bank, bank_free, ident_b, t_const, s_scr, h1_s, h1b_s, slot_s, out_s, out, logits_all, dbg,
                   wgu_r, wd_r, t_prep)
        P.op("sp", lambda e: e.nop(), waits=[(s_scr, s_scr.v)], sig=False)
        P.replay()
    return nc


def phase1(nc, P, A, S, NT, sb_outer, bank, bank_free, ident_f, t_const, s_scr,
           qT_s, kT_s, v_s, gbT_s, maT_s, dbg):
    w_in = A["w_in"]
    with ExitStack() as ps:
        def sb(name, shape, dt):
            return ps.enter_context(nc.sbuf_tensor("sb_" + name, shape, dt))

        s_w = P.sem("p1w")
        braw = sb("braw", [48, 128], F32)
        bias_fm = sb("bias_fm", [128, 48], F32)
        bq8 = sb("bq8", [128, 8], F32)
        bV = sb("bV", [128, 512], F32)
        bVAL = sb("bVAL", [128, 1024], F32)
        lng = sb("sglng", [128, 512], F32)
        lnb = sb("sglnb", [128, 512], F32)
        bsT = sb("bsT", [128, 4, 128], F32)
        wsraw = sb("wsraw", [128, 8, 128], F32)
        wsT = sb("wsT", [128, 8, 128], BF16)
        wba = sb("wba", [128, 4, D], BF16)
        P.dma("sp", lambda e: e.dma_start(out=braw[:], in_=A["b_in"][0].rearrange("(c p) -> c p", p=128)), s_w)
        P.dma("sp", lambda e: e.dma_start(out=bV[:], in_=A["b_in"][0, OFF_V:OFF_Q].partition_broadcast(128)), s_w)
        P.dma("sp", lambda e: e.dma_start(out=bVAL[:], in_=A["b_in"][0, OFF_VAL:OFF_GA].partition_broadcast(128)), s_w)
        P.dma("sp", lambda e: e.dma_start(out=lng[:], in_=A["sg_ln_g"][0].partition_broadcast(128)), s_w)
        P.dma("sp", lambda e: e.dma_start(out=lnb[:], in_=A["sg_ln_b"][0].partition_broadcast(128)), s_w)
        for g in range(8):
            P.dma("sp", lambda e, g=g: e.dma_start(out=bsT[(g % 2) * 64:(g % 2) * 64 + 64, g // 2, :],
                                                  in_=A["sg_b"][0, g].partition_broadcast(64)), s_w)
        P.dma("sp", lambda e: e.dma_start(out=wsraw[:], in_=A["sg_w"][0].rearrange("g t s -> t g s")), s_w)
        s_wba = P.sem("p1wba")
        for kc in range(4):
            P.dma("pool", lambda e, kc=kc: e.dma_start(out=wba[:, kc, :], in_=A["w_branch_a"][0, kc * 128:(kc + 1) * 128, :]), s_wba)
        t_w = (s_w, s_w.v)
        t_wba = (s_wba, s_wba.v)
        wB = sb("wB", [128, 8, 4096], BF16)
        s_wB = P.sem("wB")

        t = P.op("pe", lambda e: e.transpose(out=bank[0][:, 0:48], in_=braw[:], identity=ident_f[0:48, 0:48]),
                 waits=[t_w, t_const])
        t = P.op("dve", lambda e: e.tensor_copy(out=bias_fm[:], in_=bank[0][:, 0:48]), waits=[t])
        t_bias = P.op("dve", lambda e: e.tensor_scalar(out=bq8[:], in0=bias_fm[:, 8:16], scalar1=0.125, scalar2=None,
                                                       op0=ALU.mult), waits=[t])
        tt = []
        for g in range(8):
            tt.append(P.op("pe", lambda e, g=g: e.transpose(out=bank[1 + g // 4][:, (g % 4) * 128:(g % 4) * 128 + 128],
                                                            in_=wsraw[:, g, :], identity=ident_f[:]), waits=[t_w]))
        t = P.op("dve", lambda e: e.tensor_copy(out=wsT[:, 0:4, :], in_=bank[1][:].rearrange("p (g t) -> p g t", g=4)),
                 waits=[tt[3]])
        t = P.op("dve", lambda e: e.tensor_copy(out=wsT[:, 4:8, :], in_=bank[2][:].rearrange("p (g t) -> p g t", g=4)),
                 waits=[tt[7]])
        t_ws = P.op("dve", lambda e: e.memset(wsT[64:128, :, 0:64], 0.0))
        for b in range(3):
            bank_free[b] = t_ws

        with ExitStack() as pa:
            def sba(name, shape, dt):
                return pa.enter_context(nc.sbuf_tensor("sb_" + name, shape, dt))
            wA = sba("wA", [128, 8, 2048], BF16)
            s_wA = P.sem("wA")
            for kc in range(8):
                for (dst, src, n) in ((0, OFF_U, 1024), (1024, OFF_GA, 1024)):
                    P.dma("pool", lambda e, kc=kc, dst=dst, src=src, n=n: e.dma_start(
                        out=wA[:, kc, dst:dst + n], in_=w_in[0, kc * 128:(kc + 1) * 128, src:src + n]), s_wA)
            t_wA = (s_wA, s_wA.v)
            for kc in range(8):
                for (dst, src) in ((0, OFF_Q), (1024, OFF_K), (2048, OFF_VAL), (3072, OFF_GB)):
                    P.dma("pool", lambda e, kc=kc, dst=dst, src=src: e.dma_start(
                        out=wB[:, kc, dst:dst + 1024], in_=w_in[0, kc * 128:(kc + 1) * 128, src:src + 1024]), s_wB)
            xt = [sba("xtA0", [128, 4, D], F32)] * 2
            xt_shared = True
            xT = [sba(f"xTA{i}", [128, 8, 512], BF16) for i in range(2)]
            s_xt = [P.sem("xtA0")] * 2
            uT = sba("uT", [128, 4, 512], BF16)
            sguT = sba("sguT", [128, 4, 512], BF16)
            ga = sba("ga", [128, 8, 512], F32)
            ma_st = [sba(f"ma_st{i}", [128, 512], F32) for i in range(2)]
            s_ma = [P.sem(f"ma{i}") for i in range(2)]
            vg0 = [sba(f"vg0_{i}", [128, 512], F32) for i in range(4)]
            vg0_free = [None] * 4
            neghalfA = sba("neghalfA", [128, 1], F32)
            P.op("dve", lambda e: e.memset(neghalfA[:], -0.5))
            vg1 = sba("vg1", [128, 512], F32)
            vg2 = sba("vg2", [128, 512], F32)
            vln = [sba(f"vln{i}", [128, 512], BF16) for i in range(4)]
            mixs = sba("mixs", [128, 512], F32)
            st6 = sba("st6", [128, 6], F32)
            mv = sba("mv", [128, 2], F32)
            rs = sba("rs", [128, 2], F32)

            xt_free = [None, None]
            xT_free = [None, None]
            ma_free = [None, None]
            vln_free = [None] * 4
            uT_free = None
            sgu_free = None
            ga_free = None
            vg_free = None
            mixs_free = None
            acc_rr = [0]

            def acc_bank():
                b = 2 + acc_rr[0] % 3
                acc_rr[0] += 1
                return b

            for tt_i in range(NT):
                tok0 = tt_i * 512
                xb = tt_i % 2
                if tt_i == 0:
                    t_ld_next = P.dma("sp", lambda e: e.dma_start(
                        out=xt[0][:], in_=A["x"][0:512, :].rearrange("(b p) d -> p b d", p=128)), s_xt[0])
                t_ld = t_ld_next
                t_cp = None
                for kc in range(8):
                    pb = kc % 2
                    for tb in range(4):
                        t = P.op("pe", lambda e, pb=pb, tb=tb, kc=kc, xb=xb: e.transpose(
                            out=bank[pb][:, tb * 128:(tb + 1) * 128], in_=xt[xb][:, tb, kc * 128:(kc + 1) * 128],
                            identity=ident_f[:]), waits=[t_ld, bank_free[pb], t_const] if tb == 0 else [])
                    if kc % 2 == 0:
                        t_cp = P.op("act", lambda e, pb=pb, kc=kc, xb=xb: e.copy(out=xT[xb][:, kc, :], in_=bank[pb][:]),
                                    waits=[t, xT_free[xb]])
                    else:
                        t_cp = P.op("dve", lambda e, pb=pb, kc=kc, xb=xb: e.tensor_copy(out=xT[xb][:, kc, :], in_=bank[pb][:]),
                                    waits=[t, xT_free[xb]])
                    bank_free[pb] = t_cp
                    if kc == 6:
                        t_cp6 = t_cp
                xt_free[0] = t
                xt_free[1] = t
                t_xT = [t_cp6, t_cp]
                if tt_i + 1 < NT:
                    t_ld_next = P.dma("sp", lambda e, tok1=tok0 + 512: e.dma_start(
                        out=xt[0][:], in_=A["x"][tok1:tok1 + 512, :].rearrange("(b p) d -> p b d", p=128)),
                        s_xt[0], waits=[t])

                def fm_group(col0, b, first_waits):
                    tk = None
                    for kc in range(8):
                        tk = P.op("pe", lambda e, kc=kc, col0=col0, b=b, xb=xb: e.matmul(
                            bank[b][:], wA[:, kc, col0:col0 + 128], xT[xb][:, kc, :], start=(kc == 0), stop=(kc == 7)),
                            waits=first_waits if kc == 0 else [], sig=(kc == 7))
                    return tk

                for ch in range(4):
                    b = acc_bank()
                    t = fm_group(ch * 128, b, [t_wA, bank_free[b]] + t_xT)
                    t = P.op("act", lambda e, b=b, ch=ch: e.activation(out=uT[:, ch, :], in_=bank[b][:], func=AF.Gelu,
                                                                       bias=bias_fm[:, ch:ch + 1]),
                             waits=[t, t_bias, uT_free])
                    bank_free[b] = t
                t_uT = t
                t_vln = [None] * 4
                for tb in range(4):
                    b = acc_bank()
                    tk = None
                    for kc in range(8):
                        tk = P.op("pe", lambda e, kc=kc, b=b, tb=tb, xb=xb: e.matmul(
                            bank[b][:], xT[xb][:, kc, tb * 128:(tb + 1) * 128], wA[:, kc, 512:1024],
                            start=(kc == 0), stop=(kc == 7)),
                            waits=[t_wA, bank_free[b]] + t_xT if kc == 0 else [], sig=(kc == 7))
                    t = P.op("dve", lambda e, b=b, tb=tb: e.tensor_tensor(out=vg0[tb][:], in0=bank[b][:], in1=bV[:], op=ALU.add),
                             waits=[tk, t_w, vg0_free[tb]])
                    bank_free[b] = t
                    t = P.op("act", lambda e, tb=tb: e.activation(out=vg1[:], in_=vg0[tb][:], func=AF.Gelu), waits=[t, vg_free])
                    vg0_free[tb] = t
                    t = P.op("dve", lambda e: e.bn_stats(out=st6[:], in_=vg1[:]), waits=[t])
                    t = P.op("dve", lambda e: e.bn_aggr(out=mv[:], in_=st6[:]), waits=[t])
                    t = P.op("dve", lambda e: e.tensor_scalar(out=rs[:, 0:1], in0=mv[:, 1:2], scalar1=LN_EPS, scalar2=None,
                                                              op0=ALU.add), waits=[t])
                    t = P.op("pool", lambda e: e.tensor_tensor(out=rs[:, 1:2], in0=rs[:, 0:1], in1=neghalfA[:], op=ALU.pow), waits=[t])
                    t = P.op("dve", lambda e: e.tensor_scalar(out=vg2[:], in0=vg1[:], scalar1=mv[:, 0:1], scalar2=rs[:, 1:2],
                                                              op0=ALU.subtract, op1=ALU.mult), waits=[t])
                    vg_free = t
                    t = P.op("pool", lambda e: e.tensor_tensor(out=vg2[:], in0=vg2[:], in1=lng[:], op=ALU.mult), waits=[t])
                    t = P.op("pool", lambda e, tb=tb: e.tensor_tensor(out=vln[tb][:], in0=vg2[:], in1=lnb[:], op=ALU.add),
                             waits=[t, vln_free[tb]])
                    t_vln[tb] = t
                for m in range(8):
                    b = acc_bank()
                    t = fm_group(1024 + m * 128, b, [t_wA, bank_free[b]] + t_xT)
                    if m == 7:
                        xT_free[xb] = t
                    t = P.op("act", lambda e, b=b, m=m: e.activation(out=ga[:, m, :], in_=bank[b][:], func=AF.Sigmoid,
                                                                     bias=bias_fm[:, 32 + m:33 + m]),
                             waits=[t, t_bias, ga_free])
                    bank_free[b] = t
                t_ga = t
                for tb in range(4):
                    for gp in range(4):
                        P.op("pe", lambda e, tb=tb, gp=gp: e.matmul(
                            bank[5][:, gp * 128:(gp + 1) * 128], vln[tb][:, gp * 128:(gp + 1) * 128], wsT[:, 2 * gp, :],
                            start=True, stop=True), waits=[t_vln[tb], t_ws, bank_free[5]] if gp == 0 else [], sig=False)
                    for gp in range(4):
                        tm = P.op("pe", lambda e, tb=tb, gp=gp: e.matmul(
                            bank[6][:, gp * 128:(gp + 1) * 128], vln[tb][:, gp * 128:(gp + 1) * 128], wsT[:, 2 * gp + 1, :],
                            start=True, stop=True), waits=[bank_free[6]] if gp == 0 else [], sig=(gp == 3))
                    vln_free[tb] = tm
                    t = P.op("dve", lambda e: e.tensor_tensor(out=mixs[0:64, :], in0=bank[5][0:64, :],
                                                              in1=bsT[0:64, :, :].rearrange("p g t -> p (g t)"), op=ALU.add),
                             waits=[tm, mixs_free])
                    bank_free[5] = t
                    t = P.op("dve", lambda e: e.tensor_tensor(out=mixs[64:128, :], in0=bank[6][64:128, :],
                                                              in1=bsT[64:128, :, :].rearrange("p g t -> p (g t)"), op=ALU.add),
                             waits=[t])
                    bank_free[6] = t
                    t = P.op("dve", lambda e, tb=tb: e.tensor_tensor(
                        out=sguT[:, :, tb * 128:(tb + 1) * 128], in0=mixs[:].rearrange("p (g t) -> p g t", g=4),
                        in1=uT[:, :, tb * 128:(tb + 1) * 128], op=ALU.mult), waits=[t, t_uT, sgu_free])
                    mixs_free = t
                t_sgu = t
                uT_free = t
                for m in range(8):
                    b = acc_bank()
                    tk = None
                    for kc in range(4):
                        tk = P.op("pe", lambda e, kc=kc, b=b, m=m: e.matmul(
                            bank[b][:], wba[:, kc, m * 128:(m + 1) * 128], sguT[:, kc, :], start=(kc == 0), stop=(kc == 3)),
                            waits=[t_sgu, t_wba, bank_free[b]] if kc == 0 else [], sig=(kc == 3))
                    r = m % 2
                    t = P.op("dve", lambda e, b=b, m=m, r=r: e.tensor_tensor(out=ma_st[r][:], in0=bank[b][:], in1=ga[:, m, :],
                                                                            op=ALU.mult), waits=[tk, t_ga, ma_free[r]])
                    bank_free[b] = t
                    ma_free[r] = P.dma("sp", lambda e, m=m, r=r, tok0=tok0: e.dma_start(
                        out=maT_s[m * 128:(m + 1) * 128, tok0:tok0 + 512], in_=ma_st[r][:]), s_ma[r], waits=[t])
                    if m == 7:
                        sgu_free = tk
                ga_free = t
            t_endA = [ma_free[0], ma_free[1], t]
            for nm, tl in (("uT", uT), ("vg0", vg0[3]), ("vg1", vg1), ("vg2", vg2), ("mv", mv), ("rs", rs), ("vln1", vln[1]),
                           ("mixs", mixs), ("sguT", sguT), ("ga", ga), ("wsT", wsT), ("bsT", bsT), ("bias_fm", bias_fm), ("bV", bV), ("lng", lng), ("xTA1", xT[1])):
                dbg(nm, tl, t)
            if s_scr.v:
                t_endA.append((s_scr, s_scr.v))
            P.barrier()
            P.replay()
        with ExitStack() as pb_:
            def sbb(name, shape, dt):
                return pb_.enter_context(nc.sbuf_tensor("sb_" + name, shape, dt))
            t_wB = (s_wB, s_wB.v)
            xt = [sbb(f"xtB{i}", [128, 4, D], F32) for i in range(2)]
            xT = [sbb(f"xTB{i}", [128, 8, 512], BF16) for i in range(2)]
            s_xt = [P.sem(f"xtB{i}") for i in range(2)]
            qk_st = [sbb(f"qk_st{i}", [128, 512], BF16) for i in range(3)]
            s_qk = [P.sem(f"qk{i}") for i in range(3)]
            gb_st = [sbb(f"gb_st{i}", [128, 512], F32) for i in range(2)]
            s_gb = [P.sem(f"gb{i}") for i in range(2)]
            val_st = [sbb(f"val_st{i}", [128, 1024], BF16) for i in range(2)]
            s_val = [P.sem(f"val{i}") for i in range(2)]
            xt_free = [None, None]
            xT_free = [None, None]
            qk_free = [None] * 3
            gb_free = [None] * 2
            val_free = [None] * 2
            acc_rr = [0]
            qk_rr = 0

            def acc_bank():
                b = 2 + acc_rr[0] % 4
                acc_rr[0] += 1
                return b

            for tt_i in range(NT):
                tok0 = tt_i * 512
                xb = tt_i % 2
                if tt_i == 0:
                    t_ld_nextB = P.dma("sp", lambda e: e.dma_start(
                        out=xt[0][:], in_=A["x"][0:512, :].rearrange("(b p) d -> p b d", p=128)), s_xt[0], waits=t_endA)
                t_ld = t_ld_nextB
                if tt_i + 1 < NT:
                    xn = (tt_i + 1) % 2
                    t_ld_nextB = P.dma("sp", lambda e, xn=xn, tok1=tok0 + 512: e.dma_start(
                        out=xt[xn][:], in_=A["x"][tok1:tok1 + 512, :].rearrange("(b p) d -> p b d", p=128)),
                        s_xt[xn], waits=[xt_free[xn]] + t_endA)
                t_cp = None
                for kc in range(8):
                    pb = kc % 2
                    for tb in range(4):
                        t = P.op("pe", lambda e, pb=pb, tb=tb, kc=kc, xb=xb: e.transpose(
                            out=bank[pb][:, tb * 128:(tb + 1) * 128], in_=xt[xb][:, tb, kc * 128:(kc + 1) * 128],
                            identity=ident_f[:]), waits=[t_ld, bank_free[pb]] if tb == 0 else [])
                    if kc % 2 == 0:
                        t_cp = P.op("act", lambda e, pb=pb, kc=kc, xb=xb: e.copy(out=xT[xb][:, kc, :], in_=bank[pb][:]),
                                    waits=[t, xT_free[xb]])
                    else:
                        t_cp = P.op("dve", lambda e, pb=pb, kc=kc, xb=xb: e.tensor_copy(out=xT[xb][:, kc, :], in_=bank[pb][:]),
                                    waits=[t, xT_free[xb]])
                    bank_free[pb] = t_cp
                    if kc == 6:
                        t_cp6 = t_cp
                xt_free[xb] = t
                t_xT = [t_cp6, t_cp]

                def fm_group(col0, b, first_waits):
                    tk = None
                    for kc in range(8):
                        tk = P.op("pe", lambda e, kc=kc, col0=col0, b=b, xb=xb: e.matmul(
                            bank[b][:], wB[:, kc, col0:col0 + 128], xT[xb][:, kc, :], start=(kc == 0), stop=(kc == 7)),
                            waits=first_waits if kc == 0 else [], sig=(kc == 7))
                    return tk

                for ch in range(16):
                    isq = ch < 8
                    h = ch % 8
                    b = acc_bank()
                    t = fm_group(ch * 128, b, [t_wB, bank_free[b]] + t_xT)
                    r = qk_rr % 3
                    qk_rr += 1
                    if isq:
                        t = P.op("dve", lambda e, b=b, h=h, r=r: e.tensor_scalar(
                            out=qk_st[r][:], in0=bank[b][:], scalar1=0.125, scalar2=bq8[:, h:h + 1],
                            op0=ALU.mult, op1=ALU.add), waits=[t, t_bias, qk_free[r]])
                    else:
                        t = P.op("dve", lambda e, b=b, h=h, r=r: e.tensor_scalar(
                            out=qk_st[r][:], in0=bank[b][:], scalar1=bias_fm[:, 16 + h:17 + h], scalar2=None,
                            op0=ALU.add), waits=[t, t_bias, qk_free[r]])
                    bank_free[b] = t
                    dst = qT_s if isq else kT_s
                    for c in range(2):
                        qk_free[r] = P.dma("sp", lambda e, dst=dst, h=h, c=c, r=r, tok0=tok0: e.dma_start(
                            out=dst[h, c, :, tok0:tok0 + 512], in_=qk_st[r][c * 64:(c + 1) * 64, :]), s_qk[r], waits=[t])
                for tb in range(4):
                    r = tb % 2
                    for half in range(2):
                        b = acc_bank()
                        tk = None
                        for kc in range(8):
                            tk = P.op("pe", lambda e, kc=kc, b=b, tb=tb, half=half, xb=xb: e.matmul(
                                bank[b][:], xT[xb][:, kc, tb * 128:(tb + 1) * 128],
                                wB[:, kc, 2048 + half * 512:2048 + (half + 1) * 512], start=(kc == 0), stop=(kc == 7)),
                                waits=[t_wB, bank_free[b]] + t_xT if kc == 0 else [], sig=(kc == 7))
                        t = P.op("dve", lambda e, b=b, r=r, half=half: e.tensor_tensor(
                            out=val_st[r][:, half * 512:(half + 1) * 512], in0=bank[b][:],
                            in1=bVAL[:, half * 512:(half + 1) * 512], op=ALU.add), waits=[tk, t_w, val_free[r]])
                        bank_free[b] = t
                    val_free[r] = P.dma("sp", lambda e, r=r, tb=tb, tok0=tok0: e.dma_start(
                        out=v_s[tok0 + tb * 128:tok0 + (tb + 1) * 128, :], in_=val_st[r][:]), s_val[r], waits=[t])
                for m in range(8):
                    b = acc_bank()
                    t = fm_group(3072 + m * 128, b, [t_wB, bank_free[b]] + t_xT)
                    if m == 7:
                        xT_free[xb] = t
                    r = m % 2
                    t = P.op("act", lambda e, b=b, m=m, r=r: e.activation(out=gb_st[r][:], in_=bank[b][:], func=AF.Sigmoid,
                                                                         bias=bias_fm[:, 40 + m:41 + m]),
                             waits=[t, t_bias, gb_free[r]])
                    bank_free[b] = t
                    gb_free[r] = P.dma("sp", lambda e, m=m, r=r, tok0=tok0: e.dma_start(
                        out=gbT_s[m * 128:(m + 1) * 128, tok0:tok0 + 512], in_=gb_st[r][:]), s_gb[r], waits=[t])
            t_endB = [x for x in (qk_free + gb_free + val_free) if x is not None] + [t]
            P.barrier()
            P.replay()
    return t_endA + t_endB


def phase2(nc, P, A, S, bank, bank_free, ident_f, ident_b, t_const, s_scr, qT_s, kT_s, v_s, aT_s, dbg, prep_fns=()):
    NB = S // 128
    NQP = S // 256
    t_prev = (s_scr, s_scr.v)
    with ExitStack() as ps:
        def sb(name, shape, dt):
            return ps.enter_context(nc.sbuf_tensor("sb_" + name, shape, dt))
        qa2 = [[sb(f"qa{p}{c}", [68, S], BF16) for c in range(2)] for p in range(2)]
        ka2 = [[sb(f"ka{p}{c}", [68, S], BF16) for c in range(2)] for p in range(2)]
        Vh2 = [sb(f"Vh{p}", [128, NB, 129], BF16) for p in range(2)]
        Osb = sb("Osb", [128, 2, 2, 129], F32)
        rc4 = sb("rc4", [128, 2, 2], F32)
        r1l = sb("r1l", [128, 2], F32)
        ssq = sb("ssq", [128, 2], F32)
        rstd2 = sb("rstd2", [128, 2], F32)
        corrT = sb("corrT", [128, H, 128], BF16)
        lamt = sb("lamt", [128, 4, 64], F32)
        lamw = sb("lamw", [128, 2, 64], F32)
        lams = sb("lams", [128, 4], F32)
        neglam = sb("neglam", [128, 1], F32)
        neghalf = sb("neghalf", [128, 1], F32)
        neghalf2 = sb("neghalf2", [128, 2], F32)
        PT = [sb(f"PT{i}", [128, 512], BF16) for i in range(3)]
        rcp = [sb(f"rcp{j}", [128, 4], F32) for j in range(2)]
        Abuf = [sb(f"Abuf{j}", [128, 128], F32) for j in range(2)]
        Dbuf = [sb(f"Dbuf{j}", [128, 128], F32) for j in range(2)]
        sq = sb("sqjunk", [128, 128], F32)
        On = [[sb(f"On{j}{p}", [128, 128], BF16) for p in range(2)] for j in range(2)]
        aT_st = [sb(f"aT_st{i}", [128, 256], BF16) for i in range(2)]
        s_c2 = P.sem("p2c")
        s_ld2 = [P.sem(f"p2ld{p}") for p in range(2)]
        s_ast = [P.sem(f"p2a{i}") for i in range(2)]

        P.dma("sp", lambda e: e.dma_start(out=corrT[:], in_=A["corrT"]), s_c2, waits=[t_prev])
        for i, nm in enumerate(("lam_q1", "lam_k1", "lam_q2", "lam_k2")):
            P.dma("sp", lambda e, i=i, nm=nm: e.dma_start(out=lamt[:, i, :], in_=A[nm][0].partition_broadcast(128)), s_c2)
        t_c2 = (s_c2, s_c2.v)
        P.op("dve", lambda e: e.memset(Vh2[0][:, :, 128:129], 1.0), waits=[t_prev])
        t = P.op("dve", lambda e: e.memset(Vh2[1][:, :, 128:129], 1.0))
        t_ones = t
        P.op("dve", lambda e: e.memset(neghalf[:], -0.5), sig=False)
        P.op("dve", lambda e: e.memset(neghalf2[:], -0.5), sig=False)
        P.op("dve", lambda e: e.tensor_tensor(out=lamw[:, 0, :], in0=lamt[:, 0, :], in1=lamt[:, 1, :], op=ALU.mult),
             waits=[t_c2], sig=False)
        t = P.op("dve", lambda e: e.tensor_tensor(out=lamw[:, 1, :], in0=lamt[:, 2, :], in1=lamt[:, 3, :], op=ALU.mult))
        t = P.op("dve", lambda e: e.tensor_reduce(out=lams[:, 0:2], in_=lamw[:], axis=AX.X, op=ALU.add), waits=[t])
        t = P.op("act", lambda e: e.activation(out=lams[:, 2:4], in_=lams[:, 0:2], func=AF.Exp), waits=[t])
        t = P.op("dve", lambda e: e.tensor_tensor(out=lams[:, 0:1], in0=lams[:, 3:4], in1=lams[:, 2:3], op=ALU.subtract),
                 waits=[t])
        t_lam = P.op("dve", lambda e: e.tensor_scalar(out=neglam[:], in0=lams[:, 0:1], scalar1=-LAMBDA_INIT, scalar2=None,
                                                      op0=ALU.add), waits=[t])

        ST = [bank[0], bank[1], bank[6]]
        OB = [[bank[2], bank[3]], [bank[4], bank[5]]]
        TB = bank[7]
        st_free = [bank_free[0], bank_free[1], bank_free[6]]
        pt_free = [None, None, None]
        o_free = [[bank_free[2], bank_free[3]], [bank_free[4], bank_free[5]]]
        tb_free = bank_free[7]
        ast_free = [None, None]
        on_free = [[None, None], [None, None]]
        osb_free = [None]
        n_ast = 0

        head_done = [None] * H
        t_hlds = [None] * H

        def issue_loads(h):
            p = h % 2
            w = [t_prev] + (head_done[h - 2] if h >= 2 else [])
            sl = s_ld2[p]
            for c in range(2):
                P.dma("sp", lambda e, c=c, h=h, p=p: e.dma_start(out=qa2[p][c][0:64, :], in_=qT_s[h, c]), sl, waits=w)
                P.dma("sp", lambda e, c=c, h=h, p=p: e.dma_start(out=qa2[p][c][64:68, :], in_=A["qaug"][h]), sl)
                P.dma("sp", lambda e, c=c, h=h, p=p: e.dma_start(out=ka2[p][c][0:64, :], in_=kT_s[h, c]), sl)
                P.dma("sp", lambda e, c=c, h=h, p=p: e.dma_start(out=ka2[p][c][64:68, :], in_=A["kaug"][h]), sl)
            P.dma("sp", lambda e, h=h, p=p: e.dma_start(
                out=Vh2[p][:, :, 0:128], in_=v_s.rearrange("(kb p) d -> p kb d", p=128)[:, :, h * 128:(h + 1) * 128]), sl)
            t_hlds[h] = (sl, sl.v)

        issue_loads(0)
        slopes = _slopes()
        for h in range(H):
            if h + 1 < H:
                issue_loads(h + 1)
            npf = (len(prep_fns) + H - 1) // H
            for f in prep_fns[h * npf:(h + 1) * npf]:
                f()
            t_hld = t_hlds[h]
            qa, ka, Vh = qa2[h % 2], ka2[h % 2], Vh2[h % 2]
            def kb_first(qp, h=h):
                for kb in range(2 * qp + 2):
                    if slopes[h] * (256 * qp - (128 * kb + 127)) < 64.0:
                        return kb
                return 2 * qp
            kb0 = [kb_first(qp) for qp in range(NQP)]
            units = [(qp, kb) for qp in range(NQP) for kb in range(kb0[qp], 2 * qp + 2)]
            chain_q = []
            deferred = []
            qk_tok = {}

            def emit_qk(i):
                qp, kb = units[i]
                b = i % 3
                q0 = qp * 256
                last = (kb == 2 * qp + 1)
                diag0 = (kb == 2 * qp)
                tk = None
                for c in range(2):
                    w = ([st_free[b]] + ([t_hld, t_c2] if i < 3 else [])) if c == 0 else []
                    if last:
                        P.op("pe", lambda e, c=c, b=b, kb=kb, q0=q0, ka=ka, qa=qa: e.matmul(
                            ST[b][:, c * 256 + 128:c * 256 + 256], ka[c][0:68, kb * 128:(kb + 1) * 128],
                            qa[c][0:68, q0 + 128:q0 + 256], start=True, stop=False), waits=w, sig=False)
                        tk = P.op("pe", lambda e, c=c, b=b, h=h: e.matmul(
                            ST[b][:, c * 256 + 128:c * 256 + 256], ident_b[:], corrT[:, h, :], start=False, stop=True), sig=(c == 1))
                    else:
                        tk = P.op("pe", lambda e, c=c, b=b, kb=kb, q0=q0, diag0=diag0, ka=ka, qa=qa: e.matmul(
                            ST[b][:, c * 256:c * 256 + 256], ka[c][0:68, kb * 128:(kb + 1) * 128],
                            qa[c][0:68, q0:q0 + 256], start=True, stop=not diag0), waits=w, sig=(not diag0 and c == 1))
                        if diag0:
                            tk = P.op("pe", lambda e, c=c, b=b, h=h: e.matmul(
                                ST[b][:, c * 256:c * 256 + 128], ident_b[:], corrT[:, h, :], start=False, stop=True), sig=(c == 1))
                qk_tok[i] = tk

            emit_qk(0)
            if len(units) > 1:
                emit_qk(1)
            for i, (qp, kb) in enumerate(units):
                b = i % 3
                last = (kb == 2 * qp + 1)
                if last:
                    src = ST[b][:].rearrange("p (c j q) -> p c j q", c=2, j=2)[:, :, 1, :]
                    dst = PT[b][:].rearrange("p (c j q) -> p c j q", c=2, j=2)[:, :, 1, :]
                else:
                    src = ST[b][:]
                    dst = PT[b][:]
                t_exp = P.op("act", lambda e, src=src, dst=dst: e.activation(out=dst, in_=src, func=AF.Exp),
                             waits=[qk_tok[i], pt_free[b]])
                st_free[b] = t_exp
                if i + 2 < len(units):
                    emit_qk(i + 2)
                while deferred and deferred[0][0] <= i and deferred[0][2] <= qp - 1:
                    deferred.pop(0)[1]()
                tk = None
                pv_list = [(j, c) for j in range(2) for c in range(2) if not (last and j == 0)]
                for n_, (j, c) in enumerate(pv_list):
                    stop = (kb == 2 * qp + j)
                    w = ([t_exp] + ([t_ones] if i == 0 else [])) if n_ == 0 else []
                    if kb == kb0[qp]:
                        w = w + [o_free[j][c]]
                    tk = P.op("pe", lambda e, j=j, c=c, b=b, kb=kb, stop=stop, Vh=Vh, st_=(kb == kb0[qp]): e.matmul(
                        OB[j][c][:, 0:129], PT[b][:, c * 256 + j * 128:c * 256 + (j + 1) * 128], Vh[:, kb, :],
                        start=st_, stop=stop), waits=w, sig=(n_ == len(pv_list) - 1))
                pt_free[b] = tk
                for j in range(2):
                    if kb != 2 * qp + j:
                        continue
                    t = P.op("act", lambda e, j=j: e.copy(out=Osb[:, j, 0, :], in_=OB[j][0][:, 0:129]), waits=[tk, osb_free[0]])
                    o_free[j][0] = t
                    t = P.op("act", lambda e, j=j: e.copy(out=Osb[:, j, 1, :], in_=OB[j][1][:, 0:129]), waits=[t])
                    o_free[j][1] = t
                    chain_q.append((j, qp, t))
                if last:
                  while deferred and deferred[0][2] <= qp - 2:
                      deferred.pop(0)[1]()
                  t = chain_q[-1][2]
                  t = P.op("dve", lambda e: e.reciprocal(out=rc4[:], in_=Osb[:, :, :, 128]), waits=[t])
                  t = P.op("dve", lambda e: e.tensor_scalar(out=r1l[:], in0=rc4[:, :, 1], scalar1=neglam[:, 0:1], scalar2=None,
                                                           op0=ALU.mult), waits=[t, t_lam])
                  for j in range(2):
                      t = P.op("dve", lambda e, j=j: e.tensor_scalar(out=Abuf[j][:], in0=Osb[:, j, 0, 0:128],
                                                                    scalar1=rc4[:, j, 0:1], scalar2=None, op0=ALU.mult), waits=[t])
                      t = P.op("dve", lambda e, j=j: e.scalar_tensor_tensor(out=Dbuf[j][:], in0=Osb[:, j, 1, 0:128],
                                                                           scalar=r1l[:, j:j + 1], in1=Abuf[j][:],
                                                                           op0=ALU.mult, op1=ALU.add), waits=[t])
                      osb_free[0] = t
                      t = P.op("dve", lambda e, j=j: e.scalar_tensor_tensor(out=sq[:], in0=Dbuf[j][:], scalar=1.0, in1=Dbuf[j][:],
                                                                           op0=ALU.mult, op1=ALU.mult,
                                                                           accum_out=ssq[:, j:j + 1]), waits=[t])
                  t = P.op("dve", lambda e: e.tensor_scalar(out=ssq[:], in0=ssq[:], scalar1=1.0 / 128.0, scalar2=RMS_EPS,
                                                           op0=ALU.mult, op1=ALU.add), waits=[t])
                  t = P.op("pool", lambda e: e.tensor_tensor(out=rstd2[:], in0=ssq[:], in1=neghalf2[:], op=ALU.pow), waits=[t])
                  for (j, qp_, _t) in chain_q:
                    t_on = P.op("dve", lambda e, j=j, qpar=qp_ % 2: e.tensor_scalar(out=On[j][qpar][:], in0=Dbuf[j][:],
                                                                                   scalar1=rstd2[:, j:j + 1], scalar2=None,
                                                                                   op0=ALU.mult), waits=[t, on_free[j][qp_ % 2]])

                    def fin(j=j, t_on=t_on, qp=qp_, h=h):
                        nonlocal tb_free, n_ast
                        tt = P.op("pe", lambda e, j=j, qpar=qp % 2: e.transpose(out=TB[:, j * 128:(j + 1) * 128], in_=On[j][qpar][:],
                                                                               identity=ident_b[:]),
                                  waits=[t_on] + ([tb_free] if j == 0 else []))
                        on_free[j][qp % 2] = tt
                        if j == 1:
                            r = n_ast % 2
                            n_ast += 1
                            tc = P.op("dve", lambda e, r=r: e.tensor_copy(out=aT_st[r][:], in_=TB[:, 0:256]),
                                      waits=[tt, ast_free[r]])
                            tb_free = tc
                            ast_free[r] = P.dma("sp", lambda e, r=r, qp=qp, h=h: e.dma_start(
                                out=aT_s[h * 128:(h + 1) * 128, qp * 256:(qp + 1) * 256], in_=aT_st[r][:]), s_ast[r],
                                waits=[tc])
                    deferred.append((i + 5, fin, qp_))
                  chain_q = []
            while deferred:
                deferred.pop(0)[1]()
            head_done[h] = [tk, (P.esem["pe"], P.esem["pe"].v)]
        for bi, tkn in ((0, st_free[0]), (1, st_free[1]), (6, st_free[2]), (2, o_free[0][0]), (3, o_free[0][1]), (4, o_free[1][0]),
                        (5, o_free[1][1]), (7, tb_free)):
            bank_free[bi] = tkn
        t_end = [x for x in ast_free if x is not None] + [(P.esem[k], P.esem[k].v) for k in ("pe", "act", "dve", "pool")]
        P.barrier()
        P.replay()
    return t_end


def phase3(nc, P, A, S, bank, bank_free, ident_f, t_const, s_scr, gbT_s, maT_s, aT_s, h1_s, h1b_s, logits_all, dbg):
    NT = S // 512
    t_prev = (s_scr, s_scr.v)
    with ExitStack() as ps:
        def sb(name, shape, dt):
            return ps.enter_context(nc.sbuf_tensor("sb_" + name, shape, dt))
        wbb = sb("wbb", [128, 8, D], BF16)
        wst = [sb(f"wst{i}", [128, D], F32) for i in range(2)]
        wout = sb("wout", [128, 8, D], BF16)
        sublg = sb("sublg", [128, 1], F32)
        ln1g = sb("ln1g", [128, D], F32)
        ln1b = sb("ln1b", [128, D], F32)
        wr = sb("wr", [128, 8, 36], F32)
        rbias = sb("rbias", [128, 36], F32)
        aT = [sb(f"aT{i}", [128, 8, 512], BF16) for i in range(2)]
        NGC = 6
        gbc = [sb(f"gbc{i}", [128, 512], F32) for i in range(NGC)]
        mac = [sb(f"mac{i}", [128, 512], F32) for i in range(NGC)]
        s_gc = [P.sem(f"p3gc{i}") for i in range(NGC)]
        gc_free = [None] * NGC
        t_gc = {}

        def issue_gc(ci):
            g = ci % NGC
            tt_, m_ = divmod(ci, 8)
            P.dma("sp", lambda e, g=g, tt_=tt_, m_=m_: e.dma_start(
                out=gbc[g][:], in_=gbT_s[m_ * 128:(m_ + 1) * 128, tt_ * 512:(tt_ + 1) * 512]), s_gc[g], waits=[t_prev, gc_free[g]])
            t_gc[ci] = P.dma("sp", lambda e, g=g, tt_=tt_, m_=m_: e.dma_start(
                out=mac[g][:], in_=maT_s[m_ * 128:(m_ + 1) * 128, tt_ * 512:(tt_ + 1) * 512]), s_gc[g])
        xt = [sb(f"xt3{i}", [128, 4, D], F32) for i in range(2)]
        tmpg = [sb(f"tmpg{i}", [128, 512], F32) for i in range(2)]
        mgT = sb("mgT", [128, 8, 512], BF16)
        z = [sb(f"z{i}", [128, D], F32) for i in range(2)]
        zn = [sb(f"zn{i}", [128, D], F32) for i in range(2)]
        h1 = [sb(f"h1t{i}", [128, D], F32) for i in range(2)]
        h1b = [sb(f"h1bt{i}", [128, D], BF16) for i in range(2)]
        h1T = sb("h1T", [128, 8, 128], F32)
        st12 = sb("st12", [128, 2, 6], F32)
        mv = sb("mv3", [128, 2], F32)
        rr = sb("rr3", [128, 2], F32)
        neghalf = sb("neghalf3", [128, 1], F32)
        s_w = P.sem("p3w")
        s_wst = [P.sem(f"p3wst{i}") for i in range(2)]
        s_in = [P.sem(f"p3in{i}") for i in range(2)]
        s_xin = [P.sem(f"p3xin{i}") for i in range(2)]
        s_h1 = [P.sem(f"p3h1{i}") for i in range(2)]

        P.dma("sp", lambda e: e.dma_start(out=sublg[:], in_=A["subln_g"][0].rearrange("(p o) -> p o", o=1)), s_w, waits=[t_prev])
        P.dma("sp", lambda e: e.dma_start(out=ln1g[:], in_=A["ln1_g"][0].partition_broadcast(128)), s_w)
        P.dma("sp", lambda e: e.dma_start(out=ln1b[:], in_=A["ln1_b"][0].partition_broadcast(128)), s_w)
        P.dma("sp", lambda e: e.dma_start(out=wr[:, :, 0:4], in_=A["w_group"][0].rearrange("(kc p) n -> p kc n", p=128)), s_w)
        P.dma("sp", lambda e: e.dma_start(out=wr[:, :, 4:36], in_=A["w_expert"][0].rearrange("(kc p) n -> p kc n", p=128)), s_w)
        P.dma("sp", lambda e: e.dma_start(out=rbias[:, 0:4], in_=A["b_group"][0].partition_broadcast(128)), s_w)
        P.dma("sp", lambda e: e.dma_start(out=rbias[:, 4:36], in_=A["b_expert"][0].partition_broadcast(128)), s_w)
        t_w = (s_w, s_w.v)
        s_wo = P.sem("p3wo")
        for kc in range(8):
            P.dma("pool", lambda e, kc=kc: e.dma_start(out=wout[:, kc, :], in_=A["w_out"][0, kc * 128:(kc + 1) * 128, :]), s_wo,
                  waits=[t_prev])
        t_wo = (s_wo, s_wo.v)
        P.op("dve", lambda e: e.memset(neghalf[:], -0.5), waits=[t_prev], sig=False)
        wst_free = [None, None]
        t_wbb = None
        for hh in range(8):
            r = hh % 2
            tl = P.dma("sp", lambda e, hh=hh, r=r: e.dma_start(out=wst[r][:], in_=A["w_branch_b"][0, hh * 128:(hh + 1) * 128, :]),
                       s_wst[r], waits=[wst_free[r], t_prev])
            t_wbb = P.op("dve", lambda e, hh=hh, r=r: e.tensor_scalar(out=wbb[:, hh, :], in0=wst[r][:], scalar1=sublg[:, 0:1],
                                                                      scalar2=1.0 - LAMBDA_INIT, op0=ALU.mult, op1=ALU.mult),
                         waits=[tl, t_w])
            wst_free[r] = t_wbb

        aT_free = [None, None]
        xt_free = [None, None]
        tmp_free = [None, None]
        mg_free = [None, None]
        z_free = [None, None]
        zn_free = [None, None]
        h1_free = [[], []]
        h1b_free = [None, None]
        h1T_free = None
        rr_acc = [0]
        mgT2 = [mgT, sb("mgT1", [128, 8, 512], BF16)]
        t_aT = {}
        t_xt = {}
        t_mg_done = {}

        def acc_bank():
            b = rr_acc[0] % 4
            rr_acc[0] += 1
            return b

        def issue_in(tt_i):
            ib = tt_i % 2
            tok0 = tt_i * 512
            t_aT[tt_i] = P.dma("sp", lambda e, ib=ib, tok0=tok0: e.dma_start(
                out=aT[ib][:], in_=aT_s.rearrange("(h p) s -> p h s", p=128)[:, :, tok0:tok0 + 512]), s_in[ib],
                waits=[t_prev, aT_free[ib]])
            t_xt[tt_i] = P.dma("sp", lambda e, ib=ib, tok0=tok0: e.dma_start(
                out=xt[ib][:], in_=A["x"][tok0:tok0 + 512, :].rearrange("(b p) d -> p b d", p=128)), s_xin[ib],
                waits=[xt_free[ib]])

        def emit_yb(tt_i, m):
            ib = tt_i % 2
            b = acc_bank()
            tk = None
            for kc in range(8):
                tk = P.op("pe", lambda e, kc=kc, b=b, m=m, ib=ib: e.matmul(
                    bank[b][:], wbb[:, kc, m * 128:(m + 1) * 128], aT[ib][:, kc, :], start=(kc == 0), stop=(kc == 7)),
                    waits=[t_aT[tt_i], t_wbb, bank_free[b]] if kc == 0 else [], sig=(kc == 7))
            if m == 7:
                aT_free[ib] = tk
            r = m % 2
            ci = tt_i * 8 + m
            g = ci % NGC
            if ci + NGC - 1 < NT * 8:
                issue_gc(ci + NGC - 1)
            t = P.op("dve", lambda e, b=b, g=g, r=r: e.tensor_tensor(out=tmpg[r][:], in0=bank[b][:], in1=gbc[g][:],
                                                                    op=ALU.mult), waits=[tk, tmp_free[r], t_gc[ci]])
            bank_free[b] = t
            t_mg = P.op("pool", lambda e, m=m, g=g, r=r, ib=ib: e.tensor_tensor(out=mgT2[ib][:, m, :], in0=tmpg[r][:], in1=mac[g][:],
                                                                               op=ALU.add), waits=[t, mg_free[ib] if m == 0 else None])
            tmp_free[r] = t_mg
            gc_free[g] = t_mg
            if m == 7:
                t_mg_done[tt_i] = t_mg

        nblk = 0
        for ci in range(NGC - 1):
            issue_gc(ci)
        issue_in(0)
        for m in range(8):
            emit_yb(0, m)
        for tt_i in range(NT):
            tok0 = tt_i * 512
            ib = tt_i % 2
            if tt_i + 1 < NT:
                issue_in(tt_i + 1)
            t_mg = t_mg_done[tt_i]
            for tb in range(4):
                zb = nblk % 2
                nblk += 1
                tz = None
                for half in range(2):
                    b = acc_bank()
                    tk = None
                    for kc in range(8):
                        tk = P.op("pe", lambda e, kc=kc, b=b, tb=tb, half=half, ib=ib: e.matmul(
                            bank[b][:], mgT2[ib][:, kc, tb * 128:(tb + 1) * 128], wout[:, kc, half * 512:(half + 1) * 512],
                            start=(kc == 0), stop=(kc == 7)), waits=[t_mg, t_wo, bank_free[b]] if kc == 0 else [], sig=(kc == 7))
                    tz = P.op("dve", lambda e, b=b, tb=tb, half=half, ib=ib, zb=zb: e.scalar_tensor_tensor(
                        out=z[zb][:, half * 512:(half + 1) * 512], in0=xt[ib][:, tb, half * 512:(half + 1) * 512], scalar=ALPHA,
                        in1=bank[b][:], op0=ALU.mult, op1=ALU.add), waits=[tk, z_free[zb], t_xt[tt_i]])
                    bank_free[b] = tz
                    tz = P.op("dve", lambda e, half=half, zb=zb: e.bn_stats(out=st12[:, half, :],
                                                                           in_=z[zb][:, half * 512:(half + 1) * 512]), waits=[tz])
                if tb == 3:
                    mg_free[ib] = tk
                    xt_free[ib] = tz
                if tt_i + 1 < NT:
                    emit_yb(tt_i + 1, 2 * tb)
                    emit_yb(tt_i + 1, 2 * tb + 1)

                t = P.op("dve", lambda e: e.bn_aggr(out=mv[:], in_=st12[:].rearrange("p a b -> p (a b)")), waits=[tz])
                t = P.op("dve", lambda e: e.tensor_scalar(out=rr[:, 0:1], in0=mv[:, 1:2], scalar1=LN_EPS, scalar2=None, op0=ALU.add),
                         waits=[t])
                t = P.op("act", lambda e: e.activation(out=rr[:, 0:1], in_=rr[:, 0:1], func=AF.Sqrt), waits=[t])
                t = P.op("dve", lambda e: e.reciprocal(out=rr[:, 0:1], in_=rr[:, 0:1]), waits=[t])
                t = P.op("dve", lambda e: e.scalar_tensor_tensor(out=rr[:, 1:2], in0=mv[:, 0:1], scalar=-1.0, in1=rr[:, 0:1],
                                                                op0=ALU.mult, op1=ALU.mult), waits=[t])
                t = P.op("act", lambda e, zb=zb: e.activation(out=zn[zb][:], in_=z[zb][:], func=AF.Identity, bias=rr[:, 1:2],
                                                             scale=rr[:, 0:1]), waits=[t, zn_free[zb]])
                z_free[zb] = t
                t = P.op("dve", lambda e, zb=zb: e.tensor_tensor(out=zn[zb][:], in0=zn[zb][:], in1=ln1g[:], op=ALU.mult),
                         waits=[t, t_w])
                t_h1 = P.op("pool", lambda e, zb=zb: e.tensor_tensor(out=h1[zb][:], in0=zn[zb][:], in1=ln1b[:], op=ALU.add),
                            waits=[t] + h1_free[zb])
                zn_free[zb] = t_h1
                t_b = P.op("act", lambda e, zb=zb: e.copy(out=h1b[zb][:], in_=h1[zb][:]), waits=[t_h1, h1b_free[zb]])
                r0 = tok0 + tb * 128
                td1 = P.dma("sp", lambda e, zb=zb, r0=r0: e.dma_start(out=h1_s[r0:r0 + 128, :], in_=h1[zb][:]), s_h1[zb], waits=[t_h1])
                h1b_free[zb] = P.dma("sp", lambda e, zb=zb, r0=r0: e.dma_start(out=h1b_s[r0:r0 + 128, :], in_=h1b[zb][:]), s_h1[zb],
                                     waits=[t_b])
                h1_free[zb] = [h1b_free[zb]]
        t_h1done = [(s_h1[0], s_h1[0].v), (s_h1[1], s_h1[1].v)]
        NRB = 4
        hr = [xt[0][:, i, :] for i in range(4)]
        s_hr = [P.sem(f"p3hr{i}") for i in range(NRB)]
        hr_free = [None] * NRB
        t_hr = {}
        t_main_done = [(P.esem[k], P.esem[k].v) for k in ("pe", "act", "dve", "pool")]

        def issue_hr(bi):
            g = bi % NRB
            t_hr[bi] = P.dma("sp", lambda e, bi=bi, g=g: e.dma_start(out=hr[g], in_=h1_s[bi * 128:(bi + 1) * 128, :]), s_hr[g],
                             waits=t_h1done + t_main_done + [hr_free[g]])
        NBk = S // 128
        for bi in range(min(NRB - 1, NBk)):
            issue_hr(bi)
        for bi in range(NBk):
            g = bi % NRB
            if bi + NRB - 1 < NBk:
                issue_hr(bi + NRB - 1)
            tks = []
            for kc in range(8):
                bq = 4 + kc // 4
                tks.append(P.op("pe", lambda e, kc=kc, bq=bq, g=g: e.transpose(
                    out=bank[bq][:, (kc % 4) * 128:(kc % 4 + 1) * 128], in_=hr[g][:, kc * 128:(kc + 1) * 128],
                    identity=ident_f[:]), waits=[t_hr[bi], bank_free[bq], t_const] if kc % 4 == 0 else []))
            hr_free[g] = tks[7]
            t = P.op("act", lambda e: e.copy(out=h1T[:, 0:4, :], in_=bank[4][:].rearrange("p (k t) -> p k t", k=4)),
                     waits=[tks[3], h1T_free])
            bank_free[4] = t
            t = P.op("dve", lambda e: e.tensor_copy(out=h1T[:, 4:8, :], in_=bank[5][:].rearrange("p (k t) -> p k t", k=4)),
                     waits=[tks[7], h1T_free])
            bank_free[5] = t
            tk = None
            for kc in range(8):
                tk = P.op("pe", lambda e, kc=kc: e.matmul(bank[6][:, 0:36], h1T[:, kc, :], wr[:, kc, :], start=(kc == 0),
                                                         stop=(kc == 7)),
                          waits=[t, bank_free[4], t_w, bank_free[6]] if kc == 0 else [], sig=(kc == 7))
            h1T_free = tk
            t = P.op("dve", lambda e, bi=bi: e.tensor_tensor(out=logits_all[:, bi, :], in0=bank[6][:, 0:36], in1=rbias[:],
                                                            op=ALU.add), waits=[tk])
            bank_free[6] = t
        t_end = [(s_h1[0], s_h1[0].v), (s_h1[1], s_h1[1].v)] + [(P.esem[k], P.esem[k].v) for k in ("pe", "act", "dve", "pool")]
        dbg("logits", logits_all, t_end[-2])
        P.barrier()
        P.replay()
    return t_end


def phase4(nc, P, A, S, bank, bank_free, ident_b, t_const, s_scr, h1_s, h1b_s, slot_s, out_s, out, logits_all, dbg,
           wgu_r, wd_r, t_prep):
    NB = S // 128
    NBLK = 2 * S // MBLK + NE
    NCH = NBLK * 2
    t_prev = (s_scr, s_scr.v)
    with ExitStack() as ps:
        def sb(name, shape, dt):
            return ps.enter_context(nc.sbuf_tensor("sb_" + name, shape, dt))
        L = logits_all
        ones_b = sb("ones_b", [128, 128], BF16)
        triu_b = sb("triu_b", [128, 128], BF16)
        tokidx = sb("tokidx", [128, NB], I32)
        blkstart = sb("blkstart", [128, NBLK], F32)
        w1 = sb("w1", [128, NB], F32)
        w2 = sb("w2", [128, NB], F32)
        dest_i = [sb(f"dest_i{k}", [128, NB], I32) for k in range(2)]
        sidx = sb("sidx", [128, NCH], I32)
        be_i = sb("be_i", [128, NBLK], I32)
        widx_i = sb("widx_i", [128, NBLK], I32)
        pk = sb("pk", [128, 8], F32)
        rs = ExitStack()

        def sbt(name, shape, dt):
            return rs.enter_context(nc.sbuf_tensor("sb_" + name, shape, dt))
        gsh = sbt("gsh", [128, NB, 4], F32)
        gex = sbt("gex", [128, NB, 4], F32)
        gsum = sbt("gsum", [128, NB], F32)
        gw = sbt("gw", [128, NB], F32)
        dm = sbt("dmat", [128, NB], F32)
        oh = [sbt(f"oh{k}", [128, NB, NE], F32) for k in range(2)]
        Call = sbt("Call", [128, NB, NE], BF16)
        sm = sbt("rt_small", [128, 16], F32)
        gmask = sbt("gmask", [128, 4], F32)
        pen = sbt("pen", [128, 4], F32)
        msk = sbt("msk", [128, NE], F32)
        top8 = sbt("top8", [128, 8], F32)
        colsum = sbt("colsum", [128, NB, NE], F32)
        cum = sbt("cum", [128, NB + 1, NE], F32)
        dest_all = sbt("dest_all", [128, NB, NE], F32)
        prod = sbt("prod", [128, NB, NE], F32)
        cnt = sbt("cnt", [128, NE], F32)
        cnt_i = sbt("cnt_i", [128, NE], I32)
        padded = sbt("padded", [128, NE], F32)
        pe_a = sbt("pe_a", [128, NE], F32)
        pe_b = sbt("pe_b", [128, NE], F32)
        pstart = sbt("pstart", [128, NE], F32)
        dest_f = [sbt(f"dest_f{k}", [128, NB], F32) for k in range(2)]
        tmp_i = [sbt(f"tmp_i{k}", [128, NB], I32) for k in range(2)]
        tmp_f = [sbt(f"tmp_f{k}", [128, NB], F32) for k in range(2)]
        addr_i = [sbt(f"addr_i{k}", [128, NB], I32) for k in range(2)]
        zero_i = sbt("zero_i", [128, NCH], I32)
        be_f = sbt("be_f", [128, NBLK], F32)
        s_c = P.sem("p4c")
        P.dma("sp", lambda e: e.dma_start(out=ones_b[:], in_=A["ones_b"]), s_c, waits=[t_prev])
        P.dma("sp", lambda e: e.dma_start(out=triu_b[:], in_=A["triu_b"]), s_c)
        P.dma("sp", lambda e: e.dma_start(out=tokidx[:], in_=A["tokidx"]), s_c)
        P.dma("sp", lambda e: e.dma_start(out=blkstart[:], in_=A["blkstart"]), s_c)
        t_c = (s_c, s_c.v)

        t = None
        for b in range(NB):
            t = P.op("dve", lambda e, b=b: e.tensor_reduce(out=sm[:, 0:1], in_=L[:, b, 0:4], axis=AX.X, op=ALU.max),
                     waits=[t_prev] if b == 0 else [t])
            t = P.op("dve", lambda e, b=b: e.tensor_scalar(out=gmask[:], in0=L[:, b, 0:4], scalar1=sm[:, 0:1], scalar2=None,
                                                          op0=ALU.is_equal), waits=[t])
            t = P.op("dve", lambda e, b=b: e.tensor_scalar(out=gsh[:, b, :], in0=L[:, b, 0:4], scalar1=sm[:, 0:1], scalar2=None,
                                                          op0=ALU.subtract), waits=[t])
            t = P.op("dve", lambda e: e.tensor_scalar(out=pen[:], in0=gmask[:], scalar1=1e30, scalar2=-1e30, op0=ALU.mult,
                                                     op1=ALU.add), waits=[t])
            for g in range(4):
                t = P.op("dve", lambda e, b=b, g=g: e.tensor_scalar(out=msk[:, g * 8:(g + 1) * 8], in0=L[:, b, 4 + g * 8:12 + g * 8],
                                                                   scalar1=pen[:, g:g + 1], scalar2=None, op0=ALU.add), waits=[t])
            t = P.op("dve", lambda e: e.max(out=top8[:], in_=msk[:]), waits=[t])
            t = P.op("dve", lambda e, b=b: e.tensor_scalar(out=oh[0][:, b, :], in0=msk[:], scalar1=top8[:, 0:1], scalar2=None,
                                                          op0=ALU.is_equal), waits=[t])
            t = P.op("dve", lambda e, b=b: e.tensor_scalar(out=oh[1][:, b, :], in0=msk[:], scalar1=top8[:, 1:2], scalar2=None,
                                                          op0=ALU.is_equal), waits=[t])
            t = P.op("dve", lambda e, b=b: e.tensor_tensor(out=dm[:, b:b + 1], in0=top8[:, 0:1], in1=top8[:, 1:2],
                                                          op=ALU.subtract), waits=[t])
            t = P.op("dve", lambda e, b=b: e.tensor_tensor(out=Call[:, b, :], in0=oh[0][:, b, :], in1=oh[1][:, b, :], op=ALU.add),
                     waits=[t])
        t_route = t
        ta = P.op("act", lambda e: e.activation(out=gex[:].rearrange("p b g -> p (b g)"), in_=gsh[:].rearrange("p b g -> p (b g)"),
                                                func=AF.Exp), waits=[t_route])
        ta2 = P.op("act", lambda e: e.activation(out=dm[:], in_=dm[:], func=AF.Sigmoid), waits=[ta])
        t = P.op("dve", lambda e: e.tensor_reduce(out=gsum[:], in_=gex[:], axis=AX.X, op=ALU.add), waits=[ta])
        t = P.op("dve", lambda e: e.reciprocal(out=gw[:], in_=gsum[:]), waits=[t])
        t = P.op("dve", lambda e: e.tensor_tensor(out=w1[:], in0=gw[:], in1=dm[:], op=ALU.mult), waits=[t, ta2])
        t_w12 = P.op("dve", lambda e: e.tensor_tensor(out=w2[:], in0=gw[:], in1=w1[:], op=ALU.subtract), waits=[t])

        GB_ = 16
        for g0 in range(0, NB, GB_):
            n = min(GB_, NB - g0)
            tk = P.op("pe", lambda e, g0=g0, n=n: e.matmul(bank[0][:, 0:n * NE], ones_b[:],
                                                          Call[:, g0:g0 + n, :].rearrange("p b e -> p (b e)"), start=True, stop=True),
                      waits=[t_route, t_c, bank_free[0]])
            t = P.op("dve", lambda e, g0=g0, n=n: e.tensor_copy(out=colsum[:, g0:g0 + n, :].rearrange("p b e -> p (b e)"),
                                                               in_=bank[0][:, 0:n * NE]), waits=[tk])
            bank_free[0] = t
        t = P.op("dve", lambda e: e.tensor_reduce(out=cnt[:], in_=colsum[:].rearrange("p b e -> p e b"), axis=AX.X, op=ALU.add),
                 waits=[t])
        t = P.op("dve", lambda e: e.tensor_scalar(out=padded[:], in0=cnt[:], scalar1=float(MBLK - 1), scalar2=None, op0=ALU.add),
                 waits=[t])
        t = P.op("dve", lambda e: e.tensor_copy(out=cnt_i[:], in_=padded[:]), waits=[t])
        t = P.op("dve", lambda e: e.tensor_single_scalar(out=cnt_i[:], in_=cnt_i[:], scalar=8, op=ALU.arith_shift_right), waits=[t])
        t = P.op("dve", lambda e: e.tensor_single_scalar(out=cnt_i[:], in_=cnt_i[:], scalar=8, op=ALU.logical_shift_left), waits=[t])
        t = P.op("dve", lambda e: e.tensor_copy(out=padded[:], in_=cnt_i[:]), waits=[t])
        t = P.op("dve", lambda e: e.tensor_copy(out=pe_a[:], in_=padded[:]), waits=[t])
        src, dst = pe_a, pe_b
        sft = 1
        while sft < NE:
            t = P.op("dve", lambda e, src=src, dst=dst, sft=sft: e.tensor_copy(out=dst[:, 0:sft], in_=src[:, 0:sft]), waits=[t])
            t = P.op("dve", lambda e, src=src, dst=dst, sft=sft: e.tensor_tensor(out=dst[:, sft:NE], in0=src[:, sft:NE],
                                                                                in1=src[:, 0:NE - sft], op=ALU.add), waits=[t])
            src, dst = dst, src
            sft *= 2
        pend = src
        t = P.op("dve", lambda e: e.tensor_tensor(out=pstart[:], in0=pend[:], in1=padded[:], op=ALU.subtract), waits=[t])
        t = P.op("dve", lambda e: e.tensor_copy(out=cum[:, 0, :], in_=pstart[:]), waits=[t])
        for b in range(NB):
            t = P.op("dve", lambda e, b=b: e.tensor_tensor(out=cum[:, b + 1, :], in0=cum[:, b, :], in1=colsum[:, b, :], op=ALU.add),
                     waits=[t])
        for g0 in range(0, NB, GB_):
            n = min(GB_, NB - g0)
            tk = None
            for bb in range(n):
                tk = P.op("pe", lambda e, g0=g0, bb=bb: e.matmul(bank[0][:, bb * NE:(bb + 1) * NE], triu_b[:], Call[:, g0 + bb, :],
                                                                start=True, stop=True), waits=[bank_free[0]] if bb == 0 else [],
                          sig=(bb == n - 1))
            t = P.op("dve", lambda e, g0=g0, n=n: e.tensor_tensor(out=dest_all[:, g0:g0 + n, :].rearrange("p b e -> p (b e)"),
                                                                 in0=bank[0][:, 0:n * NE],
                                                                 in1=cum[:, g0:g0 + n, :].rearrange("p b e -> p (b e)"), op=ALU.add),
                     waits=[tk, t])
            bank_free[0] = t
        for k in range(2):
            t = P.op("dve", lambda e, k=k: e.tensor_tensor(out=prod[:].rearrange("p b e -> p (b e)"),
                                                          in0=oh[k][:].rearrange("p b e -> p (b e)"),
                                                          in1=dest_all[:].rearrange("p b e -> p (b e)"), op=ALU.mult), waits=[t])
            t = P.op("dve", lambda e, k=k: e.tensor_reduce(out=dest_f[k][:], in_=prod[:], axis=AX.X, op=ALU.add), waits=[t])
            t = P.op("dve", lambda e, k=k: e.tensor_copy(out=dest_i[k][:], in_=dest_f[k][:]), waits=[t])
            t = P.op("dve", lambda e, k=k: e.tensor_single_scalar(out=tmp_i[0][:], in_=dest_i[k][:], scalar=127, op=ALU.bitwise_and),
                     waits=[t])
            t = P.op("dve", lambda e, k=k: e.tensor_single_scalar(out=tmp_i[1][:], in_=dest_i[k][:], scalar=7,
                                                                 op=ALU.arith_shift_right), waits=[t])
            t = P.op("dve", lambda e: e.tensor_copy(out=tmp_f[0][:], in_=tmp_i[0][:]), waits=[t])
            t = P.op("dve", lambda e: e.tensor_copy(out=tmp_f[1][:], in_=tmp_i[1][:]), waits=[t])
            t = P.op("dve", lambda e: e.scalar_tensor_tensor(out=tmp_f[0][:], in0=tmp_f[0][:], scalar=float(NCH), in1=tmp_f[1][:],
                                                            op0=ALU.mult, op1=ALU.add), waits=[t])
            t = P.op("dve", lambda e, k=k: e.tensor_copy(out=addr_i[k][:], in_=tmp_f[0][:]), waits=[t])
        t_addr = t
        t = P.op("dve", lambda e: e.memset(be_f[:], 0.0), waits=[t])
        for ex in range(NE):
            t = P.op("dve", lambda e, ex=ex: e.scalar_tensor_tensor(out=be_f[:], in0=blkstart[:], scalar=pend[:, ex:ex + 1],
                                                                   in1=be_f[:], op0=ALU.is_ge, op1=ALU.add), waits=[t, t_c])
        t = P.op("dve", lambda e: e.tensor_scalar(out=be_f[:], in0=be_f[:], scalar1=float(NE - 1), scalar2=None, op0=ALU.min),
                 waits=[t])
        t = P.op("dve", lambda e: e.tensor_copy(out=be_i[:], in_=be_f[:]), waits=[t])
        widx_f = sbt("widx_f", [128, NBLK], F32)
        t = P.op("dve", lambda e: e.tensor_copy(out=pk[:, 0:1], in_=tokidx[:, 0:1]), waits=[t, t_c])
        t = P.op("dve", lambda e: e.tensor_scalar(out=widx_f[:], in0=be_f[:], scalar1=128.0, scalar2=pk[:, 0:1],
                                                 op0=ALU.mult, op1=ALU.add), waits=[t])
        t_be = P.op("dve", lambda e: e.tensor_copy(out=widx_i[:], in_=widx_f[:]), waits=[t])
        s_sl = P.sem("p4sl")
        s_sz = P.sem("p4sz")
        s_sr = P.sem("p4sr")
        t = P.op("dve", lambda e: e.memset(zero_i[:], 0), waits=[t_be])
        t = P.dma("sp", lambda e: e.dma_start(out=slot_s, in_=zero_i[:]), s_sz, waits=[t, t_prev])
        slot_flat = slot_s.rearrange("p (c o) -> (p c) o", o=1)
        for b in range(NB):
            for k in range(2):
                P.dma("pool", lambda e, b=b, k=k: e.indirect_dma_start(
                    out=slot_flat, out_offset=bass.IndirectOffsetOnAxis(ap=addr_i[k][:, b:b + 1], axis=0),
                    in_=tokidx[:, b:b + 1], in_offset=None), s_sl, waits=[t, t_addr, t_c])
        t_sc = (s_sl, s_sl.v)
        t_sidx = P.dma("sp", lambda e: e.dma_start(out=sidx[:], in_=slot_s), s_sr, waits=[t_sc])
        dbg("be_i", be_i, t_be)
        dbg("dest_i0", dest_i[0], t_addr)
        dbg("dest_i1", dest_i[1], t_addr)
        dbg("w1", w1, t_w12)
        dbg("w2", w2, t_w12)
        dbg("sidx", sidx, t_sidx)

        t_rs_end = [t_sidx, (s_scr, s_scr.v)] + [(P.esem[k], P.esem[k].v) for k in ("pe", "act", "dve", "pool")]
        P.barrier()
        P.replay()
        rs.close()
        xs = ExitStack()

        def sbx(name, shape, dt):
            return xs.enter_context(nc.sbuf_tensor("sb_" + name, shape, dt))
        wgu = [sbx(f"wgu{i}", [128, 2, 8, EH], BF16) for i in range(2)]
        wd = [sbx(f"wd{i}", [128, 4, D], BF16) for i in range(2)]
        xg = [sbx(f"xg{i}", [128, D], BF16) for i in range(6)]
        xTm = [sbx(f"xTm{i}", [128, 8, 256], BF16) for i in range(2)]
        sgt = [sbx(f"sgt{i}", [128, 256], F32) for i in range(2)]
        hT = [sbx(f"hT{i}", [128, 4, 256], BF16) for i in range(2)]
        ost = [sbx(f"ost{i}", [128, D], F32) for i in range(2)]
        s_wt = [P.sem(f"p4w{i}") for i in range(2)]
        s_xg = [P.sem(f"p4x{i}") for i in range(6)]
        s_os = [P.sem(f"p4o{i}") for i in range(2)]
        w_free = [None, None]
        xg_free = [None] * 6
        xTm_free = [None, None]
        sgt_free = [None, None]
        hT_free = [None, None]
        ost_free = [None, None]
        n_os = 0
        n_sg = 0
        reg_holder = []
        acc = [0]

        def acc_bank():
            b = 1 + acc[0] % 4
            acc[0] += 1
            return b

        def load_w(j):
            wb = j % 2
            P.dma("pool", lambda e, wb=wb, j=j: e.indirect_dma_start(
                out=wgu[wb][:].rearrange("p g k f -> p (g k f)"), out_offset=None, in_=wgu_r,
                in_offset=bass.IndirectOffsetOnAxis(ap=widx_i[:, j:j + 1], axis=0)), s_wt[wb],
                waits=[t_be, w_free[wb], t_prep] + (t_rs_end if j < 2 else []))
            tok = P.dma("pool", lambda e, wb=wb, j=j: e.indirect_dma_start(
                out=wd[wb][:].rearrange("p k f -> p (k f)"), out_offset=None, in_=wd_r,
                in_offset=bass.IndirectOffsetOnAxis(ap=widx_i[:, j:j + 1], axis=0)), s_wt[wb])
            return [tok]

        def load_x(j):
            toks = []
            for i in range(2):
                xi = (2 * j + i) % 6
                c = 2 * j + i
                toks.append(P.dma("pool", lambda e, xi=xi, c=c: e.indirect_dma_start(
                    out=xg[xi][:], out_offset=None, in_=h1b_s,
                    in_offset=bass.IndirectOffsetOnAxis(ap=sidx[:, c:c + 1], axis=0)), s_xg[xi],
                    waits=[t_sidx, xg_free[xi]] + (t_rs_end if j < 2 else [])))
            return toks

        t_wl = {0: load_w(0)}
        t_xl = {0: load_x(0)}
        if NBLK > 1:
            t_xl[1] = load_x(1)
        t_xTs = {}

        def emit_T(j):
            xb = j % 2
            t = None
            for i in range(2):
                xi = (2 * j + i) % 6
                tk = None
                for kc in range(8):
                    tk = P.op("pe", lambda e, kc=kc, xi=xi: e.transpose(out=bank[7][:, kc * 128:(kc + 1) * 128],
                                                                       in_=xg[xi][:, kc * 128:(kc + 1) * 128], identity=ident_b[:]),
                              waits=[t_xl[j][i], bank_free[7], t_const] if kc == 0 else [], sig=(kc == 7))
                xg_free[xi] = tk
                t = P.op("act", lambda e, i=i, xb=xb: e.copy(out=xTm[xb][:, :, i * 128:(i + 1) * 128],
                                                            in_=bank[7][:].rearrange("p (k t) -> p k t", k=8)),
                         waits=[tk, xTm_free[xb]] if i == 0 else [tk])
                bank_free[7] = t
            t_xTs[j] = t

        emit_T(0)
        for j in range(NBLK):
            wb = j % 2
            if j + 1 < NBLK:
                t_wl[j + 1] = load_w(j + 1)
            if j + 2 < NBLK:
                t_xl[j + 2] = load_x(j + 2)
            xb = j % 2
            t_xT = t_xTs[j]
            hb = j % 2
            for hc in range(4):
                b = acc_bank()
                tk = None
                for gu in range(2):
                    for kc in range(8):
                        tk = P.op("pe", lambda e, kc=kc, b=b, gu=gu, hc=hc, wb=wb, xb=xb: e.matmul(
                            bank[b][:, gu * 256:(gu + 1) * 256], wgu[wb][:, gu, kc, hc * 128:(hc + 1) * 128], xTm[xb][:, kc, :],
                            start=(kc == 0), stop=(kc == 7)),
                            waits=[t_xT, bank_free[b]] + t_wl[j] if (kc == 0 and gu == 0) else [], sig=(kc == 7 and gu == 1))
                r = n_sg % 2
                n_sg += 1
                t = P.op("act", lambda e, b=b, r=r: e.activation(out=sgt[r][:], in_=bank[b][:, 0:256], func=AF.Silu),
                         waits=[tk, sgt_free[r]])
                t = P.op("dve", lambda e, b=b, r=r, hc=hc, hb=hb: e.tensor_tensor(out=hT[hb][:, hc, :], in0=sgt[r][:],
                                                                                in1=bank[b][:, 256:512], op=ALU.mult),
                         waits=[t, hT_free[hb]] if hc == 0 else [t])
                sgt_free[r] = t
                bank_free[b] = t
            xTm_free[xb] = tk
            t_hT = t
            if j + 1 < NBLK:
                emit_T(j + 1)
            for i in range(2):
                r = n_os % 2
                n_os += 1
                t = None
                for oh_ in range(2):
                    b = acc_bank()
                    tk = None
                    for hc in range(4):
                        tk = P.op("pe", lambda e, hc=hc, b=b, i=i, oh_=oh_, hb=hb, wb=wb: e.matmul(
                            bank[b][:], hT[hb][:, hc, i * 128:(i + 1) * 128], wd[wb][:, hc, oh_ * 512:(oh_ + 1) * 512],
                            start=(hc == 0), stop=(hc == 3)), waits=[t_hT, bank_free[b]] if hc == 0 else [], sig=(hc == 3))
                    if oh_ == 0:
                        t = P.op("act", lambda e, b=b, r=r: e.copy(out=ost[r][:, 0:512], in_=bank[b][:]), waits=[tk, ost_free[r]])
                    else:
                        t = P.op("dve", lambda e, b=b, r=r: e.tensor_copy(out=ost[r][:, 512:1024], in_=bank[b][:]),
                                 waits=[tk, t, ost_free[r]])
                    bank_free[b] = t
                c = 2 * j + i
                ost_free[r] = P.dma("sp", lambda e, r=r, c=c: e.dma_start(out=out_s[c * 128:(c + 1) * 128, :], in_=ost[r][:]), s_os[r],
                                    waits=[t, (P.esem["act"], P.esem["act"].v)])
            hT_free[hb] = tk
            w_free[wb] = tk
        t_exp_done = [x for x in ost_free if x is not None] + [(P.esem[k], P.esem[k].v) for k in ("pe", "act", "dve", "pool")]
        P.barrier()
        P.replay()
        xs.close()

        ln2g = sb("ln2g", [128, D], F32)
        ln2b = sb("ln2b", [128, D], F32)
        NG = 4
        o1 = [sb(f"o1_{i}", [128, D], F32) for i in range(NG)]
        o2 = [sb(f"o2_{i}", [128, D], F32) for i in range(NG)]
        h1t = [sb(f"h1c{i}", [128, D], F32) for i in range(NG)]
        yb = [sb(f"yb{i}", [128, D], F32) for i in range(2)]
        zo = [sb(f"zo{i}", [128, D], F32) for i in range(2)]
        st12 = sb("st12_4", [128, 2, 6], F32)
        mv = sb("mv4", [128, 2], F32)
        rr = sb("rr4", [128, 2], F32)
        neghalf = sb("neghalf4", [128, 1], F32)
        s_l2 = P.sem("p4l2")
        s_cin = [P.sem(f"p4ci{i}") for i in range(4)]
        s_co = [P.sem(f"p4co{i}") for i in range(2)]
        s_ch = [P.sem(f"p4ch{i}") for i in range(4)]
        P.dma("sp", lambda e: e.dma_start(out=ln2g[:], in_=A["ln2_g"][0].partition_broadcast(128)), s_l2, waits=t_exp_done)
        P.dma("sp", lambda e: e.dma_start(out=ln2b[:], in_=A["ln2_b"][0].partition_broadcast(128)), s_l2)
        t_l2 = (s_l2, s_l2.v)
        P.op("dve", lambda e: e.memset(neghalf[:], -0.5), waits=t_exp_done)
        cin_free = [None] * NG
        zo_free = [None, None]
        yb_free = [None, None]
        t_ins = {}

        def issue_gather(b):
            g = b % NG
            P.dma("pool", lambda e, b=b, g=g: e.indirect_dma_start(
                out=o1[g][:], out_offset=None, in_=out_s, in_offset=bass.IndirectOffsetOnAxis(ap=dest_i[0][:, b:b + 1], axis=0)),
                s_cin[g], waits=t_exp_done + [cin_free[g]])
            ta = P.dma("pool", lambda e, b=b, g=g: e.indirect_dma_start(
                out=o2[g][:], out_offset=None, in_=out_s, in_offset=bass.IndirectOffsetOnAxis(ap=dest_i[1][:, b:b + 1], axis=0)),
                s_cin[g])
            tb_ = P.dma("sp", lambda e, b=b, g=g: e.dma_start(out=h1t[g][:], in_=h1_s[b * 128:(b + 1) * 128, :]), s_ch[g],
                        waits=[cin_free[g], t_prev] + t_exp_done)
            t_ins[b] = (ta, tb_)

        for b in range(min(NG - 1, NB)):
            issue_gather(b)
        for b in range(NB):
            r = b % 2
            g = b % NG
            if b + NG - 1 < NB:
                issue_gather(b + NG - 1)
            t_in1, t_in2 = t_ins[b]
            t = P.op("act", lambda e, b=b, r=r, g=g: e.activation(out=yb[r][:], in_=o1[g][:], func=AF.Identity, scale=w1[:, b:b + 1]),
                     waits=[t_in1, t_in2, t_w12, zo_free[r], yb_free[r]])
            t = P.op("dve", lambda e, b=b, r=r, g=g: e.scalar_tensor_tensor(out=yb[r][:], in0=o2[g][:], scalar=w2[:, b:b + 1],
                                                                           in1=yb[r][:], op0=ALU.mult, op1=ALU.add), waits=[t])
            t = P.op("dve", lambda e, r=r, g=g: e.scalar_tensor_tensor(out=yb[r][:], in0=h1t[g][:], scalar=ALPHA, in1=yb[r][:],
                                                                      op0=ALU.mult, op1=ALU.add), waits=[t])
            cin_free[g] = t
            for half in range(2):
                t = P.op("dve", lambda e, half=half, r=r: e.bn_stats(out=st12[:, half, :], in_=yb[r][:, half * 512:(half + 1) * 512]),
                         waits=[t])
            t = P.op("dve", lambda e: e.bn_aggr(out=mv[:], in_=st12[:].rearrange("p a b -> p (a b)")), waits=[t])
            t = P.op("dve", lambda e: e.tensor_scalar(out=rr[:, 0:1], in0=mv[:, 1:2], scalar1=LN_EPS, scalar2=None, op0=ALU.add),
                     waits=[t])
            t = P.op("act", lambda e: e.activation(out=rr[:, 0:1], in_=rr[:, 0:1], func=AF.Sqrt), waits=[t])
            t = P.op("dve", lambda e: e.reciprocal(out=rr[:, 0:1], in_=rr[:, 0:1]), waits=[t])
            t = P.op("dve", lambda e: e.scalar_tensor_tensor(out=rr[:, 1:2], in0=mv[:, 0:1], scalar=-1.0, in1=rr[:, 0:1],
                                                            op0=ALU.mult, op1=ALU.mult), waits=[t])
            t = P.op("act", lambda e, r=r: e.activation(out=yb[r][:], in_=yb[r][:], func=AF.Identity, bias=rr[:, 1:2],
                                                       scale=rr[:, 0:1]), waits=[t])
            t = P.op("dve", lambda e, r=r: e.tensor_tensor(out=yb[r][:], in0=yb[r][:], in1=ln2g[:], op=ALU.mult), waits=[t, t_l2])
            t = P.op("pool", lambda e, r=r: e.tensor_tensor(out=zo[r][:], in0=yb[r][:], in1=ln2b[:], op=ALU.add), waits=[t, zo_free[r]])
            yb_free[r] = t
            zo_free[r] = P.dma("sp", lambda e, b=b, r=r: e.dma_start(out=out[b * 128:(b + 1) * 128, :], in_=zo[r][:]), s_co[r], waits=[t])
        t_end = [x for x in zo_free if x is not None]
        P.op("sp", lambda e: e.nop(), waits=t_end, sig=False)
        P.replay()
    return t_end

def kernel(**inputs):
    S = 8192
    n = 8
    x = np.ascontiguousarray(np.asarray(inputs["x"], dtype=np.float32))
    nc = build(S)
    consts = make_consts(S)
    shared = {k: np.ascontiguousarray(np.asarray(inputs[k], dtype=np.float32)) for k in IN_SHAPES}
    shared.update(consts)
    in_maps = []
    for i in range(n):
        m = dict(shared)
        m["x"] = x[i]
        in_maps.append(m)
    res = run_bass_kernel_spmd(nc, in_maps, core_ids=list(range(n)))
    return np.stack([np.asarray(r["out"], dtype=np.float32) for r in res.results], axis=0)
```

```python
import math
from contextlib import ExitStack

import ml_dtypes
import numpy as np

import concourse.bass as bass
import concourse.mybir as mybir
from concourse.bass_utils import run_bass_kernel_spmd

F32 = mybir.dt.float32
BF16 = mybir.dt.bfloat16
I32 = mybir.dt.int32
AF = mybir.ActivationFunctionType
ALU = mybir.AluOpType
AX = mybir.AxisListType

D = 1024
H = 8
NE = 32
EH = 512
IN_W = 6144
OFF_U, OFF_V, OFF_Q, OFF_K, OFF_VAL, OFF_GA, OFF_GB = 0, 512, 1024, 2048, 3072, 4096, 5120
ALPHA = 2.0 ** 0.25
LN_EPS = 1e-5
RMS_EPS = 1e-5
LAMBDA_INIT = 0.8 - 0.6 * math.exp(-0.3 * 0)
MBLK = 256


class Sem:
    def __init__(self, h):
        self.h = h
        self.v = 0


class Prog:
    ENG = {"pe": "tensor", "act": "scalar", "dve": "vector", "pool": "gpsimd", "sp": "sync"}

    def __init__(self, nc, es):
        self.nc = nc
        self.es = es
        self.q = {k: [] for k in self.ENG}
        self.esem = {k: self.sem("e_" + k) for k in self.ENG}
        self.nsem = 0

    def sem(self, name):
        sm = Sem(self.es.enter_context(self.nc.semaphore(name)))
        if not hasattr(self, "all_sems"):
            self.all_sems = []
        self.all_sems.append(sm)
        return sm

    def barrier(self):
        toks = [(sm, sm.v) for sm in self.all_sems if sm.v > 0]
        for k in self.ENG:
            self.q[k].append((lambda e: e.nop(), self._w(toks), None, 1))

    def op(self, eng, fn, waits=(), sig=True):
        if eng in ("dve", "pool"):
            sig = True
            if self.esem[eng].v:
                waits = list(waits) + [(self.esem[eng], self.esem[eng].v)]
        s = self.esem[eng] if sig else None
        tok = None
        if s is not None:
            s.v += 1
            tok = (s, s.v)
        self.q[eng].append((fn, self._w(waits), s, 1))
        return tok

    def dma(self, eng, fn, sem, waits=()):
        sem.v += 16
        self.q[eng].append((fn, self._w(waits), sem, 16))
        return (sem, sem.v)

    @staticmethod
    def _w(waits):
        best = {}
        for t in waits:
            if t is None:
                continue
            s, v = t
            if id(s) not in best or best[id(s)][1] < v:
                best[id(s)] = (s, v)
        return tuple(best.values())

    def replay(self):
        nc = self.nc
        with nc.Block() as block:
            for k, bn in self.ENG.items():
                items = self.q[k]
                own = self.esem[k]

                def body(eng, items=items, own=own):
                    for fn, waits, s, amt in items:
                        for (ws, wv) in waits:
                            eng.wait_ge(ws.h, wv)
                        ins = fn(eng)
                        if s is not None:
                            ins.then_inc(s.h, amt)

                getattr(block, bn)(body)
        self.q = {k: [] for k in self.ENG}


def _slopes():
    return [2.0 ** (-8.0 * (h + 1) / H) for h in range(H)]


def make_consts(S):
    bf = ml_dtypes.bfloat16
    c = {}
    c["ident_f"] = np.eye(128, dtype=np.float32)
    c["ident_b"] = np.eye(128, dtype=np.float32).astype(bf)
    tri = (np.arange(128)[:, None] < np.arange(128)[None, :]).astype(np.float32)
    c["triu_b"] = tri.astype(bf)
    c["ones_b"] = np.ones((128, 128), np.float32).astype(bf)
    pos = np.arange(S)
    blk = (pos // 128).astype(np.float32)
    r = (pos % 128).astype(np.float32)
    qa = np.zeros((H, 4, S), np.float32)
    ka = np.zeros((H, 4, S), np.float32)
    corr = np.zeros((H, 128, 128), np.float32)
    kr = np.arange(128)[:, None]
    qr = np.arange(128)[None, :]
    for h, sl in enumerate(_slopes()):
        qa[h, 0] = 1.0
        qa[h, 1] = 1.0
        qa[h, 2] = -sl * 128.0 * blk
        qa[h, 3] = -sl * r
        ka[h, 0] = sl * 128.0 * blk
        ka[h, 1] = sl * r
        ka[h, 2] = 1.0
        ka[h, 3] = 1.0
        cm = np.where(kr > qr, -2.0 * sl * (kr - qr), 0.0)
        cm = np.where((kr // 64) > (qr // 64), -30000.0, cm)
        corr[h] = cm
    c["qaug"] = qa.astype(bf)
    c["kaug"] = ka.astype(bf)
    c["corrT"] = np.ascontiguousarray(corr.transpose(1, 0, 2)).astype(bf)
    nb = S // 128
    c["tokidx"] = (np.arange(nb)[None, :] * 128 + np.arange(128)[:, None]).astype(np.int32)
    nblk = 2 * S // MBLK + NE
    c["blkstart"] = np.broadcast_to((np.arange(nblk) * float(MBLK))[None, :], (128, nblk)).astype(np.float32).copy()
    return c


CONST_DT = {"ident_f": F32, "ident_b": BF16, "triu_b": BF16, "ones_b": BF16, "qaug": BF16, "kaug": BF16,
            "corrT": BF16, "tokidx": I32, "blkstart": F32}

IN_SHAPES = {
    "w_in": [1, D, IN_W], "b_in": [1, IN_W], "sg_ln_g": [1, 512], "sg_ln_b": [1, 512],
    "sg_w": [1, 8, 128, 128], "sg_b": [1, 8, 128], "w_branch_a": [1, 512, D],
    "lam_q1": [1, 64], "lam_k1": [1, 64], "lam_q2": [1, 64], "lam_k2": [1, 64], "subln_g": [1, 128],
    "w_branch_b": [1, D, D], "w_out": [1, D, D], "ln1_g": [1, D], "ln1_b": [1, D],
    "w_group": [1, D, 4], "b_group": [1, 4], "w_expert": [1, D, NE], "b_expert": [1, NE],
    "w_gate": [1, NE, D, EH], "w_up": [1, NE, D, EH], "w_down": [1, NE, EH, D],
    "ln2_g": [1, D], "ln2_b": [1, D],
}


def build(S, debug=False, phases=(1, 2, 3, 4)):
    nc = bass.Bass("TRN2", target_bir_lowering=False)
    NT = S // 512
    NB = S // 128
    NBLK = 2 * S // MBLK + NE
    NSLOT = NBLK * MBLK
    NCH = NSLOT // 128
    consts = make_consts(S)

    A = {}
    A["x"] = nc.dram_tensor("x", [S, D], F32, kind="ExternalInput").ap()
    for k, shp in IN_SHAPES.items():
        A[k] = nc.dram_tensor(k, shp, F32, kind="ExternalInput").ap()
    for k, v in consts.items():
        A[k] = nc.dram_tensor(k, list(v.shape), CONST_DT[k], kind="ExternalInput").ap()
    out = nc.dram_tensor("out", [S, D], F32, kind="ExternalOutput").ap()
    skind = "ExternalOutput" if debug else "Internal"

    def scratch(name, shape, dt):
        return nc.dram_tensor(name, shape, dt, kind=skind).ap()

    qT_s = scratch("qT_s", [H, 2, 64, S], BF16)
    kT_s = scratch("kT_s", [H, 2, 64, S], BF16)
    v_s = scratch("v_s", [S, D], BF16)
    gbT_s = scratch("gbT_s", [D, S], F32)
    maT_s = scratch("maT_s", [D, S], F32)
    aT_s = scratch("aT_s", [D, S], BF16)
    h1_s = scratch("h1_s", [S, D], F32)
    h1b_s = scratch("h1b_s", [S, D], BF16)
    slot_s = scratch("slot_s", [128, NCH], I32)
    out_s = scratch("out_s", [NSLOT, D], F32)
    wgu_r = nc.dram_tensor("wgu_r", [NE * 128, 2 * 8 * EH], BF16).ap()
    wd_r = nc.dram_tensor("wd_r", [NE * 128, 4 * D], BF16).ap()
    dbg_s = scratch("dbg_s", [128, 4096], F32) if debug else None

    with ExitStack() as es:
        P = Prog(nc, es)

        def sb(name, shape, dt, stack=es):
            return stack.enter_context(nc.sbuf_tensor("sb_" + name, shape, dt))

        bank = [es.enter_context(nc.psum_tensor(f"bank{i}", [128, 512], F32)) for i in range(7)]
        bank.append(es.enter_context(nc.psum_tensor("bank7b", [128, 1024], BF16)))
        bank_free = [None] * 8

        s_const = P.sem("const")
        s_scr = P.sem("scr")

        ident_f = sb("ident_f", [128, 128], F32)
        ident_b = sb("ident_b", [128, 128], BF16)
        P.dma("sp", lambda e: e.dma_start(out=ident_f[:], in_=A["ident_f"]), s_const)
        t_const = P.dma("sp", lambda e: e.dma_start(out=ident_b[:], in_=A["ident_b"]), s_const)

        def dbg(name, tile, tok):
            if not debug:
                return
            shp = list(tile.shape)
            d = nc.dram_tensor("dbg_" + name, shp, tile.dtype, kind="ExternalOutput").ap()
            P.dma("sp", lambda e: e.dma_start(out=d, in_=tile[:]), s_scr, waits=[tok])

        if 1 in phases:
            t_ph1 = phase1(nc, P, A, S, NT, sb, bank, bank_free, ident_f, t_const, s_scr,
                           qT_s, kT_s, v_s, gbT_s, maT_s, dbg)
        NBk = S // 128
        logits_all = sb("logits_all", [128, NBk, 36], F32)
        s_prep = P.sem("wprep")
        prep_fns = []
        if 4 in phases:
            for ex in range(NE):
                for gu, nm in enumerate(("w_gate", "w_up")):
                    prep_fns.append(lambda ex=ex, gu=gu, nm=nm: P.dma("pool", lambda e: e.dma_start(
                        out=wgu_r[ex * 128:(ex + 1) * 128, gu * 8 * EH:(gu + 1) * 8 * EH].rearrange("p (kc f) -> p kc f", kc=8),
                        in_=A[nm][0, ex].rearrange("(kc p) f -> p kc f", p=128)), s_prep))
                prep_fns.append(lambda ex=ex: P.dma("pool", lambda e: e.dma_start(
                    out=wd_r[ex * 128:(ex + 1) * 128, :].rearrange("p (kc f) -> p kc f", kc=4),
                    in_=A["w_down"][0, ex].rearrange("(kc p) f -> p kc f", p=128)), s_prep))
        if 2 not in phases:
            for f in prep_fns:
                f()
            prep_fns = []
        if 2 in phases:
            phase2(nc, P, A, S, bank, bank_free, ident_f, ident_b, t_const, s_scr, qT_s, kT_s, v_s, aT_s, dbg, prep_fns)
        t_prep = (s_prep, s_prep.v)
        if 3 in phases:
            phase3(nc, P, A, S, bank, bank_free, ident_f, t_const, s_scr, gbT_s, maT_s, aT_s, h1_s, h1b_s, logits_all, dbg)
        if 4 in phases:
            phase4(nc, P, A, S, bank, bank_free, ident_b, t_const, s_scr, h1_s, h1b_s, slot_s, out_s, out, logits_all, dbg,
                   wgu_r, wd_r, t_prep)
        P.op("sp", lambda e: e.nop(), waits=[(s_scr, s_scr.v)], sig=False)
        P.replay()
    return nc


def phase1(nc, P, A, S, NT, sb_outer, bank, bank_free, ident_f, t_const, s_scr,
           qT_s, kT_s, v_s, gbT_s, maT_s, dbg):
    w_in = A["w_in"]
    with ExitStack() as ps:
        def sb(name, shape, dt):
            return ps.enter_context(nc.sbuf_tensor("sb_" + name, shape, dt))

        s_w = P.sem("p1w")
        braw = sb("braw", [48, 128], F32)
        bias_fm = sb("bias_fm", [128, 48], F32)
        bq8 = sb("bq8", [128, 8], F32)
        bV = sb("bV", [128, 512], F32)
        bVAL = sb("bVAL", [128, 1024], F32)
        lng = sb("sglng", [128, 512], F32)
        lnb = sb("sglnb", [128, 512], F32)
        bsT = sb("bsT", [128, 4, 128], F32)
        wsraw = sb("wsraw", [128, 8, 128], F32)
        wsT = sb("wsT", [128, 8, 128], BF16)
        wba = sb("wba", [128, 4, D], BF16)
        P.dma("sp", lambda e: e.dma_start(out=braw[:], in_=A["b_in"][0].rearrange("(c p) -> c p", p=128)), s_w)
        P.dma("sp", lambda e: e.dma_start(out=bV[:], in_=A["b_in"][0, OFF_V:OFF_Q].partition_broadcast(128)), s_w)
        P.dma("sp", lambda e: e.dma_start(out=bVAL[:], in_=A["b_in"][0, OFF_VAL:OFF_GA].partition_broadcast(128)), s_w)
        P.dma("sp", lambda e: e.dma_start(out=lng[:], in_=A["sg_ln_g"][0].partition_broadcast(128)), s_w)
        P.dma("sp", lambda e: e.dma_start(out=lnb[:], in_=A["sg_ln_b"][0].partition_broadcast(128)), s_w)
        for g in range(8):
            P.dma("sp", lambda e, g=g: e.dma_start(out=bsT[(g % 2) * 64:(g % 2) * 64 + 64, g // 2, :],
                                                  in_=A["sg_b"][0, g].partition_broadcast(64)), s_w)
        P.dma("sp", lambda e: e.dma_start(out=wsraw[:], in_=A["sg_w"][0].rearrange("g t s -> t g s")), s_w)
        s_wba = P.sem("p1wba")
        for kc in range(4):
            P.dma("pool", lambda e, kc=kc: e.dma_start(out=wba[:, kc, :], in_=A["w_branch_a"][0, kc * 128:(kc + 1) * 128, :]), s_wba)
        t_w = (s_w, s_w.v)
        t_wba = (s_wba, s_wba.v)
        wB = sb("wB", [128, 8, 4096], BF16)
        s_wB = P.sem("wB")

        t = P.op("pe", lambda e: e.transpose(out=bank[0][:, 0:48], in_=braw[:], identity=ident_f[0:48, 0:48]),
                 waits=[t_w, t_const])
        t = P.op("dve", lambda e: e.tensor_copy(out=bias_fm[:], in_=bank[0][:, 0:48]), waits=[t])
        t_bias = P.op("dve", lambda e: e.tensor_scalar(out=bq8[:], in0=bias_fm[:, 8:16], scalar1=0.125, scalar2=None,
                                                       op0=ALU.mult), waits=[t])
        tt = []
        for g in range(8):
            tt.append(P.op("pe", lambda e, g=g: e.transpose(out=bank[1 + g // 4][:, (g % 4) * 128:(g % 4) * 128 + 128],
                                                            in_=wsraw[:, g, :], identity=ident_f[:]), waits=[t_w]))
        t = P.op("dve", lambda e: e.tensor_copy(out=wsT[:, 0:4, :], in_=bank[1][:].rearrange("p (g t) -> p g t", g=4)),
                 waits=[tt[3]])
        t = P.op("dve", lambda e: e.tensor_copy(out=wsT[:, 4:8, :], in_=bank[2][:].rearrange("p (g t) -> p g t", g=4)),
                 waits=[tt[7]])
        t_ws = P.op("dve", lambda e: e.memset(wsT[64:128, :, 0:64], 0.0))
        for b in range(3):
            bank_free[b] = t_ws

        with ExitStack() as pa:
            def sba(name, shape, dt):
                return pa.enter_context(nc.sbuf_tensor("sb_" + name, shape, dt))
            wA = sba("wA", [128, 8, 2048], BF16)
            s_wA = P.sem("wA")
            for kc in range(8):
                for (dst, src, n) in ((0, OFF_U, 1024), (1024, OFF_GA, 1024)):
                    P.dma("pool", lambda e, kc=kc, dst=dst, src=src, n=n: e.dma_start(
                        out=wA[:, kc, dst:dst + n], in_=w_in[0, kc * 128:(kc + 1) * 128, src:src + n]), s_wA)
            t_wA = (s_wA, s_wA.v)
            for kc in range(8):
                for (dst, src) in ((0, OFF_Q), (1024, OFF_K), (2048, OFF_VAL), (3072, OFF_GB)):
                    P.dma("pool", lambda e, kc=kc, dst=dst, src=src: e.dma_start(
                        out=wB[:, kc, dst:dst + 1024], in_=w_in[0, kc * 128:(kc + 1) * 128, src:src + 1024]), s_wB)
            xt = [sba("xtA0", [128, 4, D], F32)] * 2
            xt_shared = True
            xT = [sba(f"xTA{i}", [128, 8, 512], BF16) for i in range(2)]
            s_xt = [P.sem("xtA0")] * 2
            uT = sba("uT", [128, 4, 512], BF16)
            sguT = sba("sguT", [128, 4, 512], BF16)
            ga = sba("ga", [128, 8, 512], F32)
            ma_st = [sba(f"ma_st{i}", [128, 512], F32) for i in range(2)]
            s_ma = [P.sem(f"ma{i}") for i in range(2)]
            vg0 = [sba(f"vg0_{i}", [128, 512], F32) for i in range(4)]
            vg0_free = [None] * 4
            neghalfA = sba("neghalfA", [128, 1], F32)
            P.op("dve", lambda e: e.memset(neghalfA[:], -0.5))
            vg1 = sba("vg1", [128, 512], F32)
            vg2 = sba("vg2", [128, 512], F32)
            vln = [sba(f"vln{i}", [128, 512], BF16) for i in range(4)]
            mixs = sba("mixs", [128, 512], F32)
            st6 = sba("st6", [128, 6], F32)
            mv = sba("mv", [128, 2], F32)
            rs = sba("rs", [128, 2], F32)

            xt_free = [None, None]
            xT_free = [None, None]
            ma_free = [None, None]
            vln_free = [None] * 4
            uT_free = None
            sgu_free = None
            ga_free = None
            vg_free = None
            mixs_free = None
            acc_rr = [0]

            def acc_bank():
                b = 2 + acc_rr[0] % 3
                acc_rr[0] += 1
                return b

            for tt_i in range(NT):
                tok0 = tt_i * 512
                xb = tt_i % 2
                if tt_i == 0:
                    t_ld_next = P.dma("sp", lambda e: e.dma_start(
                        out=xt[0][:], in_=A["x"][0:512, :].rearrange("(b p) d -> p b d", p=128)), s_xt[0])
                t_ld = t_ld_next
                t_cp = None
                for kc in range(8):
                    pb = kc % 2
                    for tb in range(4):
                        t = P.op("pe", lambda e, pb=pb, tb=tb, kc=kc, xb=xb: e.transpose(
                            out=bank[pb][:, tb * 128:(tb + 1) * 128], in_=xt[xb][:, tb, kc * 128:(kc + 1) * 128],
                            identity=ident_f[:]), waits=[t_ld, bank_free[pb], t_const] if tb == 0 else [])
                    if kc % 2 == 0:
                        t_cp = P.op("act", lambda e, pb=pb, kc=kc, xb=xb: e.copy(out=xT[xb][:, kc, :], in_=bank[pb][:]),
                                    waits=[t, xT_free[xb]])
                    else:
                        t_cp = P.op("dve", lambda e, pb=pb, kc=kc, xb=xb: e.tensor_copy(out=xT[xb][:, kc, :], in_=bank[pb][:]),
                                    waits=[t, xT_free[xb]])
                    bank_free[pb] = t_cp
                    if kc == 6:
                        t_cp6 = t_cp
                xt_free[0] = t
                xt_free[1] = t
                t_xT = [t_cp6, t_cp]
                if tt_i + 1 < NT:
                    t_ld_next = P.dma("sp", lambda e, tok1=tok0 + 512: e.dma_start(
                        out=xt[0][:], in_=A["x"][tok1:tok1 + 512, :].rearrange("(b p) d -> p b d", p=128)),
                        s_xt[0], waits=[t])

                def fm_group(col0, b, first_waits):
                    tk = None
                    for kc in range(8):
                        tk = P.op("pe", lambda e, kc=kc, col0=col0, b=b, xb=xb: e.matmul(
                            bank[b][:], wA[:, kc, col0:col0 + 128], xT[xb][:, kc, :], start=(kc == 0), stop=(kc == 7)),
                            waits=first_waits if kc == 0 else [], sig=(kc == 7))
                    return tk

                for ch in range(4):
                    b = acc_bank()
                    t = fm_group(ch * 128, b, [t_wA, bank_free[b]] + t_xT)
                    t = P.op("act", lambda e, b=b, ch=ch: e.activation(out=uT[:, ch, :], in_=bank[b][:], func=AF.Gelu,
                                                                       bias=bias_fm[:, ch:ch + 1]),
                             waits=[t, t_bias, uT_free])
                    bank_free[b] = t
                t_uT = t
                t_vln = [None] * 4
                for tb in range(4):
                    b = acc_bank()
                    tk = None
                    for kc in range(8):
                        tk = P.op("pe", lambda e, kc=kc, b=b, tb=tb, xb=xb: e.matmul(
                            bank[b][:], xT[xb][:, kc, tb * 128:(tb + 1) * 128], wA[:, kc, 512:1024],
                            start=(kc == 0), stop=(kc == 7)),
                            waits=[t_wA, bank_free[b]] + t_xT if kc == 0 else [], sig=(kc == 7))
                    t = P.op("dve", lambda e, b=b, tb=tb: e.tensor_tensor(out=vg0[tb][:], in0=bank[b][:], in1=bV[:], op=ALU.add),
                             waits=[tk, t_w, vg0_free[tb]])
                    bank_free[b] = t
                    t = P.op("act", lambda e, tb=tb: e.activation(out=vg1[:], in_=vg0[tb][:], func=AF.Gelu), waits=[t, vg_free])
                    vg0_free[tb] = t
                    t = P.op("dve", lambda e: e.bn_stats(out=st6[:], in_=vg1[:]), waits=[t])
                    t = P.op("dve", lambda e: e.bn_aggr(out=mv[:], in_=st6[:]), waits=[t])
                    t = P.op("dve", lambda e: e.tensor_scalar(out=rs[:, 0:1], in0=mv[:, 1:2], scalar1=LN_EPS, scalar2=None,
                                                              op0=ALU.add), waits=[t])
                    t = P.op("pool", lambda e: e.tensor_tensor(out=rs[:, 1:2], in0=rs[:, 0:1], in1=neghalfA[:], op=ALU.pow), waits=[t])
                    t = P.op("dve", lambda e: e.tensor_scalar(out=vg2[:], in0=vg1[:], scalar1=mv[:, 0:1], scalar2=rs[:, 1:2],
                                                              op0=ALU.subtract, op1=ALU.mult), waits=[t])
                    vg_free = t
                    t = P.op("pool", lambda e: e.tensor_tensor(out=vg2[:], in0=vg2[:], in1=lng[:], op=ALU.mult), waits=[t])
                    t = P.op("pool", lambda e, tb=tb: e.tensor_tensor(out=vln[tb][:], in0=vg2[:], in1=lnb[:], op=ALU.add),
                             waits=[t, vln_free[tb]])
                    t_vln[tb] = t
                for m in range(8):
                    b = acc_bank()
                    t = fm_group(1024 + m * 128, b, [t_wA, bank_free[b]] + t_xT)
                    if m == 7:
                        xT_free[xb] = t
                    t = P.op("act", lambda e, b=b, m=m: e.activation(out=ga[:, m, :], in_=bank[b][:], func=AF.Sigmoid,
                                                                     bias=bias_fm[:, 32 + m:33 + m]),
                             waits=[t, t_bias, ga_free])
                    bank_free[b] = t
                t_ga = t
                for tb in range(4):
                    for gp in range(4):
                        P.op("pe", lambda e, tb=tb, gp=gp: e.matmul(
                            bank[5][:, gp * 128:(gp + 1) * 128], vln[tb][:, gp * 128:(gp + 1) * 128], wsT[:, 2 * gp, :],
                            start=True, stop=True), waits=[t_vln[tb], t_ws, bank_free[5]] if gp == 0 else [], sig=False)
                    for gp in range(4):
                        tm = P.op("pe", lambda e, tb=tb, gp=gp: e.matmul(
                            bank[6][:, gp * 128:(gp + 1) * 128], vln[tb][:, gp * 128:(gp + 1) * 128], wsT[:, 2 * gp + 1, :],
                            start=True, stop=True), waits=[bank_free[6]] if gp == 0 else [], sig=(gp == 3))
                    vln_free[tb] = tm
                    t = P.op("dve", lambda e: e.tensor_tensor(out=mixs[0:64, :], in0=bank[5][0:64, :],
                                                              in1=bsT[0:64, :, :].rearrange("p g t -> p (g t)"), op=ALU.add),
                             waits=[tm, mixs_free])
                    bank_free[5] = t
                    t = P.op("dve", lambda e: e.tensor_tensor(out=mixs[64:128, :], in0=bank[6][64:128, :],
                                                              in1=bsT[64:128, :, :].rearrange("p g t -> p (g t)"), op=ALU.add),
                             waits=[t])
                    bank_free[6] = t
                    t = P.op("dve", lambda e, tb=tb: e.tensor_tensor(
                        out=sguT[:, :, tb * 128:(tb + 1) * 128], in0=mixs[:].rearrange("p (g t) -> p g t", g=4),
                        in1=uT[:, :, tb * 128:(tb + 1) * 128], op=ALU.mult), waits=[t, t_uT, sgu_free])
                    mixs_free = t
                t_sgu = t
                uT_free = t
                for m in range(8):
                    b = acc_bank()
                    tk = None
                    for kc in range(4):
                        tk = P.op("pe", lambda e, kc=kc, b=b, m=m: e.matmul(
                            bank[b][:], wba[:, kc, m * 128:(m + 1) * 128], sguT[:, kc, :], start=(kc == 0), stop=(kc == 3)),
                            waits=[t_sgu, t_wba, bank_free[b]] if kc == 0 else [], sig=(kc == 3))
                    r = m % 2
                    t = P.op("dve", lambda e, b=b, m=m, r=r: e.tensor_tensor(out=ma_st[r][:], in0=bank[b][:], in1=ga[:, m, :],
                                                                            op=ALU.mult), waits=[tk, t_ga, ma_free[r]])
                    bank_free[b] = t
                    ma_free[r] = P.dma("sp", lambda e, m=m, r=r, tok0=tok0: e.dma_start(
                        out=maT_s[m * 128:(m + 1) * 128, tok0:tok0 + 512], in_=ma_st[r][:]), s_ma[r], waits=[t])
                    if m == 7:
                        sgu_free = tk
                ga_free = t
            t_endA = [ma_free[0], ma_free[1], t]
            for nm, tl in (("uT", uT), ("vg0", vg0[3]), ("vg1", vg1), ("vg2", vg2), ("mv", mv), ("rs", rs), ("vln1", vln[1]),
                           ("mixs", mixs), ("sguT", sguT), ("ga", ga), ("wsT", wsT), ("bsT", bsT), ("bias_fm", bias_fm), ("bV", bV), ("lng", lng), ("xTA1", xT[1])):
                dbg(nm, tl, t)
            if s_scr.v:
                t_endA.append((s_scr, s_scr.v))
            P.barrier()
            P.replay()
        with ExitStack() as pb_:
            def sbb(name, shape, dt):
                return pb_.enter_context(nc.sbuf_tensor("sb_" + name, shape, dt))
            t_wB = (s_wB, s_wB.v)
            xt = [sbb(f"xtB{i}", [128, 4, D], F32) for i in range(2)]
            xT = [sbb(f"xTB{i}", [128, 8, 512], BF16) for i in range(2)]
            s_xt = [P.sem(f"xtB{i}") for i in range(2)]
            qk_st = [sbb(f"qk_st{i}", [128, 512], BF16) for i in range(3)]
            s_qk = [P.sem(f"qk{i}") for i in range(3)]
            gb_st = [sbb(f"gb_st{i}", [128, 512], F32) for i in range(2)]
            s_gb = [P.sem(f"gb{i}") for i in range(2)]
            val_st = [sbb(f"val_st{i}", [128, 1024], BF16) for i in range(2)]
            s_val = [P.sem(f"val{i}") for i in range(2)]
            xt_free = [None, None]
            xT_free = [None, None]
            qk_free = [None] * 3
            gb_free = [None] * 2
            val_free = [None] * 2
            acc_rr = [0]
            qk_rr = 0

            def acc_bank():
                b = 2 + acc_rr[0] % 5
                acc_rr[0] += 1
                return b

            for tt_i in range(NT):
                tok0 = tt_i * 512
                xb = tt_i % 2
                if tt_i == 0:
                    t_ld_nextB = P.dma("sp", lambda e: e.dma_start(
                        out=xt[0][:], in_=A["x"][0:512, :].rearrange("(b p) d -> p b d", p=128)), s_xt[0], waits=t_endA)
                t_ld = t_ld_nextB
                if tt_i + 1 < NT:
                    xn = (tt_i + 1) % 2
                    t_ld_nextB = P.dma("sp", lambda e, xn=xn, tok1=tok0 + 512: e.dma_start(
                        out=xt[xn][:], in_=A["x"][tok1:tok1 + 512, :].rearrange("(b p) d -> p b d", p=128)),
                        s_xt[xn], waits=[xt_free[xn]] + t_endA)
                t_cp = None
                for kc in range(8):
                    pb = kc % 2
                    for tb in range(4):
                        t = P.op("pe", lambda e, pb=pb, tb=tb, kc=kc, xb=xb: e.transpose(
                            out=bank[pb][:, tb * 128:(tb + 1) * 128], in_=xt[xb][:, tb, kc * 128:(kc + 1) * 128],
                            identity=ident_f[:]), waits=[t_ld, bank_free[pb]] if tb == 0 else [])
                    if kc % 2 == 0:
                        t_cp = P.op("act", lambda e, pb=pb, kc=kc, xb=xb: e.copy(out=xT[xb][:, kc, :], in_=bank[pb][:]),
                                    waits=[t, xT_free[xb]])
                    else:
                        t_cp = P.op("dve", lambda e, pb=pb, kc=kc, xb=xb: e.tensor_copy(out=xT[xb][:, kc, :], in_=bank[pb][:]),
                                    waits=[t, xT_free[xb]])
                    bank_free[pb] = t_cp
                    if kc == 6:
                        t_cp6 = t_cp
                xt_free[xb] = t
                t_xT = [t_cp6, t_cp]

                def fm_group(col0, b, first_waits):
                    tk = None
                    for kc in range(8):
                        tk = P.op("pe", lambda e, kc=kc, col0=col0, b=b, xb=xb: e.matmul(
                            bank[b][:], wB[:, kc, col0:col0 + 128], xT[xb][:, kc, :], start=(kc == 0), stop=(kc == 7)),
                            waits=first_waits if kc == 0 else [], sig=(kc == 7))
                    return tk

                for ch in range(16):
                    isq = ch < 8
                    h = ch % 8
                    b = acc_bank()
                    t = fm_group(ch * 128, b, [t_wB, bank_free[b]] + t_xT)
                    r = qk_rr % 3
                    qk_rr += 1
                    if isq:
                        t = P.op("dve", lambda e, b=b, h=h, r=r: e.tensor_scalar(
                            out=qk_st[r][:], in0=bank[b][:], scalar1=0.125, scalar2=bq8[:, h:h + 1],
                            op0=ALU.mult, op1=ALU.add), waits=[t, t_bias, qk_free[r]])
                    else:
                        t = P.op("dve", lambda e, b=b, h=h, r=r: e.tensor_scalar(
                            out=qk_st[r][:], in0=bank[b][:], scalar1=bias_fm[:, 16 + h:17 + h], scalar2=None,
                            op0=ALU.add), waits=[t, t_bias, qk_free[r]])
                    bank_free[b] = t
                    dst = qT_s if isq else kT_s
                    for c in range(2):
                        qk_free[r] = P.dma("sp", lambda e, dst=dst, h=h, c=c, r=r, tok0=tok0: e.dma_start(
                            out=dst[h, c, :, tok0:tok0 + 512], in_=qk_st[r][c * 64:(c + 1) * 64, :]), s_qk[r], waits=[t])
                for tb in range(4):
                    r = tb % 2
                    for half in range(2):
                        b = acc_bank()
                        tk = None
                        for kc in range(8):
                            tk = P.op("pe", lambda e, kc=kc, b=b, tb=tb, half=half, xb=xb: e.matmul(
                                bank[b][:], xT[xb][:, kc, tb * 128:(tb + 1) * 128],
                                wB[:, kc, 2048 + half * 512:2048 + (half + 1) * 512], start=(kc == 0), stop=(kc == 7)),
                                waits=[t_wB, bank_free[b]] + t_xT if kc == 0 else [], sig=(kc == 7))
                        t = P.op("dve", lambda e, b=b, r=r, half=half: e.tensor_tensor(
                            out=val_st[r][:, half * 512:(half + 1) * 512], in0=bank[b][:],
                            in1=bVAL[:, half * 512:(half + 1) * 512], op=ALU.add), waits=[tk, t_w, val_free[r]])
                        bank_free[b] = t
                    val_free[r] = P.dma("sp", lambda e, r=r, tb=tb, tok0=tok0: e.dma_start(
                        out=v_s[tok0 + tb * 128:tok0 + (tb + 1) * 128, :], in_=val_st[r][:]), s_val[r], waits=[t])
                for m in range(8):
                    b = acc_bank()
                    t = fm_group(3072 + m * 128, b, [t_wB, bank_free[b]] + t_xT)
                    if m == 7:
                        xT_free[xb] = t
                    r = m % 2
                    t = P.op("act", lambda e, b=b, m=m, r=r: e.activation(out=gb_st[r][:], in_=bank[b][:], func=AF.Sigmoid,
                                                                         bias=bias_fm[:, 40 + m:41 + m]),
                             waits=[t, t_bias, gb_free[r]])
                    bank_free[b] = t
                    gb_free[r] = P.dma("sp", lambda e, m=m, r=r, tok0=tok0: e.dma_start(
                        out=gbT_s[m * 128:(m + 1) * 128, tok0:tok0 + 512], in_=gb_st[r][:]), s_gb[r], waits=[t])
            t_endB = [x for x in (qk_free + gb_free + val_free) if x is not None] + [t]
            P.barrier()
            P.replay()
    return t_endA + t_endB


def phase2(nc, P, A, S, bank, bank_free, ident_f, ident_b, t_const, s_scr, qT_s, kT_s, v_s, aT_s, dbg, prep_fns=()):
    NB = S // 128
    NQP = S // 256
    t_prev = (s_scr, s_scr.v)
    with ExitStack() as ps:
        def sb(name, shape, dt):
            return ps.enter_context(nc.sbuf_tensor("sb_" + name, shape, dt))
        qa2 = [[sb(f"qa{p}{c}", [68, S], BF16) for c in range(2)] for p in range(2)]
        ka2 = [[sb(f"ka{p}{c}", [68, S], BF16) for c in range(2)] for p in range(2)]
        Vh2 = [sb(f"Vh{p}", [128, NB, 129], BF16) for p in range(2)]
        Osb = sb("Osb", [128, 2, 2, 129], F32)
        rc4 = sb("rc4", [128, 2, 2], F32)
        r1l = sb("r1l", [128, 2], F32)
        ssq = sb("ssq", [128, 2], F32)
        rstd2 = sb("rstd2", [128, 2], F32)
        corrT = sb("corrT", [128, H, 128], BF16)
        lamt = sb("lamt", [128, 4, 64], F32)
        lamw = sb("lamw", [128, 2, 64], F32)
        lams = sb("lams", [128, 4], F32)
        neglam = sb("neglam", [128, 1], F32)
        neghalf = sb("neghalf", [128, 1], F32)
        neghalf2 = sb("neghalf2", [128, 2], F32)
        PT = [sb(f"PT{i}", [128, 512], BF16) for i in range(3)]
        rcp = [sb(f"rcp{j}", [128, 4], F32) for j in range(2)]
        Abuf = [sb(f"Abuf{j}", [128, 128], F32) for j in range(2)]
        Dbuf = [sb(f"Dbuf{j}", [128, 128], F32) for j in range(2)]
        sq = sb("sqjunk", [128, 128], F32)
        On = [[sb(f"On{j}{p}", [128, 128], BF16) for p in range(2)] for j in range(2)]
        aT_st = [sb(f"aT_st{i}", [128, 256], BF16) for i in range(2)]
        s_c2 = P.sem("p2c")
        s_ld2 = [P.sem(f"p2ld{p}") for p in range(2)]
        s_ast = [P.sem(f"p2a{i}") for i in range(2)]

        P.dma("sp", lambda e: e.dma_start(out=corrT[:], in_=A["corrT"]), s_c2, waits=[t_prev])
        for i, nm in enumerate(("lam_q1", "lam_k1", "lam_q2", "lam_k2")):
            P.dma("sp", lambda e, i=i, nm=nm: e.dma_start(out=lamt[:, i, :], in_=A[nm][0].partition_broadcast(128)), s_c2)
        t_c2 = (s_c2, s_c2.v)
        P.op("dve", lambda e: e.memset(Vh2[0][:, :, 128:129], 1.0), waits=[t_prev])
        t = P.op("dve", lambda e: e.memset(Vh2[1][:, :, 128:129], 1.0))
        t_ones = t
        P.op("dve", lambda e: e.memset(neghalf[:], -0.5), sig=False)
        P.op("dve", lambda e: e.memset(neghalf2[:], -0.5), sig=False)
        P.op("dve", lambda e: e.tensor_tensor(out=lamw[:, 0, :], in0=lamt[:, 0, :], in1=lamt[:, 1, :], op=ALU.mult),
             waits=[t_c2], sig=False)
        t = P.op("dve", lambda e: e.tensor_tensor(out=lamw[:, 1, :], in0=lamt[:, 2, :], in1=lamt[:, 3, :], op=ALU.mult))
        t = P.op("dve", lambda e: e.tensor_reduce(out=lams[:, 0:2], in_=lamw[:], axis=AX.X, op=ALU.add), waits=[t])
        t = P.op("act", lambda e: e.activation(out=lams[:, 2:4], in_=lams[:, 0:2], func=AF.Exp), waits=[t])
        t = P.op("dve", lambda e: e.tensor_tensor(out=lams[:, 0:1], in0=lams[:, 3:4], in1=lams[:, 2:3], op=ALU.subtract),
                 waits=[t])
        t_lam = P.op("dve", lambda e: e.tensor_scalar(out=neglam[:], in0=lams[:, 0:1], scalar1=-LAMBDA_INIT, scalar2=None,
                                                      op0=ALU.add), waits=[t])

        ST = [bank[0], bank[1], bank[6]]
        OB = [[bank[2], bank[3]], [bank[4], bank[5]]]
        TB = bank[7]
        st_free = [bank_free[0], bank_free[1], bank_free[6]]
        pt_free = [None, None, None]
        o_free = [[bank_free[2], bank_free[3]], [bank_free[4], bank_free[5]]]
        tb_free = bank_free[7]
        ast_free = [None, None]
        on_free = [[None, None], [None, None]]
        osb_free = [None]
        n_ast = 0

        head_done = [None] * H
        t_hlds = [None] * H

        def issue_loads(h):
            p = h % 2
            w = [t_prev] + (head_done[h - 2] if h >= 2 else [])
            sl = s_ld2[p]
            for c in range(2):
                P.dma("sp", lambda e, c=c, h=h, p=p: e.dma_start(out=qa2[p][c][0:64, :], in_=qT_s[h, c]), sl, waits=w)
                P.dma("sp", lambda e, c=c, h=h, p=p: e.dma_start(out=qa2[p][c][64:68, :], in_=A["qaug"][h]), sl)
                P.dma("sp", lambda e, c=c, h=h, p=p: e.dma_start(out=ka2[p][c][0:64, :], in_=kT_s[h, c]), sl)
                P.dma("sp", lambda e, c=c, h=h, p=p: e.dma_start(out=ka2[p][c][64:68, :], in_=A["kaug"][h]), sl)
            P.dma("sp", lambda e, h=h, p=p: e.dma_start(
                out=Vh2[p][:, :, 0:128], in_=v_s.rearrange("(kb p) d -> p kb d", p=128)[:, :, h * 128:(h + 1) * 128]), sl)
            t_hlds[h] = (sl, sl.v)

        issue_loads(0)
        slopes = _slopes()
        for h in range(H):
            if h + 1 < H:
                issue_loads(h + 1)
            npf = (len(prep_fns) + H - 1) // H
            for f in prep_fns[h * npf:(h + 1) * npf]:
                f()
            t_hld = t_hlds[h]
            qa, ka, Vh = qa2[h % 2], ka2[h % 2], Vh2[h % 2]
            def kb_first(qp, h=h):
                for kb in range(2 * qp + 2):
                    if slopes[h] * (256 * qp - (128 * kb + 127)) < 64.0:
                        return kb
                return 2 * qp
            kb0 = [kb_first(qp) for qp in range(NQP)]
            units = [(qp, kb) for qp in range(NQP) for kb in range(kb0[qp], 2 * qp + 2)]
            chain_q = []
            deferred = []
            qk_tok = {}

            def emit_qk(i):
                qp, kb = units[i]
                b = i % 3
                q0 = qp * 256
                last = (kb == 2 * qp + 1)
                diag0 = (kb == 2 * qp)
                tk = None
                for c in range(2):
                    w = ([st_free[b]] + ([t_hld, t_c2] if i < 3 else [])) if c == 0 else []
                    if last:
                        P.op("pe", lambda e, c=c, b=b, kb=kb, q0=q0, ka=ka, qa=qa: e.matmul(
                            ST[b][:, c * 256 + 128:c * 256 + 256], ka[c][0:68, kb * 128:(kb + 1) * 128],
                            qa[c][0:68, q0 + 128:q0 + 256], start=True, stop=False), waits=w, sig=False)
                        tk = P.op("pe", lambda e, c=c, b=b, h=h: e.matmul(
                            ST[b][:, c * 256 + 128:c * 256 + 256], ident_b[:], corrT[:, h, :], start=False, stop=True), sig=(c == 1))
                    else:
                        tk = P.op("pe", lambda e, c=c, b=b, kb=kb, q0=q0, diag0=diag0, ka=ka, qa=qa: e.matmul(
                            ST[b][:, c * 256:c * 256 + 256], ka[c][0:68, kb * 128:(kb + 1) * 128],
                            qa[c][0:68, q0:q0 + 256], start=True, stop=not diag0), waits=w, sig=(not diag0 and c == 1))
                        if diag0:
                            tk = P.op("pe", lambda e, c=c, b=b, h=h: e.matmul(
                                ST[b][:, c * 256:c * 256 + 128], ident_b[:], corrT[:, h, :], start=False, stop=True), sig=(c == 1))
                qk_tok[i] = tk

            emit_qk(0)
            if len(units) > 1:
                emit_qk(1)
            for i, (qp, kb) in enumerate(units):
                b = i % 3
                last = (kb == 2 * qp + 1)
                if last:
                    src = ST[b][:].rearrange("p (c j q) -> p c j q", c=2, j=2)[:, :, 1, :]
                    dst = PT[b][:].rearrange("p (c j q) -> p c j q", c=2, j=2)[:, :, 1, :]
                else:
                    src = ST[b][:]
                    dst = PT[b][:]
                t_exp = P.op("act", lambda e, src=src, dst=dst: e.activation(out=dst, in_=src, func=AF.Exp),
                             waits=[qk_tok[i], pt_free[b]])
                st_free[b] = t_exp
                if i + 2 < len(units):
                    emit_qk(i + 2)
                while deferred and deferred[0][0] <= i and deferred[0][2] <= qp - 1:
                    deferred.pop(0)[1]()
                tk = None
                pv_list = [(j, c) for j in range(2) for c in range(2) if not (last and j == 0)]
                for n_, (j, c) in enumerate(pv_list):
                    stop = (kb == 2 * qp + j)
                    w = ([t_exp] + ([t_ones] if i == 0 else [])) if n_ == 0 else []
                    if kb == kb0[qp]:
                        w = w + [o_free[j][c]]
                    tk = P.op("pe", lambda e, j=j, c=c, b=b, kb=kb, stop=stop, Vh=Vh, st_=(kb == kb0[qp]): e.matmul(
                        OB[j][c][:, 0:129], PT[b][:, c * 256 + j * 128:c * 256 + (j + 1) * 128], Vh[:, kb, :],
                        start=st_, stop=stop), waits=w, sig=(n_ == len(pv_list) - 1))
                pt_free[b] = tk
                for j in range(2):
                    if kb != 2 * qp + j:
                        continue
                    t = P.op("act", lambda e, j=j: e.copy(out=Osb[:, j, 0, :], in_=OB[j][0][:, 0:129]), waits=[tk, osb_free[0]])
                    o_free[j][0] = t
                    t = P.op("act", lambda e, j=j: e.copy(out=Osb[:, j, 1, :], in_=OB[j][1][:, 0:129]), waits=[t])
                    o_free[j][1] = t
                    chain_q.append((j, qp, t))
                if last:
                  while deferred and deferred[0][2] <= qp - 2:
                      deferred.pop(0)[1]()
                  t = chain_q[-1][2]
                  t = P.op("dve", lambda e: e.reciprocal(out=rc4[:], in_=Osb[:, :, :, 128]), waits=[t])
                  t = P.op("dve", lambda e: e.tensor_scalar(out=r1l[:], in0=rc4[:, :, 1], scalar1=neglam[:, 0:1], scalar2=None,
                                                           op0=ALU.mult), waits=[t, t_lam])
                  for j in range(2):
                      t = P.op("dve", lambda e, j=j: e.tensor_scalar(out=Abuf[j][:], in0=Osb[:, j, 0, 0:128],
                                                                    scalar1=rc4[:, j, 0:1], scalar2=None, op0=ALU.mult), waits=[t])
                      t = P.op("dve", lambda e, j=j: e.scalar_tensor_tensor(out=Dbuf[j][:], in0=Osb[:, j, 1, 0:128],
                                                                           scalar=r1l[:, j:j + 1], in1=Abuf[j][:],
                                                                           op0=ALU.mult, op1=ALU.add), waits=[t])
                      osb_free[0] = t
                      t = P.op("dve", lambda e, j=j: e.scalar_tensor_tensor(out=sq[:], in0=Dbuf[j][:], scalar=1.0, in1=Dbuf[j][:],
                                                                           op0=ALU.mult, op1=ALU.mult,
                                                                           accum_out=ssq[:, j:j + 1]), waits=[t])
                  t = P.op("dve", lambda e: e.tensor_scalar(out=ssq[:], in0=ssq[:], scalar1=1.0 / 128.0, scalar2=RMS_EPS,
                                                           op0=ALU.mult, op1=ALU.add), waits=[t])
                  t = P.op("pool", lambda e: e.tensor_tensor(out=rstd2[:], in0=ssq[:], in1=neghalf2[:], op=ALU.pow), waits=[t])
                  for (j, qp_, _t) in chain_q:
                    t_on = P.op("dve", lambda e, j=j, qpar=qp_ % 2: e.tensor_scalar(out=On[j][qpar][:], in0=Dbuf[j][:],
                                                                                   scalar1=rstd2[:, j:j + 1], scalar2=None,
                                                                                   op0=ALU.mult), waits=[t, on_free[j][qp_ % 2]])

                    def fin(j=j, t_on=t_on, qp=qp_, h=h):
                        nonlocal tb_free, n_ast
                        tt = P.op("pe", lambda e, j=j, qpar=qp % 2: e.transpose(out=TB[:, j * 128:(j + 1) * 128], in_=On[j][qpar][:],
                                                                               identity=ident_b[:]),
                                  waits=[t_on] + ([tb_free] if j == 0 else []))
                        on_free[j][qp % 2] = tt
                        if j == 1:
                            r = n_ast % 2
                            n_ast += 1
                            tc = P.op("dve", lambda e, r=r: e.tensor_copy(out=aT_st[r][:], in_=TB[:, 0:256]),
                                      waits=[tt, ast_free[r]])
                            tb_free = tc
                            ast_free[r] = P.dma("sp", lambda e, r=r, qp=qp, h=h: e.dma_start(
                                out=aT_s[h * 128:(h + 1) * 128, qp * 256:(qp + 1) * 256], in_=aT_st[r][:]), s_ast[r],
                                waits=[tc])
                    deferred.append((i + 5, fin, qp_))
                  chain_q = []
            while deferred:
                deferred.pop(0)[1]()
            head_done[h] = [tk, (P.esem["pe"], P.esem["pe"].v)]
        for bi, tkn in ((0, st_free[0]), (1, st_free[1]), (6, st_free[2]), (2, o_free[0][0]), (3, o_free[0][1]), (4, o_free[1][0]),
                        (5, o_free[1][1]), (7, tb_free)):
            bank_free[bi] = tkn
        t_end = [x for x in ast_free if x is not None] + [(P.esem[k], P.esem[k].v) for k in ("pe", "act", "dve", "pool")]
        P.barrier()
        P.replay()
    return t_end


def phase3(nc, P, A, S, bank, bank_free, ident_f, t_const, s_scr, gbT_s, maT_s, aT_s, h1_s, h1b_s, logits_all, dbg):
    NT = S // 512
    t_prev = (s_scr, s_scr.v)
    with ExitStack() as ps:
        def sb(name, shape, dt):
            return ps.enter_context(nc.sbuf_tensor("sb_" + name, shape, dt))
        wbb = sb("wbb", [128, 8, D], BF16)
        wst = [sb(f"wst{i}", [128, D], F32) for i in range(2)]
        wout = sb("wout", [128, 8, D], BF16)
        sublg = sb("sublg", [128, 1], F32)
        ln1g = sb("ln1g", [128, D], F32)
        ln1b = sb("ln1b", [128, D], F32)
        wr = sb("wr", [128, 8, 36], F32)
        rbias = sb("rbias", [128, 36], F32)
        aT = [sb(f"aT{i}", [128, 8, 512], BF16) for i in range(2)]
        NGC = 6
        gbc = [sb(f"gbc{i}", [128, 512], F32) for i in range(NGC)]
        mac = [sb(f"mac{i}", [128, 512], F32) for i in range(NGC)]
        s_gc = [P.sem(f"p3gc{i}") for i in range(NGC)]
        gc_free = [None] * NGC
        t_gc = {}

        def issue_gc(ci):
            g = ci % NGC
            tt_, m_ = divmod(ci, 8)
            P.dma("sp", lambda e, g=g, tt_=tt_, m_=m_: e.dma_start(
                out=gbc[g][:], in_=gbT_s[m_ * 128:(m_ + 1) * 128, tt_ * 512:(tt_ + 1) * 512]), s_gc[g], waits=[t_prev, gc_free[g]])
            t_gc[ci] = P.dma("sp", lambda e, g=g, tt_=tt_, m_=m_: e.dma_start(
                out=mac[g][:], in_=maT_s[m_ * 128:(m_ + 1) * 128, tt_ * 512:(tt_ + 1) * 512]), s_gc[g])
        xt = [sb(f"xt3{i}", [128, 4, D], F32) for i in range(2)]
        tmpg = [sb(f"tmpg{i}", [128, 512], F32) for i in range(2)]
        mgT = sb("mgT", [128, 8, 512], BF16)
        z = [sb(f"z{i}", [128, D], F32) for i in range(2)]
        zn = [sb(f"zn{i}", [128, D], F32) for i in range(2)]
        h1 = [sb(f"h1t{i}", [128, D], F32) for i in range(2)]
        h1b = [sb(f"h1bt{i}", [128, D], BF16) for i in range(2)]
        h1T = sb("h1T", [128, 8, 128], F32)
        st12 = sb("st12", [128, 2, 6], F32)
        mv = sb("mv3", [128, 2], F32)
        rr = sb("rr3", [128, 2], F32)
        neghalf = sb("neghalf3", [128, 1], F32)
        s_w = P.sem("p3w")
        s_wst = [P.sem(f"p3wst{i}") for i in range(2)]
        s_in = [P.sem(f"p3in{i}") for i in range(2)]
        s_xin = [P.sem(f"p3xin{i}") for i in range(2)]
        s_h1 = [P.sem(f"p3h1{i}") for i in range(2)]

        P.dma("sp", lambda e: e.dma_start(out=sublg[:], in_=A["subln_g"][0].rearrange("(p o) -> p o", o=1)), s_w, waits=[t_prev])
        P.dma("sp", lambda e: e.dma_start(out=ln1g[:], in_=A["ln1_g"][0].partition_broadcast(128)), s_w)
        P.dma("sp", lambda e: e.dma_start(out=ln1b[:], in_=A["ln1_b"][0].partition_broadcast(128)), s_w)
        P.dma("sp", lambda e: e.dma_start(out=wr[:, :, 0:4], in_=A["w_group"][0].rearrange("(kc p) n -> p kc n", p=128)), s_w)
        P.dma("sp", lambda e: e.dma_start(out=wr[:, :, 4:36], in_=A["w_expert"][0].rearrange("(kc p) n -> p kc n", p=128)), s_w)
        P.dma("sp", lambda e: e.dma_start(out=rbias[:, 0:4], in_=A["b_group"][0].partition_broadcast(128)), s_w)
        P.dma("sp", lambda e: e.dma_start(out=rbias[:, 4:36], in_=A["b_expert"][0].partition_broadcast(128)), s_w)
        t_w = (s_w, s_w.v)
        s_wo = P.sem("p3wo")
        for kc in range(8):
            P.dma("pool", lambda e, kc=kc: e.dma_start(out=wout[:, kc, :], in_=A["w_out"][0, kc * 128:(kc + 1) * 128, :]), s_wo,
                  waits=[t_prev])
        t_wo = (s_wo, s_wo.v)
        P.op("dve", lambda e: e.memset(neghalf[:], -0.5), waits=[t_prev], sig=False)
        wst_free = [None, None]
        t_wbb = None
        for hh in range(8):
            r = hh % 2
            tl = P.dma("sp", lambda e, hh=hh, r=r: e.dma_start(out=wst[r][:], in_=A["w_branch_b"][0, hh * 128:(hh + 1) * 128, :]),
                       s_wst[r], waits=[wst_free[r], t_prev])
            t_wbb = P.op("dve", lambda e, hh=hh, r=r: e.tensor_scalar(out=wbb[:, hh, :], in0=wst[r][:], scalar1=sublg[:, 0:1],
                                                                      scalar2=1.0 - LAMBDA_INIT, op0=ALU.mult, op1=ALU.mult),
                         waits=[tl, t_w])
            wst_free[r] = t_wbb

        aT_free = [None, None]
        xt_free = [None, None]
        tmp_free = [None, None]
        mg_free = [None, None]
        z_free = [None, None]
        zn_free = [None, None]
        h1_free = [[], []]
        h1b_free = [None, None]
        h1T_free = None
        rr_acc = [0]
        mgT2 = [mgT, sb("mgT1", [128, 8, 512], BF16)]
        t_aT = {}
        t_xt = {}
        t_mg_done = {}

        def acc_bank():
            b = rr_acc[0] % 7
            rr_acc[0] += 1
            return b

        def issue_in(tt_i):
            ib = tt_i % 2
            tok0 = tt_i * 512
            t_aT[tt_i] = P.dma("sp", lambda e, ib=ib, tok0=tok0: e.dma_start(
                out=aT[ib][:], in_=aT_s.rearrange("(h p) s -> p h s", p=128)[:, :, tok0:tok0 + 512]), s_in[ib],
                waits=[t_prev, aT_free[ib]])
            t_xt[tt_i] = P.dma("sp", lambda e, ib=ib, tok0=tok0: e.dma_start(
                out=xt[ib][:], in_=A["x"][tok0:tok0 + 512, :].rearrange("(b p) d -> p b d", p=128)), s_xin[ib],
                waits=[xt_free[ib]])

        def emit_yb(tt_i, m):
            ib = tt_i % 2
            b = acc_bank()
            tk = None
            for kc in range(8):
                tk = P.op("pe", lambda e, kc=kc, b=b, m=m, ib=ib: e.matmul(
                    bank[b][:], wbb[:, kc, m * 128:(m + 1) * 128], aT[ib][:, kc, :], start=(kc == 0), stop=(kc == 7)),
                    waits=[t_aT[tt_i], t_wbb, bank_free[b]] if kc == 0 else [], sig=(kc == 7))
            if m == 7:
                aT_free[ib] = tk
            r = m % 2
            ci = tt_i * 8 + m
            g = ci % NGC
            if ci + NGC - 1 < NT * 8:
                issue_gc(ci + NGC - 1)
            t = P.op("dve", lambda e, b=b, g=g, r=r: e.tensor_tensor(out=tmpg[r][:], in0=bank[b][:], in1=gbc[g][:],
                                                                    op=ALU.mult), waits=[tk, tmp_free[r], t_gc[ci]])
            bank_free[b] = t
            t_mg = P.op("pool", lambda e, m=m, g=g, r=r, ib=ib: e.tensor_tensor(out=mgT2[ib][:, m, :], in0=tmpg[r][:], in1=mac[g][:],
                                                                               op=ALU.add), waits=[t, mg_free[ib] if m == 0 else None])
            tmp_free[r] = t_mg
            gc_free[g] = t_mg
            if m == 7:
                t_mg_done[tt_i] = t_mg

        nblk = 0
        for ci in range(NGC - 1):
            issue_gc(ci)
        issue_in(0)
        for m in range(8):
            emit_yb(0, m)
        for tt_i in range(NT):
            tok0 = tt_i * 512
            ib = tt_i % 2
            if tt_i + 1 < NT:
                issue_in(tt_i + 1)
            t_mg = t_mg_done[tt_i]
            for tb in range(4):
                zb = nblk % 2
                nblk += 1
                tz = None
                for half in range(2):
                    b = acc_bank()
                    tk = None
                    for kc in range(8):
                        tk = P.op("pe", lambda e, kc=kc, b=b, tb=tb, half=half, ib=ib: e.matmul(
                            bank[b][:], mgT2[ib][:, kc, tb * 128:(tb + 1) * 128], wout[:, kc, half * 512:(half + 1) * 512],
                            start=(kc == 0), stop=(kc == 7)), waits=[t_mg, t_wo, bank_free[b]] if kc == 0 else [], sig=(kc == 7))
                    tz = P.op("dve", lambda e, b=b, tb=tb, half=half, ib=ib, zb=zb: e.scalar_tensor_tensor(
                        out=z[zb][:, half * 512:(half + 1) * 512], in0=xt[ib][:, tb, half * 512:(half + 1) * 512], scalar=ALPHA,
                        in1=bank[b][:], op0=ALU.mult, op1=ALU.add), waits=[tk, z_free[zb], t_xt[tt_i]])
                    bank_free[b] = tz
                    tz = P.op("dve", lambda e, half=half, zb=zb: e.bn_stats(out=st12[:, half, :],
                                                                           in_=z[zb][:, half * 512:(half + 1) * 512]), waits=[tz])
                if tb == 3:
                    mg_free[ib] = tk
                    xt_free[ib] = tz
                if tt_i + 1 < NT:
                    emit_yb(tt_i + 1, 2 * tb)
                    emit_yb(tt_i + 1, 2 * tb + 1)

                t = P.op("dve", lambda e: e.bn_aggr(out=mv[:], in_=st12[:].rearrange("p a b -> p (a b)")), waits=[tz])
                t = P.op("dve", lambda e: e.tensor_scalar(out=rr[:, 0:1], in0=mv[:, 1:2], scalar1=LN_EPS, scalar2=None, op0=ALU.add),
                         waits=[t])
                t = P.op("act", lambda e: e.activation(out=rr[:, 0:1], in_=rr[:, 0:1], func=AF.Sqrt), waits=[t])
                t = P.op("dve", lambda e: e.reciprocal(out=rr[:, 0:1], in_=rr[:, 0:1]), waits=[t])
                t = P.op("dve", lambda e: e.scalar_tensor_tensor(out=rr[:, 1:2], in0=mv[:, 0:1], scalar=-1.0, in1=rr[:, 0:1],
                                                                op0=ALU.mult, op1=ALU.mult), waits=[t])
                t = P.op("act", lambda e, zb=zb: e.activation(out=zn[zb][:], in_=z[zb][:], func=AF.Identity, bias=rr[:, 1:2],
                                                             scale=rr[:, 0:1]), waits=[t, zn_free[zb]])
                z_free[zb] = t
                t = P.op("dve", lambda e, zb=zb: e.tensor_tensor(out=zn[zb][:], in0=zn[zb][:], in1=ln1g[:], op=ALU.mult),
                         waits=[t, t_w])
                t_h1 = P.op("pool", lambda e, zb=zb: e.tensor_tensor(out=h1[zb][:], in0=zn[zb][:], in1=ln1b[:], op=ALU.add),
                            waits=[t] + h1_free[zb])
                zn_free[zb] = t_h1
                t_b = P.op("act", lambda e, zb=zb: e.copy(out=h1b[zb][:], in_=h1[zb][:]), waits=[t_h1, h1b_free[zb]])
                r0 = tok0 + tb * 128
                td1 = P.dma("sp", lambda e, zb=zb, r0=r0: e.dma_start(out=h1_s[r0:r0 + 128, :], in_=h1[zb][:]), s_h1[zb], waits=[t_h1])
                h1b_free[zb] = P.dma("sp", lambda e, zb=zb, r0=r0: e.dma_start(out=h1b_s[r0:r0 + 128, :], in_=h1b[zb][:]), s_h1[zb],
                                     waits=[t_b])
                h1_free[zb] = [h1b_free[zb]]
        t_h1done = [(s_h1[0], s_h1[0].v), (s_h1[1], s_h1[1].v)]
        NRB = 4
        hr = [xt[0][:, i, :] for i in range(4)]
        s_hr = [P.sem(f"p3hr{i}") for i in range(NRB)]
        hr_free = [None] * NRB
        t_hr = {}
        t_main_done = [(P.esem[k], P.esem[k].v) for k in ("pe", "act", "dve", "pool")]

        def issue_hr(bi):
            g = bi % NRB
            t_hr[bi] = P.dma("sp", lambda e, bi=bi, g=g: e.dma_start(out=hr[g], in_=h1_s[bi * 128:(bi + 1) * 128, :]), s_hr[g],
                             waits=t_h1done + t_main_done + [hr_free[g]])
        NBk = S // 128
        for bi in range(min(NRB - 1, NBk)):
            issue_hr(bi)
        for bi in range(NBk):
            g = bi % NRB
            if bi + NRB - 1 < NBk:
                issue_hr(bi + NRB - 1)
            tks = []
            for kc in range(8):
                bq = 4 + kc // 4
                tks.append(P.op("pe", lambda e, kc=kc, bq=bq, g=g: e.transpose(
                    out=bank[bq][:, (kc % 4) * 128:(kc % 4 + 1) * 128], in_=hr[g][:, kc * 128:(kc + 1) * 128],
                    identity=ident_f[:]), waits=[t_hr[bi], bank_free[bq], t_const] if kc % 4 == 0 else []))
            hr_free[g] = tks[7]
            t = P.op("act", lambda e: e.copy(out=h1T[:, 0:4, :], in_=bank[4][:].rearrange("p (k t) -> p k t", k=4)),
                     waits=[tks[3], h1T_free])
            bank_free[4] = t
            t = P.op("dve", lambda e: e.tensor_copy(out=h1T[:, 4:8, :], in_=bank[5][:].rearrange("p (k t) -> p k t", k=4)),
                     waits=[tks[7], h1T_free])
            bank_free[5] = t
            tk = None
            for kc in range(8):
                tk = P.op("pe", lambda e, kc=kc: e.matmul(bank[6][:, 0:36], h1T[:, kc, :], wr[:, kc, :], start=(kc == 0),
                                                         stop=(kc == 7)),
                          waits=[t, bank_free[4], t_w, bank_free[6]] if kc == 0 else [], sig=(kc == 7))
            h1T_free = tk
            t = P.op("dve", lambda e, bi=bi: e.tensor_tensor(out=logits_all[:, bi, :], in0=bank[6][:, 0:36], in1=rbias[:],
                                                            op=ALU.add), waits=[tk])
            bank_free[6] = t
        t_end = [(s_h1[0], s_h1[0].v), (s_h1[1], s_h1[1].v)] + [(P.esem[k], P.esem[k].v) for k in ("pe", "act", "dve", "pool")]
        dbg("logits", logits_all, t_end[-2])
        P.barrier()
        P.replay()
    return t_end


def phase4(nc, P, A, S, bank, bank_free, ident_b, t_const, s_scr, h1_s, h1b_s, slot_s, out_s, out, logits_all, dbg,
           wgu_r, wd_r, t_prep):
    NB = S // 128
    NBLK = 2 * S // MBLK + NE
    NCH = NBLK * 2
    t_prev = (s_scr, s_scr.v)
    with ExitStack() as ps:
        def sb(name, shape, dt):
            return ps.enter_context(nc.sbuf_tensor("sb_" + name, shape, dt))
        L = logits_all
        ones_b = sb("ones_b", [128, 128], BF16)
        triu_b = sb("triu_b", [128, 128], BF16)
        tokidx = sb("tokidx", [128, NB], I32)
        blkstart = sb("blkstart", [128, NBLK], F32)
        w1 = sb("w1", [128, NB], F32)
        w2 = sb("w2", [128, NB], F32)
        dest_i = [sb(f"dest_i{k}", [128, NB], I32) for k in range(2)]
        sidx = sb("sidx", [128, NCH], I32)
        be_i = sb("be_i", [128, NBLK], I32)
        widx_i = sb("widx_i", [128, NBLK], I32)
        pk = sb("pk", [128, 8], F32)
        rs = ExitStack()

        def sbt(name, shape, dt):
            return rs.enter_context(nc.sbuf_tensor("sb_" + name, shape, dt))
        gsh = sbt("gsh", [128, NB, 4], F32)
        gex = sbt("gex", [128, NB, 4], F32)
        gsum = sbt("gsum", [128, NB], F32)
        gw = sbt("gw", [128, NB], F32)
        dm = sbt("dmat", [128, NB], F32)
        oh = [sbt(f"oh{k}", [128, NB, NE], F32) for k in range(2)]
        Call = sbt("Call", [128, NB, NE], BF16)
        sm = sbt("rt_small", [128, 16], F32)
        gmask = sbt("gmask", [128, 4], F32)
        pen = sbt("pen", [128, 4], F32)
        msk = sbt("msk", [128, NE], F32)
        top8 = sbt("top8", [128, 8], F32)
        colsum = sbt("colsum", [128, NB, NE], F32)
        cum = sbt("cum", [128, NB + 1, NE], F32)
        dest_all = sbt("dest_all", [128, NB, NE], F32)
        prod = sbt("prod", [128, NB, NE], F32)
        cnt = sbt("cnt", [128, NE], F32)
        cnt_i = sbt("cnt_i", [128, NE], I32)
        padded = sbt("padded", [128, NE], F32)
        pe_a = sbt("pe_a", [128, NE], F32)
        pe_b = sbt("pe_b", [128, NE], F32)
        pstart = sbt("pstart", [128, NE], F32)
        dest_f = [sbt(f"dest_f{k}", [128, NB], F32) for k in range(2)]
        tmp_i = [sbt(f"tmp_i{k}", [128, NB], I32) for k in range(2)]
        tmp_f = [sbt(f"tmp_f{k}", [128, NB], F32) for k in range(2)]
        addr_i = [sbt(f"addr_i{k}", [128, NB], I32) for k in range(2)]
        zero_i = sbt("zero_i", [128, NCH], I32)
        be_f = sbt("be_f", [128, NBLK], F32)
        s_c = P.sem("p4c")
        P.dma("sp", lambda e: e.dma_start(out=ones_b[:], in_=A["ones_b"]), s_c, waits=[t_prev])
        P.dma("sp", lambda e: e.dma_start(out=triu_b[:], in_=A["triu_b"]), s_c)
        P.dma("sp", lambda e: e.dma_start(out=tokidx[:], in_=A["tokidx"]), s_c)
        P.dma("sp", lambda e: e.dma_start(out=blkstart[:], in_=A["blkstart"]), s_c)
        t_c = (s_c, s_c.v)

        t = None
        for b in range(NB):
            t = P.op("dve", lambda e, b=b: e.tensor_reduce(out=sm[:, 0:1], in_=L[:, b, 0:4], axis=AX.X, op=ALU.max),
                     waits=[t_prev] if b == 0 else [t])
            t = P.op("dve", lambda e, b=b: e.tensor_scalar(out=gmask[:], in0=L[:, b, 0:4], scalar1=sm[:, 0:1], scalar2=None,
                                                          op0=ALU.is_equal), waits=[t])
            t = P.op("dve", lambda e, b=b: e.tensor_scalar(out=gsh[:, b, :], in0=L[:, b, 0:4], scalar1=sm[:, 0:1], scalar2=None,
                                                          op0=ALU.subtract), waits=[t])
            t = P.op("dve", lambda e: e.tensor_scalar(out=pen[:], in0=gmask[:], scalar1=1e30, scalar2=-1e30, op0=ALU.mult,
                                                     op1=ALU.add), waits=[t])
            for g in range(4):
                t = P.op("dve", lambda e, b=b, g=g: e.tensor_scalar(out=msk[:, g * 8:(g + 1) * 8], in0=L[:, b, 4 + g * 8:12 + g * 8],
                                                                   scalar1=pen[:, g:g + 1], scalar2=None, op0=ALU.add), waits=[t])
            t = P.op("dve", lambda e: e.max(out=top8[:], in_=msk[:]), waits=[t])
            t = P.op("dve", lambda e, b=b: e.tensor_scalar(out=oh[0][:, b, :], in0=msk[:], scalar1=top8[:, 0:1], scalar2=None,
                                                          op0=ALU.is_equal), waits=[t])
            t = P.op("dve", lambda e, b=b: e.tensor_scalar(out=oh[1][:, b, :], in0=msk[:], scalar1=top8[:, 1:2], scalar2=None,
                                                          op0=ALU.is_equal), waits=[t])
            t = P.op("dve", lambda e, b=b: e.tensor_tensor(out=dm[:, b:b + 1], in0=top8[:, 0:1], in1=top8[:, 1:2],
                                                          op=ALU.subtract), waits=[t])
            t = P.op("dve", lambda e, b=b: e.tensor_tensor(out=Call[:, b, :], in0=oh[0][:, b, :], in1=oh[1][:, b, :], op=ALU.add),
                     waits=[t])
        t_route = t
        ta = P.op("act", lambda e: e.activation(out=gex[:].rearrange("p b g -> p (b g)"), in_=gsh[:].rearrange("p b g -> p (b g)"),
                                                func=AF.Exp), waits=[t_route])
        ta2 = P.op("act", lambda e: e.activation(out=dm[:], in_=dm[:], func=AF.Sigmoid), waits=[ta])
        t = P.op("dve", lambda e: e.tensor_reduce(out=gsum[:], in_=gex[:], axis=AX.X, op=ALU.add), waits=[ta])
        t = P.op("dve", lambda e: e.reciprocal(out=gw[:], in_=gsum[:]), waits=[t])
        t = P.op("dve", lambda e: e.tensor_tensor(out=w1[:], in0=gw[:], in1=dm[:], op=ALU.mult), waits=[t, ta2])
        t_w12 = P.op("dve", lambda e: e.tensor_tensor(out=w2[:], in0=gw[:], in1=w1[:], op=ALU.subtract), waits=[t])

        GB_ = 16
        for g0 in range(0, NB, GB_):
            n = min(GB_, NB - g0)
            tk = P.op("pe", lambda e, g0=g0, n=n: e.matmul(bank[0][:, 0:n * NE], ones_b[:],
                                                          Call[:, g0:g0 + n, :].rearrange("p b e -> p (b e)"), start=True, stop=True),
                      waits=[t_route, t_c, bank_free[0]])
            t = P.op("dve", lambda e, g0=g0, n=n: e.tensor_copy(out=colsum[:, g0:g0 + n, :].rearrange("p b e -> p (b e)"),
                                                               in_=bank[0][:, 0:n * NE]), waits=[tk])
            bank_free[0] = t
        t = P.op("dve", lambda e: e.tensor_reduce(out=cnt[:], in_=colsum[:].rearrange("p b e -> p e b"), axis=AX.X, op=ALU.add),
                 waits=[t])
        t = P.op("dve", lambda e: e.tensor_scalar(out=padded[:], in0=cnt[:], scalar1=float(MBLK - 1), scalar2=None, op0=ALU.add),
                 waits=[t])
        t = P.op("dve", lambda e: e.tensor_copy(out=cnt_i[:], in_=padded[:]), waits=[t])
        t = P.op("dve", lambda e: e.tensor_single_scalar(out=cnt_i[:], in_=cnt_i[:], scalar=8, op=ALU.arith_shift_right), waits=[t])
        t = P.op("dve", lambda e: e.tensor_single_scalar(out=cnt_i[:], in_=cnt_i[:], scalar=8, op=ALU.logical_shift_left), waits=[t])
        t = P.op("dve", lambda e: e.tensor_copy(out=padded[:], in_=cnt_i[:]), waits=[t])
        t = P.op("dve", lambda e: e.tensor_copy(out=pe_a[:], in_=padded[:]), waits=[t])
        src, dst = pe_a, pe_b
        sft = 1
        while sft < NE:
            t = P.op("dve", lambda e, src=src, dst=dst, sft=sft: e.tensor_copy(out=dst[:, 0:sft], in_=src[:, 0:sft]), waits=[t])
            t = P.op("dve", lambda e, src=src, dst=dst, sft=sft: e.tensor_tensor(out=dst[:, sft:NE], in0=src[:, sft:NE],
                                                                                in1=src[:, 0:NE - sft], op=ALU.add), waits=[t])
            src, dst = dst, src
            sft *= 2
        pend = src
        t = P.op("dve", lambda e: e.tensor_tensor(out=pstart[:], in0=pend[:], in1=padded[:], op=ALU.subtract), waits=[t])
        t = P.op("dve", lambda e: e.tensor_copy(out=cum[:, 0, :], in_=pstart[:]), waits=[t])
        for b in range(NB):
            t = P.op("dve", lambda e, b=b: e.tensor_tensor(out=cum[:, b + 1, :], in0=cum[:, b, :], in1=colsum[:, b, :], op=ALU.add),
                     waits=[t])
        for g0 in range(0, NB, GB_):
            n = min(GB_, NB - g0)
            tk = None
            for bb in range(n):
                tk = P.op("pe", lambda e, g0=g0, bb=bb: e.matmul(bank[0][:, bb * NE:(bb + 1) * NE], triu_b[:], Call[:, g0 + bb, :],
                                                                start=True, stop=True), waits=[bank_free[0]] if bb == 0 else [],
                          sig=(bb == n - 1))
            t = P.op("dve", lambda e, g0=g0, n=n: e.tensor_tensor(out=dest_all[:, g0:g0 + n, :].rearrange("p b e -> p (b e)"),
                                                                 in0=bank[0][:, 0:n * NE],
                                                                 in1=cum[:, g0:g0 + n, :].rearrange("p b e -> p (b e)"), op=ALU.add),
                     waits=[tk, t])
            bank_free[0] = t
        for k in range(2):
            t = P.op("dve", lambda e, k=k: e.tensor_tensor(out=prod[:].rearrange("p b e -> p (b e)"),
                                                          in0=oh[k][:].rearrange("p b e -> p (b e)"),
                                                          in1=dest_all[:].rearrange("p b e -> p (b e)"), op=ALU.mult), waits=[t])
            t = P.op("dve", lambda e, k=k: e.tensor_reduce(out=dest_f[k][:], in_=prod[:], axis=AX.X, op=ALU.add), waits=[t])
            t = P.op("dve", lambda e, k=k: e.tensor_copy(out=dest_i[k][:], in_=dest_f[k][:]), waits=[t])
            t = P.op("dve", lambda e, k=k: e.tensor_single_scalar(out=tmp_i[0][:], in_=dest_i[k][:], scalar=127, op=ALU.bitwise_and),
                     waits=[t])
            t = P.op("dve", lambda e, k=k: e.tensor_single_scalar(out=tmp_i[1][:], in_=dest_i[k][:], scalar=7,
                                                                 op=ALU.arith_shift_right), waits=[t])
            t = P.op("dve", lambda e: e.tensor_copy(out=tmp_f[0][:], in_=tmp_i[0][:]), waits=[t])
            t = P.op("dve", lambda e: e.tensor_copy(out=tmp_f[1][:], in_=tmp_i[1][:]), waits=[t])
            t = P.op("dve", lambda e: e.scalar_tensor_tensor(out=tmp_f[0][:], in0=tmp_f[0][:], scalar=float(NCH), in1=tmp_f[1][:],
                                                            op0=ALU.mult, op1=ALU.add), waits=[t])
            t = P.op("dve", lambda e, k=k: e.tensor_copy(out=addr_i[k][:], in_=tmp_f[0][:]), waits=[t])
        t_addr = t
        t = P.op("dve", lambda e: e.memset(be_f[:], 0.0), waits=[t])
        for ex in range(NE):
            t = P.op("dve", lambda e, ex=ex: e.scalar_tensor_tensor(out=be_f[:], in0=blkstart[:], scalar=pend[:, ex:ex + 1],
                                                                   in1=be_f[:], op0=ALU.is_ge, op1=ALU.add), waits=[t, t_c])
        t = P.op("dve", lambda e: e.tensor_scalar(out=be_f[:], in0=be_f[:], scalar1=float(NE - 1), scalar2=None, op0=ALU.min),
                 waits=[t])
        t = P.op("dve", lambda e: e.tensor_copy(out=be_i[:], in_=be_f[:]), waits=[t])
        widx_f = sbt("widx_f", [128, NBLK], F32)
        t = P.op("dve", lambda e: e.tensor_copy(out=pk[:, 0:1], in_=tokidx[:, 0:1]), waits=[t, t_c])
        t = P.op("dve", lambda e: e.tensor_scalar(out=widx_f[:], in0=be_f[:], scalar1=128.0, scalar2=pk[:, 0:1],
                                                 op0=ALU.mult, op1=ALU.add), waits=[t])
        t_be = P.op("dve", lambda e: e.tensor_copy(out=widx_i[:], in_=widx_f[:]), waits=[t])
        s_sl = P.sem("p4sl")
        s_sz = P.sem("p4sz")
        s_sr = P.sem("p4sr")
        t = P.op("dve", lambda e: e.memset(zero_i[:], 0), waits=[t_be])
        t = P.dma("sp", lambda e: e.dma_start(out=slot_s, in_=zero_i[:]), s_sz, waits=[t, t_prev])
        slot_flat = slot_s.rearrange("p (c o) -> (p c) o", o=1)
        for b in range(NB):
            for k in range(2):
                P.dma("pool", lambda e, b=b, k=k: e.indirect_dma_start(
                    out=slot_flat, out_offset=bass.IndirectOffsetOnAxis(ap=addr_i[k][:, b:b + 1], axis=0),
                    in_=tokidx[:, b:b + 1], in_offset=None), s_sl, waits=[t, t_addr, t_c])
        t_sc = (s_sl, s_sl.v)
        t_sidx = P.dma("sp", lambda e: e.dma_start(out=sidx[:], in_=slot_s), s_sr, waits=[t_sc])
        dbg("be_i", be_i, t_be)
        dbg("dest_i0", dest_i[0], t_addr)
        dbg("dest_i1", dest_i[1], t_addr)
        dbg("w1", w1, t_w12)
        dbg("w2", w2, t_w12)
        dbg("sidx", sidx, t_sidx)

        t_rs_end = [t_sidx, (s_scr, s_scr.v)] + [(P.esem[k], P.esem[k].v) for k in ("pe", "act", "dve", "pool")]
        P.barrier()
        P.replay()
        rs.close()
        xs = ExitStack()

        def sbx(name, shape, dt):
            return xs.enter_context(nc.sbuf_tensor("sb_" + name, shape, dt))
        wgu = [sbx(f"wgu{i}", [128, 2, 8, EH], BF16) for i in range(2)]
        wd = [sbx(f"wd{i}", [128, 4, D], BF16) for i in range(2)]
        xg = [sbx(f"xg{i}", [128, D], BF16) for i in range(6)]
        xTm = [sbx(f"xTm{i}", [128, 8, 256], BF16) for i in range(2)]
        sgt = [sbx(f"sgt{i}", [128, 256], F32) for i in range(2)]
        hT = [sbx(f"hT{i}", [128, 4, 256], BF16) for i in range(2)]
        ost = [sbx(f"ost{i}", [128, D], F32) for i in range(2)]
        s_wt = [P.sem(f"p4w{i}") for i in range(2)]
        s_xg = [P.sem(f"p4x{i}") for i in range(6)]
        s_os = [P.sem(f"p4o{i}") for i in range(2)]
        w_free = [None, None]
        xg_free = [None] * 6
        xTm_free = [None, None]
        sgt_free = [None, None]
        hT_free = [None, None]
        ost_free = [None, None]
        n_os = 0
        n_sg = 0
        reg_holder = []
        acc = [0]

        def acc_bank():
            b = acc[0] % 7
            acc[0] += 1
            return b

        def load_w(j):
            wb = j % 2
            P.dma("pool", lambda e, wb=wb, j=j: e.indirect_dma_start(
                out=wgu[wb][:].rearrange("p g k f -> p (g k f)"), out_offset=None, in_=wgu_r,
                in_offset=bass.IndirectOffsetOnAxis(ap=widx_i[:, j:j + 1], axis=0)), s_wt[wb],
                waits=[t_be, w_free[wb], t_prep] + (t_rs_end if j < 2 else []))
            tok = P.dma("pool", lambda e, wb=wb, j=j: e.indirect_dma_start(
                out=wd[wb][:].rearrange("p k f -> p (k f)"), out_offset=None, in_=wd_r,
                in_offset=bass.IndirectOffsetOnAxis(ap=widx_i[:, j:j + 1], axis=0)), s_wt[wb])
            return [tok]

        def load_x(j):
            toks = []
            for i in range(2):
                xi = (2 * j + i) % 6
                c = 2 * j + i
                toks.append(P.dma("pool", lambda e, xi=xi, c=c: e.indirect_dma_start(
                    out=xg[xi][:], out_offset=None, in_=h1b_s,
                    in_offset=bass.IndirectOffsetOnAxis(ap=sidx[:, c:c + 1], axis=0)), s_xg[xi],
                    waits=[t_sidx, xg_free[xi]] + (t_rs_end if j < 2 else [])))
            return toks

        t_wl = {0: load_w(0)}
        t_xl = {0: load_x(0)}
        if NBLK > 1:
            t_xl[1] = load_x(1)
        t_xTs = {}

        def emit_T(j):
            xb = j % 2
            t = None
            for i in range(2):
                xi = (2 * j + i) % 6
                tk = None
                for kc in range(8):
                    tk = P.op("pe", lambda e, kc=kc, xi=xi: e.transpose(out=bank[7][:, kc * 128:(kc + 1) * 128],
                                                                       in_=xg[xi][:, kc * 128:(kc + 1) * 128], identity=ident_b[:]),
                              waits=[t_xl[j][i], bank_free[7], t_const] if kc == 0 else [], sig=(kc == 7))
                xg_free[xi] = tk
                t = P.op("act", lambda e, i=i, xb=xb: e.copy(out=xTm[xb][:, :, i * 128:(i + 1) * 128],
                                                            in_=bank[7][:].rearrange("p (k t) -> p k t", k=8)),
                         waits=[tk, xTm_free[xb]] if i == 0 else [tk])
                bank_free[7] = t
            t_xTs[j] = t

        emit_T(0)
        for j in range(NBLK):
            wb = j % 2
            if j + 1 < NBLK:
                t_wl[j + 1] = load_w(j + 1)
            if j + 2 < NBLK:
                t_xl[j + 2] = load_x(j + 2)
            xb = j % 2
            t_xT = t_xTs[j]
            hb = j % 2
            for hc in range(4):
                b = acc_bank()
                tk = None
                for gu in range(2):
                    for kc in range(8):
                        tk = P.op("pe", lambda e, kc=kc, b=b, gu=gu, hc=hc, wb=wb, xb=xb: e.matmul(
                            bank[b][:, gu * 256:(gu + 1) * 256], wgu[wb][:, gu, kc, hc * 128:(hc + 1) * 128], xTm[xb][:, kc, :],
                            start=(kc == 0), stop=(kc == 7)),
                            waits=[t_xT, bank_free[b]] + t_wl[j] if (kc == 0 and gu == 0) else [], sig=(kc == 7 and gu == 1))
                r = n_sg % 2
                n_sg += 1
                t = P.op("act", lambda e, b=b, r=r: e.activation(out=sgt[r][:], in_=bank[b][:, 0:256], func=AF.Silu),
                         waits=[tk, sgt_free[r]])
                t = P.op("dve", lambda e, b=b, r=r, hc=hc, hb=hb: e.tensor_tensor(out=hT[hb][:, hc, :], in0=sgt[r][:],
                                                                                in1=bank[b][:, 256:512], op=ALU.mult),
                         waits=[t, hT_free[hb]] if hc == 0 else [t])
                sgt_free[r] = t
                bank_free[b] = t
            xTm_free[xb] = tk
            t_hT = t
            if j + 1 < NBLK:
                emit_T(j + 1)
            for i in range(2):
                r = n_os % 2
                n_os += 1
                t = None
                for oh_ in range(2):
                    b = acc_bank()
                    tk = None
                    for hc in range(4):
                        tk = P.op("pe", lambda e, hc=hc, b=b, i=i, oh_=oh_, hb=hb, wb=wb: e.matmul(
                            bank[b][:], hT[hb][:, hc, i * 128:(i + 1) * 128], wd[wb][:, hc, oh_ * 512:(oh_ + 1) * 512],
                            start=(hc == 0), stop=(hc == 3)), waits=[t_hT, bank_free[b]] if hc == 0 else [], sig=(hc == 3))
                    if oh_ == 0:
                        t = P.op("act", lambda e, b=b, r=r: e.copy(out=ost[r][:, 0:512], in_=bank[b][:]), waits=[tk, ost_free[r]])
                    else:
                        t = P.op("dve", lambda e, b=b, r=r: e.tensor_copy(out=ost[r][:, 512:1024], in_=bank[b][:]),
                                 waits=[tk, t, ost_free[r]])
                    bank_free[b] = t
                c = 2 * j + i
                ost_free[r] = P.dma("sp", lambda e, r=r, c=c: e.dma_start(out=out_s[c * 128:(c + 1) * 128, :], in_=ost[r][:]), s_os[r],
                                    waits=[t, (P.esem["act"], P.esem["act"].v)])
            hT_free[hb] = tk
            w_free[wb] = tk
        t_exp_done = [x for x in ost_free if x is not None] + [(P.esem[k], P.esem[k].v) for k in ("pe", "act", "dve", "pool")]
        P.barrier()
        P.replay()
        xs.close()

        ln2g = sb("ln2g", [128, D], F32)
        ln2b = sb("ln2b", [128, D], F32)
        NG = 4
        o1 = [sb(f"o1_{i}", [128, D], F32) for i in range(NG)]
        o2 = [sb(f"o2_{i}", [128, D], F32) for i in range(NG)]
        h1t = [sb(f"h1c{i}", [128, D], F32) for i in range(NG)]
        yb = [sb(f"yb{i}", [128, D], F32) for i in range(2)]
        zo = [sb(f"zo{i}", [128, D], F32) for i in range(2)]
        st12 = sb("st12_4", [128, 2, 6], F32)
        mv = sb("mv4", [128, 2], F32)
        rr = sb("rr4", [128, 2], F32)
        neghalf = sb("neghalf4", [128, 1], F32)
        s_l2 = P.sem("p4l2")
        s_cin = [P.sem(f"p4ci{i}") for i in range(4)]
        s_co = [P.sem(f"p4co{i}") for i in range(2)]
        s_ch = [P.sem(f"p4ch{i}") for i in range(4)]
        P.dma("sp", lambda e: e.dma_start(out=ln2g[:], in_=A["ln2_g"][0].partition_broadcast(128)), s_l2, waits=t_exp_done)
        P.dma("sp", lambda e: e.dma_start(out=ln2b[:], in_=A["ln2_b"][0].partition_broadcast(128)), s_l2)
        t_l2 = (s_l2, s_l2.v)
        P.op("dve", lambda e: e.memset(neghalf[:], -0.5), waits=t_exp_done)
        cin_free = [None] * NG
        zo_free = [None, None]
        yb_free = [None, None]
        t_ins = {}

        def issue_gather(b):
            g = b % NG
            P.dma("pool", lambda e, b=b, g=g: e.indirect_dma_start(
                out=o1[g][:], out_offset=None, in_=out_s, in_offset=bass.IndirectOffsetOnAxis(ap=dest_i[0][:, b:b + 1], axis=0)),
                s_cin[g], waits=t_exp_done + [cin_free[g]])
            ta = P.dma("pool", lambda e, b=b, g=g: e.indirect_dma_start(
                out=o2[g][:], out_offset=None, in_=out_s, in_offset=bass.IndirectOffsetOnAxis(ap=dest_i[1][:, b:b + 1], axis=0)),
                s_cin[g])
            tb_ = P.dma("sp", lambda e, b=b, g=g: e.dma_start(out=h1t[g][:], in_=h1_s[b * 128:(b + 1) * 128, :]), s_ch[g],
                        waits=[cin_free[g], t_prev] + t_exp_done)
            t_ins[b] = (ta, tb_)

        for b in range(min(NG - 1, NB)):
            issue_gather(b)
        for b in range(NB):
            r = b % 2
            g = b % NG
            if b + NG - 1 < NB:
                issue_gather(b + NG - 1)
            t_in1, t_in2 = t_ins[b]
            t = P.op("act", lambda e, b=b, r=r, g=g: e.activation(out=yb[r][:], in_=o1[g][:], func=AF.Identity, scale=w1[:, b:b + 1]),
                     waits=[t_in1, t_in2, t_w12, zo_free[r], yb_free[r]])
            t = P.op("dve", lambda e, b=b, r=r, g=g: e.scalar_tensor_tensor(out=yb[r][:], in0=o2[g][:], scalar=w2[:, b:b + 1],
                                                                           in1=yb[r][:], op0=ALU.mult, op1=ALU.add), waits=[t])
            t = P.op("dve", lambda e, r=r, g=g: e.scalar_tensor_tensor(out=yb[r][:], in0=h1t[g][:], scalar=ALPHA, in1=yb[r][:],
                                                                      op0=ALU.mult, op1=ALU.add), waits=[t])
            cin_free[g] = t
            for half in range(2):
                t = P.op("dve", lambda e, half=half, r=r: e.bn_stats(out=st12[:, half, :], in_=yb[r][:, half * 512:(half + 1) * 512]),
                         waits=[t])
            t = P.op("dve", lambda e: e.bn_aggr(out=mv[:], in_=st12[:].rearrange("p a b -> p (a b)")), waits=[t])
            t = P.op("dve", lambda e: e.tensor_scalar(out=rr[:, 0:1], in0=mv[:, 1:2], scalar1=LN_EPS, scalar2=None, op0=ALU.add),
                     waits=[t])
            t = P.op("act", lambda e: e.activation(out=rr[:, 0:1], in_=rr[:, 0:1], func=AF.Sqrt), waits=[t])
            t = P.op("dve", lambda e: e.reciprocal(out=rr[:, 0:1], in_=rr[:, 0:1]), waits=[t])
            t = P.op("dve", lambda e: e.scalar_tensor_tensor(out=rr[:, 1:2], in0=mv[:, 0:1], scalar=-1.0, in1=rr[:, 0:1],
                                                            op0=ALU.mult, op1=ALU.mult), waits=[t])
            t = P.op("act", lambda e, r=r: e.activation(out=yb[r][:], in_=yb[r][:], func=AF.Identity, bias=rr[:, 1:2],
                                                       scale=rr[:, 0:1]), waits=[t])
            t = P.op("dve", lambda e, r=r: e.tensor_tensor(out=yb[r][:], in0=yb[r][:], in1=ln2g[:], op=ALU.mult), waits=[t, t_l2])
            t = P.op("pool", lambda e, r=r: e.tensor_tensor(out=zo[r][:], in0=yb[r][:], in1=ln2b[:], op=ALU.add), waits=[t, zo_free[r]])
            yb_free[r] = t
            zo_free[r] = P.dma("sp", lambda e, b=b, r=r: e.dma_start(out=out[b * 128:(b + 1) * 128, :], in_=zo[r][:]), s_co[r], waits=[t])
        t_end = [x for x in zo_free if x is not None]
        P.op("sp", lambda e: e.nop(), waits=t_end, sig=False)
        P.replay()
    return t_end

def kernel(**inputs):
    S = 8192
    n = 8
    x = np.ascontiguousarray(np.asarray(inputs["x"], dtype=np.float32))
    nc = build(S)
    consts = make_consts(S)
    shared = {k: np.ascontiguousarray(np.asarray(inputs[k], dtype=np.float32)) for k in IN_SHAPES}
    shared.update(consts)
    in_maps = []
    for i in range(n):
        m = dict(shared)
        m["x"] = x[i]
        in_maps.append(m)
    res = run_bass_kernel_spmd(nc, in_maps, core_ids=list(range(n)))
    return np.stack([np.asarray(r["out"], dtype=np.float32) for r in res.results], axis=0)
```
